# Optimizing a Trainium2 kernel written in Bass

```python
import math
import jax
import jax.numpy as jnp
from jax import lax
import numpy as np


D_MODEL = 1024
BATCH = 2
SEQ = 8192
DEPTH = 2

GRID_W = 64
CTX_LEN = 256
HEAD_DIM = 64
N_BRANCH = 4
BRANCH_W = 512
Q_BLOCK = 128
ROPE_BASE = 10000.0
EPS = 1e-6
NEG = -1e30

WIN_HEADS = 8
WIN_KV_HEADS = 2
WIN_RADIUS = 128
WIN_BLOCK = 128
DIF_HEADS = 4
DIF_QK_DIM = 64
DIF_V_DIM = 128
NAT_HEADS = 8
NAT_WIN_ROWS = 8
NAT_WIN_COLS = 16
MLA_HEADS = 8
MLA_NOPE = 64
MLA_ROPE = 32
MLA_V = 64
MLA_Q_LORA = 256
MLA_KV_LORA = 128
N_EXPERTS = 16
N_GROUPS = 4
TOP_K = 2
D_EXPERT = 512
MOE_BLOCK = 256

IN_SIZES = (WIN_HEADS * HEAD_DIM, WIN_KV_HEADS * HEAD_DIM, WIN_KV_HEADS * HEAD_DIM,
            DIF_HEADS * 2 * DIF_QK_DIM, DIF_HEADS * 2 * DIF_QK_DIM, DIF_HEADS * DIF_V_DIM,
            NAT_HEADS * HEAD_DIM, NAT_HEADS * HEAD_DIM, NAT_HEADS * HEAD_DIM,
            MLA_Q_LORA, MLA_KV_LORA + MLA_ROPE, N_BRANCH * D_MODEL)
D_IN = sum(IN_SIZES)

kernel_name = 'hybrid_gated_mixers_grouped_moe_diffusion_block'


def rms_norm(x, g):
    xf = x.astype(jnp.float32)
    y = xf * lax.rsqrt(jnp.mean(xf * xf, axis=-1, keepdims=True) + EPS)
    return (y * g.astype(jnp.float32)).astype(x.dtype)


def rope_cos_sin(n_tok, dim, dtype):
    t = jnp.arange(n_tok)
    rows = (t // GRID_W).astype(jnp.float32)
    cols = (t % GRID_W).astype(jnp.float32)
    quarter = dim // 4
    inv_freq = jnp.exp(-math.log(ROPE_BASE) * jnp.arange(quarter, dtype=jnp.float32) / quarter)
    ang = jnp.concatenate([rows[:, None] * inv_freq, cols[:, None] * inv_freq], axis=-1)
    return jnp.cos(ang).astype(dtype), jnp.sin(ang).astype(dtype)


def apply_rope(x, cos, sin):
    half = x.shape[-1] // 2
    x1, x2 = x[..., :half], x[..., half:]
    c = cos[:, None, :]
    s = sin[:, None, :]
    return jnp.concatenate([x1 * c - x2 * s, x1 * s + x2 * c], axis=-1)


def joint_softmax(parts):
    m = parts[0].max(axis=-1, keepdims=True)
    for p in parts[1:]:
        m = jnp.maximum(m, p.max(axis=-1, keepdims=True))
    ex = [jnp.exp(p - m) for p in parts]
    z = ex[0].sum(axis=-1, keepdims=True)
    for e in ex[1:]:
        z = z + e.sum(axis=-1, keepdims=True)
    return [e / z for e in ex]


def sweep_query_blocks(fn, q):
    b, t = q.shape[:2]
    nb = t // Q_BLOCK
    qb = jnp.moveaxis(q.reshape((b, nb, Q_BLOCK) + q.shape[2:]), 1, 0)
    out = lax.map(fn, qb)
    return jnp.moveaxis(out, 0, 1).reshape((b, t) + out.shape[3:])


def flat_heads(y):
    return y.reshape(y.shape[0], y.shape[1], -1)


def dense_attention(q, k, v):
    scale = q.shape[-1] ** -0.5

    def block(qb):
        logits = jnp.einsum('bqhd,bkhd->bhqk', qb, k).astype(jnp.float32) * scale
        p = jax.nn.softmax(logits, axis=-1)
        return jnp.einsum('bhqk,bkhd->bqhd', p.astype(v.dtype), v)

    return sweep_query_blocks(block, q)


def diff_attention(q, k, v, lam):
    scale = q.shape[-1] ** -0.5

    def block(qb):
        logits = jnp.einsum('bqhid,bkhid->bhiqk', qb, k).astype(jnp.float32) * scale
        p = jax.nn.softmax(logits, axis=-1)
        a = p[:, :, 0] - lam * p[:, :, 1]
        return jnp.einsum('bhqk,bkhd->bqhd', a.astype(v.dtype), v)

    return sweep_query_blocks(block, q)


def window_attention(q, k, v, kc, vc, sink):
    b, s, _, hd = q.shape
    g = WIN_HEADS // WIN_KV_HEADS
    nb = s // WIN_BLOCK
    scale = hd ** -0.5
    qb = q.reshape(b, nb, WIN_BLOCK, WIN_KV_HEADS, g, hd)

    def band(t):
        tp = jnp.pad(t, ((0, 0), (WIN_BLOCK, WIN_BLOCK), (0, 0), (0, 0)))
        tp = tp.reshape(b, nb + 2, WIN_BLOCK, WIN_KV_HEADS, hd)
        return jnp.concatenate([tp[:, :-2], tp[:, 1:-1], tp[:, 2:]], axis=2)

    kb, vb = band(k), band(v)
    qi = jnp.arange(WIN_BLOCK)
    kj = jnp.arange(3 * WIN_BLOCK)
    rel = kj[None, :] - WIN_BLOCK - qi[:, None]
    key_pos = (jnp.arange(nb)[:, None] - 1) * WIN_BLOCK + kj[None, :]
    valid = (jnp.abs(rel) <= WIN_RADIUS)[None] & ((key_pos >= 0) & (key_pos < s))[:, None, :]
    s_band = jnp.einsum('bnqkgd,bnjkd->bnkgqj', qb, kb).astype(jnp.float32) * scale
    s_band = jnp.where(valid[None, :, None, None], s_band, NEG)
    s_ctx = jnp.einsum('bnqkgd,bckd->bnkgqc', qb, kc).astype(jnp.float32) * scale
    s_sink = jnp.broadcast_to(sink.astype(jnp.float32).reshape(1, 1, WIN_KV_HEADS, g, 1, 1),
                              s_ctx.shape[:-1] + (1,))
    p_band, p_ctx, _ = joint_softmax([s_band, s_ctx, s_sink])
    out = (jnp.einsum('bnkgqj,bnjkd->bnqkgd', p_band.astype(v.dtype), vb)
           + jnp.einsum('bnkgqc,bckd->bnqkgd', p_ctx.astype(v.dtype), vc))
    return out.reshape(b, s, WIN_HEADS * hd)


def window_attention_context(qc, kc, vc, sink):
    b, n, _, hd = qc.shape
    g = WIN_HEADS // WIN_KV_HEADS
    scale = hd ** -0.5
    qg = qc.reshape(b, n, WIN_KV_HEADS, g, hd)
    sc = jnp.einsum('bqkgd,bckd->bkgqc', qg, kc).astype(jnp.float32) * scale
    s_sink = jnp.broadcast_to(sink.astype(jnp.float32).reshape(1, WIN_KV_HEADS, g, 1, 1), sc.shape[:-1] + (1,))
    p, _ = joint_softmax([sc, s_sink])
    out = jnp.einsum('bkgqc,bckd->bqkgd', p.astype(vc.dtype), vc)
    return out.reshape(b, n, WIN_HEADS * hd)


def neighbourhood_attention(q, k, v, kc, vc, rpb):
    b, s, h, hd = q.shape
    rows = s // GRID_W
    wr = min(NAT_WIN_ROWS, rows)
    wc = NAT_WIN_COLS
    scale = hd ** -0.5
    r = jnp.arange(rows)
    row_idx = jnp.clip(r - wr // 2, 0, rows - wr)[:, None] + jnp.arange(wr)[None, :]
    col = jnp.arange(GRID_W)
    col_start = jnp.clip(col - wc // 2, 0, GRID_W - wc)
    col_ok = (col[None, :] >= col_start[:, None]) & (col[None, :] < col_start[:, None] + wc)
    qg = q.reshape(b, rows, GRID_W, h, hd)
    kg = k.reshape(b, rows, GRID_W, h, hd)[:, row_idx]
    vg = v.reshape(b, rows, GRID_W, h, hd)[:, row_idx]
    d_row = row_idx - r[:, None] + (NAT_WIN_ROWS - 1)
    d_col = jnp.clip(col[None, :] - col[:, None] + (wc - 1), 0, 2 * wc - 2)
    bias = rpb[:, d_row[:, None, :, None], d_col[None, :, None, :]].astype(jnp.float32)
    s_nb = jnp.einsum('brqhd,brakhd->bhrqak', qg, kg).astype(jnp.float32) * scale + bias[None]
    s_nb = jnp.where(col_ok[:, None, :], s_nb, NEG).reshape(b, h, rows, GRID_W, wr * GRID_W)
    s_cx = jnp.einsum('brqhd,bchd->bhrqc', qg, kc).astype(jnp.float32) * scale
    p_nb, p_cx = joint_softmax([s_nb, s_cx])
    p_nb = p_nb.reshape(b, h, rows, GRID_W, wr, GRID_W).astype(v.dtype)
    out = (jnp.einsum('bhrqak,brakhd->brqhd', p_nb, vg)
           + jnp.einsum('bhrqc,bchd->brqhd', p_cx.astype(v.dtype), vc))
    return out.reshape(b, s, h * hd)


def project_heads(proj, rope, win_q_norm, win_k_norm, dif_q_norm, dif_k_norm, nat_q_norm, nat_k_norm,
                  mla_q_a_norm, mla_wq_b, mla_kv_a_norm, mla_wkv_b, mla_q_norm, mla_k_norm):
    split_at = np.cumsum(IN_SIZES)[:-1].tolist()
    (wq, wk, wv, dq, dk, dv, nq, nk, nv, mqa, mkva, gates) = jnp.split(proj, split_at, axis=-1)
    b, t = proj.shape[:2]
    wq = rms_norm(wq.reshape(b, t, WIN_HEADS, HEAD_DIM), win_q_norm)
    wk = rms_norm(wk.reshape(b, t, WIN_KV_HEADS, HEAD_DIM), win_k_norm)
    wv = wv.reshape(b, t, WIN_KV_HEADS, HEAD_DIM)
    dq = rms_norm(dq.reshape(b, t, 2 * DIF_HEADS, DIF_QK_DIM), dif_q_norm)
    dk = rms_norm(dk.reshape(b, t, 2 * DIF_HEADS, DIF_QK_DIM), dif_k_norm)
    dv = dv.reshape(b, t, DIF_HEADS, DIF_V_DIM)
    nq = rms_norm(nq.reshape(b, t, NAT_HEADS, HEAD_DIM), nat_q_norm)
    nk = rms_norm(nk.reshape(b, t, NAT_HEADS, HEAD_DIM), nat_k_norm)
    nv = nv.reshape(b, t, NAT_HEADS, HEAD_DIM)
    mq = (rms_norm(mqa, mla_q_a_norm) @ mla_wq_b).reshape(b, t, MLA_HEADS, MLA_NOPE + MLA_ROPE)
    ckv, k_rope = mkva[..., :MLA_KV_LORA], mkva[..., MLA_KV_LORA:]
    kv = (rms_norm(ckv, mla_kv_a_norm) @ mla_wkv_b).reshape(b, t, MLA_HEADS, MLA_NOPE + MLA_V)
    mk = jnp.concatenate([kv[..., :MLA_NOPE],
                          jnp.broadcast_to(k_rope[:, :, None, :], (b, t, MLA_HEADS, MLA_ROPE))], axis=-1)
    mv = kv[..., MLA_NOPE:]
    mq = rms_norm(mq, mla_q_norm)
    mk = rms_norm(mk, mla_k_norm)
    if rope is not None:
        cos_h, sin_h, cos_m, sin_m = rope
        wq = apply_rope(wq, cos_h, sin_h)
        wk = apply_rope(wk, cos_h, sin_h)
        dq = apply_rope(dq, cos_h, sin_h)
        dk = apply_rope(dk, cos_h, sin_h)
        mq = jnp.concatenate([mq[..., :MLA_NOPE], apply_rope(mq[..., MLA_NOPE:], cos_m, sin_m)], axis=-1)
        mk = jnp.concatenate([mk[..., :MLA_NOPE], apply_rope(mk[..., MLA_NOPE:], cos_m, sin_m)], axis=-1)
    dq = dq.reshape(b, t, DIF_HEADS, 2, DIF_QK_DIM)
    dk = dk.reshape(b, t, DIF_HEADS, 2, DIF_QK_DIM)
    return (wq, wk, wv, dq, dk, dv, nq, nk, nv, mq, mk, mv, gates)


def merge_branches(ys, gates, w_branch, w_out):
    b, t = gates.shape[:2]
    yb = jnp.einsum('btnw,nwd->btnd', jnp.stack(ys, axis=2), w_branch)
    g = jax.nn.sigmoid(gates.reshape(b, t, N_BRANCH, D_MODEL))
    return (g * yb).sum(axis=2) @ w_out


def moe(tok, router_w, router_b, w1, w3, w2):
    n, d = tok.shape
    per_group = N_EXPERTS // N_GROUPS
    scores = jax.nn.sigmoid((tok @ router_w).astype(jnp.float32))
    biased = scores + router_b.astype(jnp.float32)
    group_score = lax.top_k(biased.reshape(n, N_GROUPS, per_group), 2)[0].sum(-1)
    group = jnp.argmax(group_score, axis=-1)
    in_group = (jnp.arange(N_EXPERTS) // per_group)[None, :] == group[:, None]
    _, idx = lax.top_k(jnp.where(in_group, biased, -jnp.inf), TOP_K)
    wts = jnp.take_along_axis(scores, idx, axis=-1)
    wts = wts / wts.sum(-1, keepdims=True)
    flat_e = idx.reshape(-1).astype(jnp.int32)
    flat_w = wts.reshape(-1)
    flat_t = jnp.repeat(jnp.arange(n, dtype=jnp.int32), TOP_K)
    order = jnp.argsort(flat_e)
    se, st, sw = flat_e[order], flat_t[order], flat_w[order]
    counts = jnp.bincount(flat_e, length=N_EXPERTS)
    padded = (counts + MOE_BLOCK - 1) // MOE_BLOCK * MOE_BLOCK
    start = jnp.cumsum(counts) - counts
    pad_end = jnp.cumsum(padded)
    pad_start = pad_end - padded
    dest = pad_start[se] + jnp.arange(n * TOP_K) - start[se]
    n_blk = (n * TOP_K + N_EXPERTS * (MOE_BLOCK - 1) + MOE_BLOCK - 1) // MOE_BLOCK
    cap = n_blk * MOE_BLOCK
    buf_t = jnp.full((cap,), n, jnp.int32).at[dest].set(st)
    buf_w = jnp.zeros((cap,), tok.dtype).at[dest].set(sw.astype(tok.dtype))
    blk_e = jnp.minimum(jnp.searchsorted(pad_end, jnp.arange(n_blk) * MOE_BLOCK, side='right'), N_EXPERTS - 1)
    xs = jnp.concatenate([tok, jnp.zeros((1, d), tok.dtype)], axis=0)[buf_t].reshape(n_blk, MOE_BLOCK, d)

    def expert_block(args):
        xb, e = args
        return (jax.nn.silu(xb @ w1[e]) * (xb @ w3[e])) @ w2[e]

    ys = lax.map(expert_block, (xs, blk_e)).reshape(cap, d) * buf_w[:, None]
    return jnp.zeros((n + 1, d), tok.dtype).at[buf_t].add(ys)[:n]


def trunk_layer(x, xc, c, c_ctx, rope, lam_init, want_ctx,
                ada_w, ada_b, norm1_g, norm2_g, w_in,
                win_q_norm, win_k_norm, win_sink,
                dif_q_norm, dif_k_norm, dif_lambda, dif_subln,
                nat_q_norm, nat_k_norm, nat_rpb,
                mla_q_a_norm, mla_wq_b, mla_kv_a_norm, mla_wkv_b, mla_q_norm, mla_k_norm,
                w_branch, w_out, router_w, router_b, moe_w1, moe_w3, moe_w2):
    b, s, d = x.shape
    mod = jax.nn.silu(c) @ ada_w + ada_b
    mod_c = jax.nn.silu(c_ctx) @ ada_w + ada_b
    sh1, sc1, g1, sh2, sc2, g2 = [m[:, None, :] for m in jnp.split(mod, 6, axis=-1)]
    sh1c, sc1c, g1c, sh2c, sc2c, g2c = jnp.split(mod_c, 6, axis=-1)

    h = rms_norm(x, norm1_g) * (1.0 + sc1) + sh1
    hc = rms_norm(xc, norm1_g) * (1.0 + sc1c) + sh1c
    head_params = (win_q_norm, win_k_norm, dif_q_norm, dif_k_norm, nat_q_norm, nat_k_norm,
                   mla_q_a_norm, mla_wq_b, mla_kv_a_norm, mla_wkv_b, mla_q_norm, mla_k_norm)
    (wq, wk, wv, dq, dk, dv, nq, nk, nv, mq, mk, mv, gates) = project_heads(h @ w_in, rope, *head_params)
    (wqc, wkc, wvc, dqc, dkc, dvc, nqc, nkc, nvc, mqc, mkc, mvc, gates_c) = project_heads(hc @ w_in, None, *head_params)

    lam_f = dif_lambda.astype(jnp.float32)
    lam = jnp.exp(jnp.sum(lam_f[0] * lam_f[1])) - jnp.exp(jnp.sum(lam_f[2] * lam_f[3])) + lam_init

    def dif_out(y):
        return flat_heads(rms_norm(y, dif_subln) * (1.0 - lam_init))

    ys = [
        window_attention(wq, wk, wv, wkc, wvc, win_sink),
        dif_out(diff_attention(dq, jnp.concatenate([dkc, dk], axis=1), jnp.concatenate([dvc, dv], axis=1), lam)),
        neighbourhood_attention(nq, nk, nv, nkc, nvc, nat_rpb),
        flat_heads(dense_attention(mq, jnp.concatenate([mkc, mk], axis=1), jnp.concatenate([mvc, mv], axis=1))),
    ]
    x = x + g1 * merge_branches(ys, gates, w_branch, w_out)
    h2 = rms_norm(x, norm2_g) * (1.0 + sc2) + sh2
    tokens = h2.reshape(b * s, d)
    if want_ctx:
        ysc = [
            window_attention_context(wqc, wkc, wvc, win_sink),
            dif_out(diff_attention(dqc, dkc, dvc, lam)),
            flat_heads(dense_attention(nqc, nkc, nvc)),
            flat_heads(dense_attention(mqc, mkc, mvc)),
        ]
        xc = xc + g1c * merge_branches(ysc, gates_c, w_branch, w_out)
        h2c = rms_norm(xc, norm2_g) * (1.0 + sc2c) + sh2c
        tokens = jnp.concatenate([tokens, h2c.reshape(-1, d)], axis=0)
    f = moe(tokens, router_w, router_b, moe_w1, moe_w3, moe_w2)
    x = x + g2 * f[: b * s].reshape(b, s, d)
    if want_ctx:
        xc = xc + g2c * f[b * s:].reshape(xc.shape)
    return x, xc


def _normal(k, shape, std):
    return jax.random.normal(k, shape, jnp.float32) * std


def setup_inputs(seed: int = 0) -> dict:
    key = jax.random.key(seed)
    k = jax.random.split(key, 32)
    L, D = DEPTH, D_MODEL
    return {
        'x': _normal(k[0], (BATCH, SEQ, D), 1.0),
        'c': _normal(k[1], (BATCH, D), 1.0),
        'ctx': _normal(k[2], (BATCH, CTX_LEN, D), 1.0),
        'c_ctx': _normal(k[3], (D,), 1.0),
        'ada_w': _normal(k[4], (L, D, 6 * D), 0.5 * D ** -0.5),
        'ada_b': _normal(k[5], (L, 6 * D), 0.02),
        'norm1_g': 1.0 + _normal(k[6], (L, D), 0.05),
        'norm2_g': 1.0 + _normal(k[7], (L, D), 0.05),
        'w_in': _normal(k[8], (L, D, D_IN), D ** -0.5),
        'win_q_norm': 1.0 + _normal(k[9], (L, HEAD_DIM), 0.05),
        'win_k_norm': 1.0 + _normal(k[10], (L, HEAD_DIM), 0.05),
        'win_sink': _normal(k[11], (L, WIN_HEADS), 0.5),
        'dif_q_norm': 1.0 + _normal(k[12], (L, DIF_QK_DIM), 0.05),
        'dif_k_norm': 1.0 + _normal(k[13], (L, DIF_QK_DIM), 0.05),
        'dif_lambda': _normal(k[14], (L, 4, DIF_QK_DIM), 0.1),
        'dif_subln': 1.0 + _normal(k[15], (L, DIF_V_DIM), 0.05),
        'nat_q_norm': 1.0 + _normal(k[16], (L, HEAD_DIM), 0.05),
        'nat_k_norm': 1.0 + _normal(k[17], (L, HEAD_DIM), 0.05),
        'nat_rpb': _normal(k[18], (L, NAT_HEADS, 2 * NAT_WIN_ROWS - 1, 2 * NAT_WIN_COLS - 1), 0.1),
        'mla_q_a_norm': 1.0 + _normal(k[19], (L, MLA_Q_LORA), 0.05),
        'mla_wq_b': _normal(k[20], (L, MLA_Q_LORA, MLA_HEADS * (MLA_NOPE + MLA_ROPE)), MLA_Q_LORA ** -0.5),
        'mla_kv_a_norm': 1.0 + _normal(k[21], (L, MLA_KV_LORA), 0.05),
        'mla_wkv_b': _normal(k[22], (L, MLA_KV_LORA, MLA_HEADS * (MLA_NOPE + MLA_V)), MLA_KV_LORA ** -0.5),
        'mla_q_norm': 1.0 + _normal(k[23], (L, MLA_NOPE + MLA_ROPE), 0.05),
        'mla_k_norm': 1.0 + _normal(k[24], (L, MLA_NOPE + MLA_ROPE), 0.05),
        'w_branch': _normal(k[25], (L, N_BRANCH, BRANCH_W, D), BRANCH_W ** -0.5),
        'w_out': _normal(k[26], (L, D, D), D ** -0.5),
        'router_w': _normal(k[27], (D, N_EXPERTS), D ** -0.5),
        'router_b': _normal(k[28], (N_EXPERTS,), 0.01),
        'moe_w1': _normal(k[29], (L, N_EXPERTS, D, D_EXPERT), D ** -0.5),
        'moe_w3': _normal(k[30], (L, N_EXPERTS, D, D_EXPERT), D ** -0.5),
        'moe_w2': _normal(k[31], (L, N_EXPERTS, D_EXPERT, D), D_EXPERT ** -0.5),
    }


def reference(x, c, ctx, c_ctx, ada_w, ada_b, norm1_g, norm2_g, w_in,
              win_q_norm, win_k_norm, win_sink,
              dif_q_norm, dif_k_norm, dif_lambda, dif_subln,
              nat_q_norm, nat_k_norm, nat_rpb,
              mla_q_a_norm, mla_wq_b, mla_kv_a_norm, mla_wkv_b, mla_q_norm, mla_k_norm,
              w_branch, w_out, router_w, router_b, moe_w1, moe_w3, moe_w2):
    s = x.shape[1]
    rope = rope_cos_sin(s, HEAD_DIM, x.dtype) + rope_cos_sin(s, MLA_ROPE, x.dtype)
    xc = ctx
    for l in range(DEPTH):
        lam_init = 0.8 - 0.6 * math.exp(-0.3 * l)
        x, xc = trunk_layer(
            x, xc, c, c_ctx, rope, lam_init, l < DEPTH - 1,
            ada_w[l], ada_b[l], norm1_g[l], norm2_g[l], w_in[l],
            win_q_norm[l], win_k_norm[l], win_sink[l],
            dif_q_norm[l], dif_k_norm[l], dif_lambda[l], dif_subln[l],
            nat_q_norm[l], nat_k_norm[l], nat_rpb[l],
            mla_q_a_norm[l], mla_wq_b[l], mla_kv_a_norm[l], mla_wkv_b[l], mla_q_norm[l], mla_k_norm[l],
            w_branch[l], w_out[l], router_w, router_b, moe_w1[l], moe_w3[l], moe_w2[l])
    return x
```

```python
import numpy as np
import ml_dtypes
import concourse.bass as bass
import concourse.mybir as mybir
from concourse.bass_utils import run_bass_kernel_spmd

F32 = mybir.dt.float32
BF16 = mybir.dt.bfloat16
I32 = mybir.dt.int32
AF = mybir.ActivationFunctionType
ALU = mybir.AluOpType
AX = mybir.AxisListType


class Buf:
    __slots__ = ("t", "w", "r", "name")

    def __init__(self, t, name=""):
        self.t = t
        self.w = None
        self.r = {}
        self.name = name


class _Eng:
    def __init__(self, name, eng, sem):
        self.name, self.eng, self.sem = name, eng, sem
        self.count = 0
        self.seen = {}
        self.dq = []
        self.dn = 0


class FW:
    NDMA = 6

    def __init__(self, nc):
        self.nc = nc
        self.stack = None
        self.E = {}
        self.out_tokens = []

    def __enter__(self):
        from contextlib import ExitStack
        self.stack = ExitStack()
        self.stack.__enter__()
        nc = self.nc
        for name, eng in (("pe", nc.tensor), ("act", nc.scalar), ("dve", nc.vector),
                          ("pool", nc.gpsimd), ("sp", nc.sync)):
            sem = self.stack.enter_context(nc.semaphore("s_" + name))
            self.E[name] = _Eng(name, eng, sem)
        for q in ("sp", "act", "pool"):
            for i in range(self.NDMA):
                self.E[q].dq.append(self.stack.enter_context(nc.semaphore("d_%s%d" % (q, i))))
        return self

    def __exit__(self, *a):
        return self.stack.__exit__(*a)

    nalloc = 0

    def sb(self, name, shape, dt):
        self.nalloc += 1
        name = "%s_u%d" % (name, self.nalloc)
        return Buf(self.stack.enter_context(self.nc.sbuf_tensor(name, list(shape), dt)), name)

    def ps(self, name, shape, dt=F32):
        self.nalloc += 1
        name = "%s_u%d" % (name, self.nalloc)
        return Buf(self.stack.enter_context(self.nc.psum_tensor(name, list(shape), dt)), name)

    def scope(self):
        fw = self

        class _S:
            def __enter__(s):
                from contextlib import ExitStack
                s.prev = fw.stack
                fw.stack = ExitStack()
                fw.stack.__enter__()
                return s

            def __exit__(s, *a):
                fw.barrier()
                r = fw.stack.__exit__(*a)
                fw.stack = s.prev
                return r
        return _S()

    def _wait(self, E, tok):
        sem, val = tok
        if E.name == "pe" and sem is E.sem:
            return
        key = id(sem)
        if E.seen.get(key, 0) >= val:
            return
        E.eng.wait_ge(sem, val)
        E.seen[key] = val

    def _deps(self, E, reads, writes):
        toks = []
        for b in reads:
            if b.w is not None:
                toks.append(b.w)
        for b in writes:
            if b.w is not None:
                toks.append(b.w)
            toks.extend(b.r.values())
        for tok in toks:
            self._wait(E, tok)

    def _mark(self, tok, reads, writes):
        key = id(tok[0])
        for b in reads:
            old = b.r.get(key)
            if old is None or old[1] < tok[1]:
                b.r[key] = tok
        for b in writes:
            b.w = tok
            b.r = {}

    nops = 0
    maxops = 10 ** 9
    rec = None

    def op(self, en, fn, reads=(), writes=()):
        if self.rec is not None:
            self.rec.append((0, en, fn, tuple(reads), tuple(writes)))
            return
        self.nops += 1
        if self.nops > self.maxops:
            return
        E = self.E[en]
        self._deps(E, reads, writes)
        ins = fn(E.eng)
        E.count += 1
        ins.then_inc(E.sem, 1)
        self._mark((E.sem, E.count), reads, writes)

    def dma(self, q, out_ap, in_ap, reads=(), writes=(), out=False):
        if self.rec is not None:
            self.rec.append((1, q, out_ap, in_ap, tuple(reads), tuple(writes), out))
            return
        self.nops += 1
        if self.nops > self.maxops:
            return
        E = self.E[q]
        self._deps(E, reads, writes)
        k = E.dn % self.NDMA
        rnd = E.dn // self.NDMA
        sem = E.dq[k]
        if rnd > 0:
            self._wait(E, (sem, 16 * rnd))
        E.eng.dma_start(out=out_ap, in_=in_ap).then_inc(sem, 16)
        E.dn += 1
        tok = (sem, 16 * (rnd + 1))
        self._mark(tok, reads, writes)
        if out:
            self.out_tokens.append(tok)
        return tok

    def emit_interleaved(self, streams):
        n = max(len(st) for st in streams)
        for i in range(n):
            for st in streams:
                if i < len(st):
                    r = st[i]
                    if r[0] == 0:
                        self.op(r[1], r[2], r[3], r[4])
                    else:
                        self.dma(r[1], r[2], r[3], r[4], r[5], r[6])

    def barrier(self):
        toks = []
        for E in self.E.values():
            if E.count:
                toks.append((E.sem, E.count))
            for k, sem in enumerate(E.dq):
                n = (E.dn - k + self.NDMA - 1) // self.NDMA
                if n > 0:
                    toks.append((sem, 16 * n))
        for E in self.E.values():
            for tok in toks:
                sem, val = tok
                key = id(sem)
                if E.seen.get(key, 0) >= val:
                    continue
                E.eng.wait_ge(sem, val)
                E.seen[key] = val

    def finish(self):
        self.barrier()


D = 1024
SEQ = 8192
NCTX = 256
NT_ALL = 66
NT_LOC = 24
NQT = 18
EPS = 1e-6
C_WQ, C_WK, C_WV, C_DQ, C_DK, C_DV, C_NQ, C_NK, C_NV, C_MQA, C_MKVA, C_G = (
    0, 512, 640, 768, 1280, 1792, 2304, 2816, 3328, 3840, 4096, 4256)
V_WQN, V_WKN, V_DQN, V_DKN, V_NQN, V_NKN = 0, 64, 128, 192, 256, 320
V_MQN, V_MKN, V_MQAN, V_MKVAN, V_SUBLN, V_SINK, V_LAM, V_RB = 384, 480, 576, 832, 960, 1088, 1096, 1352
V_ADAB, V_N1, V_N2 = 1368, 1368 + 6144, 1368 + 6144 + 1024
NVEC = V_N2 + 1024
NSMALL = 1368


def bcast_rows(ap2d, row, c0, n, parts=128):
    t = ap2d.tensor
    cols = ap2d.shape[1]
    return bass.AP(t, ap2d.offset + row * cols + c0, [[0, parts], [1, n]])


CFG = {'phases': 'all', 'nA1': NT_ALL, 'nA2': NT_LOC}


def build_fused(debug=False, layers=(0, 1), segs0=(0, 1, 2, 3)):
    nc = bass.Bass("TRN2", target_bir_lowering=False)

    def din(name, shape, dt=F32):
        return nc.dram_tensor(name, list(shape), dt, kind="ExternalInput").ap()

    def dscr(name, shape, dt=BF16, out=False):
        kind = "ExternalOutput" if (out or debug) else "Internal"
        return nc.dram_tensor(name, list(shape), dt, kind=kind).ap()

    xall = din("xall", [NT_ALL * 128, D])
    rope_all = din("rope_all", [NT_ALL * 128, 96])
    xloc_s = [din("xloc%d" % j, [NT_LOC * 128, D]) for j in range(4)]
    rope_loc_s = [din("rope_loc%d" % j, [NT_LOC * 128, 96]) for j in range(4)]
    wmask_s = [din("wmask%d" % j, [4, 128, 128]) for j in range(4)]
    natv_s = [din("natv%d" % j, [5, 7, 128, 128]) for j in range(4)]
    hsel_in = din("hsel", [128, 8])
    cvec = din("cvec", [128, 16])
    ident_in = din("ident", [128, 128])
    router_w = din("router_w", [D, 16])
    LW = []
    for l in range(2):
        LW.append(dict(
            vec=din("vec_%d" % l, [1, NVEC]), ada_w=din("ada_w_%d" % l, [D, 6 * D]), w_in=din("w_in_%d" % l, [D, 8352]),
            wq_b=din("wq_b_%d" % l, [256, 768]), wkv_b=din("wkv_b_%d" % l, [128, 1024]),
            w_branch=din("w_branch_%d" % l, [2048, D]), w_out=din("w_out_%d" % l, [D, D]),
            moe_w1=din("moe_w1_%d" % l, [16, D, 512]), moe_w3=din("moe_w3_%d" % l, [16, D, 512]),
            moe_w2=din("moe_w2_%d" % l, [16, 512, D]), natb=din("natb_%d" % l, [7, 128, 8, 128])))

    x_out = dscr("x_out", [2048, D], F32, out=True)
    X1 = dscr("X1", [SEQ, D], F32)
    XC1 = dscr("XC1", [NCTX, D], F32)

    KD = dscr("KD", [512, NT_ALL * 128])
    VD = dscr("VD", [NT_ALL * 128, 512])
    KM = dscr("KM", [8, 96, NT_ALL * 128])
    VM = dscr("VM", [NT_ALL * 128, 512])
    KW = dscr("KW", [128, NT_LOC * 128])
    VW = dscr("VW", [NT_LOC * 128, 128])
    KN = dscr("KN", [512, NT_LOC * 128])
    VN = dscr("VN", [NT_LOC * 128, 512])
    QW = dscr("QW", [512, NQT * 128])
    QD = dscr("QD", [512, NQT * 128])
    QN = dscr("QN", [512, NQT * 128])
    QM = dscr("QM", [8, 96, NQT * 128])
    GATES = dscr("GATES", [NQT * 128, 4096])
    Y = dscr("Y", [NQT * 128, 2048])
    XN = dscr("XN", [NQT * 128, D], F32)
    MODS = dscr("MODS", [2, 128, 6 * D], F32)
    HTS = dscr("HTS", [NQT, 128, D])

    fw = FW(nc)
    with fw:
        op, dma = fw.op, fw.dma

        def mm(out, lhsT, rhs, start, stop, reads, writes):
            op("pe", lambda e: e.matmul(out, lhsT, rhs, start=start, stop=stop), reads, writes)

        def tr(out, in_, idn, reads, writes):
            op("pe", lambda e: e.transpose(out, in_, idn), reads, writes)

        def tt(en, out, a, b, alu, reads, writes):
            op(en, lambda e: e.tensor_tensor(out, a, b, alu), reads, writes)

        def ts(en, out, a, s1, s2, o0, o1, reads, writes):
            if s2 is None:
                op(en, lambda e: e.tensor_scalar(out, a, s1, None, o0), reads, writes)
            else:
                op(en, lambda e: e.tensor_scalar(out, a, s1, s2, o0, o1), reads, writes)

        def stt(en, out, a, s, b, o0, o1, reads, writes):
            op(en, lambda e: e.scalar_tensor_tensor(out, a, s, b, o0, o1), reads, writes)

        def act(out, in_, func, reads, writes, **kw):
            op("act", lambda e: e.activation(out, in_, func, **kw), reads, writes)

        def cp(en, out, in_, reads, writes):
            if en == "act":
                act(out, in_, AF.Copy, reads, writes)
            else:
                op(en, lambda e: e.tensor_copy(out, in_), reads, writes)

        identb = fw.sb("identb", [128, 128], BF16)
        identf = fw.sb("identf", [128, 128], F32)
        VEC = fw.sb("VEC", [128, NSMALL], F32)
        dma("pool", identb.t[:], ident_in, [], [identb])
        dma("sp", identf.t[:], ident_in, [], [identf])
        gv = lambda off, n: VEC.t[:, off:off + n]

        HSEL = fw.sb("HSEL", [128, 8], F32)
        dma("sp", HSEL.t[:], hsel_in, [], [HSEL])

        def layer_pass(lidx, seg, want_ctx, first):
            lam_init = 0.8 - 0.6 * float(np.exp(-0.3 * lidx))
            W_ = LW[lidx]
            vec, ada_w, w_in, wq_b, wkv_b = W_["vec"], W_["ada_w"], W_["w_in"], W_["wq_b"], W_["wkv_b"]
            w_branch, w_out, moe_w1, moe_w3, moe_w2, natb = W_["w_branch"], W_["w_out"], W_["moe_w1"], W_["moe_w3"], W_["moe_w2"], W_["natb"]
            rope_loc, wmask, natv = rope_loc_s[seg], wmask_s[seg], natv_s[seg]
            if first:
                dma("sp", VEC.t[:], bcast_rows(vec, 0, 0, NSMALL), [], [VEC])

            def dense_src(i):
                if lidx == 0:
                    return xall[i * 128:(i + 1) * 128, :]
                return XC1[i * 128:(i + 1) * 128, :] if i < 2 else X1[(i - 2) * 128:(i - 1) * 128, :]

            def local_src(i):
                if lidx == 0:
                    return xloc_s[seg][i * 128:(i + 1) * 128, :]
                if 3 <= i < 19:
                    return X1[(i - 3) * 128:(i - 2) * 128, :]
                if i >= 22:
                    return XC1[(i - 22) * 128:(i - 21) * 128, :]
                if i < 3:
                    return [(X1[(r * 16 + 13 + i) * 128:(r * 16 + 14 + i) * 128, :], HSEL.t[:, r:r + 1]) for r in (1, 2, 3)]
                return [(X1[(r * 16 + i - 19) * 128:(r * 16 + i - 18) * 128, :], HSEL.t[:, 4 + r:5 + r]) for r in (1, 2, 3)]

            if first:
              with fw.scope():
                cs = fw.sb("cs", [128, 2, 8], F32)
                CB = fw.sb("CB", [128, 2, 8, 128], F32)
                NG = fw.sb("NG", [128, 2, D], F32)
                dma("sp", cs.t[:].rearrange("p w j -> p (w j)"), cvec, [], [cs])
                dma("sp", NG.t[:, 0, :], bcast_rows(vec, 0, V_N1, D), [], [NG])
                dma("sp", NG.t[:, 1, :], bcast_rows(vec, 0, V_N2, D), [], [NG])
                act(cs.t[:], cs.t[:], AF.Silu, [cs], [cs])
                cp("dve", CB.t[:], cs.t[:].unsqueeze(3).to_broadcast([128, 2, 8, 128]), [cs], [CB])
                aw = [fw.sb("aw%d" % i, [128, 8, 512], F32) for i in range(2)]
                ab = [fw.sb("ab%d" % i, [128, 512], F32) for i in range(2)]
                mps = [fw.ps("mps%d" % i, [128, 512], F32) for i in range(2)]
                mo = [fw.sb("mo%d" % i, [128, 512], F32) for i in range(2)]
                n = 0
                for blk in range(12):
                    a_, b_ = aw[blk % 2], ab[blk % 2]
                    dma("sp", a_.t[:], ada_w[:, blk * 512:(blk + 1) * 512].rearrange("(j p) c -> p j c", p=128), [], [a_])
                    dma("sp", b_.t[:], bcast_rows(vec, 0, V_ADAB + blk * 512, 512), [], [b_])
                    chunk = blk // 2
                    half = blk % 2
                    for w in range(2):
                        p_, o_ = mps[n % 2], mo[n % 2]
                        n += 1
                        for j in range(8):
                            mm(p_.t[:], CB.t[:, w, j, :], a_.t[:, j, :], j == 0, j == 7, [CB, a_], [p_])
                        tt("dve", o_.t[:], p_.t[:], b_.t[:], ALU.add, [p_, b_], [o_])
                        if chunk in (1, 4):
                            g = NG.t[:, 0 if chunk == 1 else 1, half * 512:(half + 1) * 512]
                            stt("dve", o_.t[:], o_.t[:], 1.0, g, ALU.add, ALU.mult, [o_, NG], [o_])
                        dma("sp", MODS[w, :, blk * 512:(blk + 1) * 512], o_.t[:], [o_], [])
            M_SH1, M_A1, M_G1, M_SH2, M_A2, M_G2 = [i * D for i in range(6)]

            def load_mod(buf, which, off):
                dma("sp", buf.t[:], MODS[which, :, off:off + D], [], [buf])

            with fw.scope():
                A1 = [fw.sb("A1_%d" % w, [128, D], F32) for w in range(2)]
                SH1 = [fw.sb("SH1_%d" % w, [128, D], F32) for w in range(2)]
                for w in range(2):
                    load_mod(A1[w], w, M_A1)
                    load_mod(SH1[w], w, M_SH1)
                wkvb = fw.sb("wkvb", [128, 1024], BF16)
                wqb = fw.sb("wqb", [128, 2, 768], BF16)
                dma("pool", wkvb.t[:], wkv_b, [], [wkvb])
                dma("pool", wqb.t[:], wq_b.rearrange("(j p) c -> p j c", p=128), [], [wqb])
                junk = fw.sb("junk", [128, D], F32)

                class _Set:
                    pass

                def mkset(k):
                    S = _Set()
                    S.xt = fw.sb("xt", [128, D], F32)
                    S.rp = fw.sb("rp", [128, 96], F32)
                    S.st = fw.sb("st", [128, 16], F32)
                    S.hb = fw.sb("hb", [128, D], BF16)
                    S.hT = fw.sb("hT", [128, D], BF16)
                    S.ptr = fw.ps("ptr", [128, D], BF16)
                    S.pp = [fw.ps("pp", [128, 512], F32) for _ in range(3)]
                    S.xs = fw.sb("xs", [128, 1024], F32)
                    S.sq = fw.sb("sq", [128, 1024], F32)
                    S.ss = fw.sb("ss", [128, 32], F32)
                    S.yb = fw.sb("yb", [128, 1024], F32)
                    S.rt = fw.sb("rt", [128, 4, 8 * 48], F32)
                    S.ob = [fw.sb("ob", [128, 1024], BF16) for _ in range(2)]
                    S.oT = [fw.sb("oT", [128, 1024], BF16) for _ in range(2)]
                    S.mkt = fw.sb("mkt", [128, 8, 96], F32)
                    S.cnt = {"g": 0, "o": 0, "t": 0, "p": 0}
                    return S
                sets = [mkset(k) for k in range(2)]

                def hnr(S, src_aps, src_bufs, H, Dh, gain, rope):
                    g = S.cnt["g"]
                    S.cnt["g"] += 1
                    N = H * Dh
                    x_, s_, y_, r_, sq = S.xs, S.ss, S.yb, S.rt, S.sq
                    so = (g % 2) * 16
                    o_ = S.ob[S.cnt["o"] % 2]
                    S.cnt["o"] += 1
                    v3 = lambda ap: ap.rearrange("p (h d) -> p h d", h=H)
                    for (sap, c0_, nc_) in src_aps:
                        cp("act", x_.t[:, c0_:c0_ + nc_], sap, src_bufs, [x_])
                    tt("pool", sq.t[:, :N], x_.t[:, :N], x_.t[:, :N], ALU.mult, [x_], [sq])
                    op("dve", lambda e: e.tensor_reduce(s_.t[:, so:so + H], v3(sq.t[:, :N]), AX.X, ALU.add), [sq], [s_])
                    act(s_.t[:, so + 8:so + 8 + H], s_.t[:, so:so + H], AF.Sqrt, [s_], [s_], bias=EPS, scale=1.0 / Dh)
                    op("dve", lambda e: e.reciprocal(s_.t[:, so:so + H], s_.t[:, so + 8:so + 8 + H]), [s_], [s_])
                    tt("dve", v3(y_.t[:, :N]), v3(x_.t[:, :N]), s_.t[:, so:so + H].unsqueeze(2).to_broadcast([128, H, Dh]),
                       ALU.mult, [x_, s_], [y_])
                    gb = gain.unsqueeze(1).to_broadcast([128, H, Dh])
                    if rope is None:
                        tt("pool", v3(o_.t[:, :N]), v3(y_.t[:, :N]), gb, ALU.mult, [y_, VEC], [o_])
                        return o_
                    r0, n, cos_ap, sin_ap, rbuf = rope
                    hlf = n // 2
                    tt("pool", v3(y_.t[:, :N]), v3(y_.t[:, :N]), gb, ALU.mult, [y_, VEC], [y_])
                    y3 = v3(y_.t[:, :N])
                    o3 = v3(o_.t[:, :N])
                    x1, x2 = y3[:, :, r0:r0 + hlf], y3[:, :, r0 + hlf:r0 + n]
                    cb = cos_ap.unsqueeze(1).to_broadcast([128, H, hlf])
                    sb_ = sin_ap.unsqueeze(1).to_broadcast([128, H, hlf])
                    t = [r_.t[:, i, :H * hlf].rearrange("p (h d) -> p h d", h=H) for i in range(4)]
                    tt("dve", t[0], x1, cb, ALU.mult, [y_, rbuf], [r_])
                    tt("pool", t[1], x2, sb_, ALU.mult, [y_, rbuf], [r_])
                    tt("dve", t[2], x1, sb_, ALU.mult, [y_, rbuf], [r_])
                    tt("pool", t[3], x2, cb, ALU.mult, [y_, rbuf], [r_])
                    tt("dve", o3[:, :, r0:r0 + hlf], t[0], t[1], ALU.subtract, [r_], [o_])
                    tt("pool", o3[:, :, r0 + hlf:r0 + n], t[2], t[3], ALU.add, [r_], [o_])
                    if r0 > 0:
                        cp("act", o3[:, :, 0:r0], y3[:, :, 0:r0], [y_], [o_])
                    return o_

                def transp(S, o_, nblk, bw):
                    k = S.cnt["t"]
                    S.cnt["t"] += 1
                    p_, t_ = S.ptr, S.oT[k % 2]
                    for i in range(nblk):
                        tr(p_.t[0:bw, i * 128:(i + 1) * 128], o_.t[:, i * bw:(i + 1) * bw], identb.t[:], [o_, identb], [p_])
                    cp("dve" if k % 2 else "act", t_.t[0:bw, 0:nblk * 128], p_.t[0:bw, 0:nblk * 128], [p_], [t_])
                    return t_

                def transp_store(S, o_, nblk, bw, dst_ap):
                    t_ = transp(S, o_, nblk, bw)
                    dma("sp", dst_ap, t_.t[0:bw, 0:nblk * 128].rearrange("p (i t) -> p i t", i=nblk), [t_], [])

                def plain_store(S, src_ap, src_bufs, N, dst_ap):
                    o_ = S.ob[S.cnt["o"] % 2]
                    S.cnt["o"] += 1
                    cp("act", o_.t[:, :N], src_ap, src_bufs, [o_])
                    dma("sp", dst_ap, o_.t[:, :N], [o_], [])

                def make_hT(S, xsrc, rsrc, row0, which):
                    x_, r_, s_, hT_, h1 = S.xt, S.rp, S.st, S.hT, S.yb
                    if isinstance(xsrc, list):
                        for ci_, (cap, wap) in enumerate(xsrc):
                            dma("sp", S.sq.t[:], cap, [], [S.sq])
                            if ci_ == 0:
                                ts("dve", x_.t[:], S.sq.t[:], wap, None, ALU.mult, None, [S.sq, HSEL], [x_])
                            else:
                                stt("dve", x_.t[:], S.sq.t[:], wap, x_.t[:], ALU.mult, ALU.add, [S.sq, HSEL, x_], [x_])
                    else:
                        dma("sp", x_.t[:], xsrc, [], [x_])
                    dma("sp", r_.t[:], rsrc[row0:row0 + 128, :], [], [r_])
                    act(junk.t[:], x_.t[:], AF.Square, [x_], [junk, s_], accum_out=s_.t[:, 0:1])
                    act(s_.t[:, 1:2], s_.t[:, 0:1], AF.Sqrt, [s_], [s_], bias=EPS, scale=1.0 / D)
                    op("dve", lambda e: e.reciprocal(s_.t[:, 2:3], s_.t[:, 1:2]), [s_], [s_])
                    stt("dve", h1.t[:], x_.t[:], s_.t[:, 2:3], A1[which].t[:], ALU.mult, ALU.mult, [x_, s_, A1[which]], [h1])
                    tt("pool", S.hb.t[:], h1.t[:], SH1[which].t[:], ALU.add, [h1, SH1[which]], [S.hb])
                    p_ = S.ptr
                    for j in range(8):
                        tr(p_.t[:, j * 128:(j + 1) * 128], S.hb.t[:, j * 128:(j + 1) * 128], identb.t[:], [S.hb, identb], [p_])
                    cp("dve", hT_.t[:], p_.t[:], [p_], [hT_])
                    return hT_, r_

                def nextp(S):
                    S.cnt["p"] += 1
                    return S.pp[S.cnt["p"] % 3]

                def project(S, hT_, W, c0, ncol):
                    pbuf = nextp(S)
                    for j in range(8):
                        mm(pbuf.t[:, 0:ncol], hT_.t[:, j * 128:(j + 1) * 128], W.t[:, j, c0:c0 + ncol], j == 0, j == 7, [hT_, W], [pbuf])
                    return pbuf

                def run_interleaved(tile_fn, tiles):
                    for a in range(0, len(tiles), 2):
                        streams = []
                        for k, i in enumerate(tiles[a:a + 2]):
                            fw.rec = []
                            tile_fn(i, sets[k])
                            streams.append(fw.rec)
                            fw.rec = None
                        fw.emit_interleaved(streams)

                if first:
                  with fw.scope():
                    Wd = fw.sb("Wd", [128, 8, 1184], BF16)
                    dma("pool", Wd.t[:, :, 0:1024], w_in[:, C_DK:C_DK + 1024].rearrange("(j p) c -> p j c", p=128), [], [Wd])
                    dma("pool", Wd.t[:, :, 1024:1184], w_in[:, C_MKVA:C_MKVA + 160].rearrange("(j p) c -> p j c", p=128), [], [Wd])

                    def a1_tile(i, S):
                        which = 1 if i < 2 else 0
                        hT_, r_ = make_hT(S, dense_src(i), rope_all, i * 128, which)
                        t0 = i * 128
                        cosh, sinh, cosm, sinm = r_.t[:, 0:32], r_.t[:, 32:64], r_.t[:, 64:80], r_.t[:, 80:96]
                        mkt = S.mkt
                        p_ = project(S, hT_, Wd, 0, 512)
                        o_ = hnr(S, [(p_.t[:, 0:512], 0, 512)], [p_], 8, 64, gv(V_DKN, 64), (0, 64, cosh, sinh, r_))
                        transp_store(S, o_, 4, 128, KD.rearrange("(i q) t -> q i t", q=128)[:, :, t0:t0 + 128])
                        p_ = project(S, hT_, Wd, 512, 512)
                        plain_store(S, p_.t[:, 0:512], [p_], 512, VD[t0:t0 + 128, :])
                        p_ = project(S, hT_, Wd, 1024, 160)
                        cp("act", mkt.t[:, :, 64:96], p_.t[:, 128:160].unsqueeze(1).to_broadcast([128, 8, 32]), [p_], [mkt])
                        o_ = hnr(S, [(p_.t[:, 0:128], 0, 128)], [p_], 1, 128, gv(V_MKVAN, 128), None)
                        t_ = transp(S, o_, 1, 128)
                        o2 = S.ob[S.cnt["o"] % 2]
                        S.cnt["o"] += 1
                        for hh in range(2):
                            p2 = nextp(S)
                            mm(p2.t[:, 0:512], t_.t[:, 0:128], wkvb.t[:, hh * 512:(hh + 1) * 512], True, True, [t_, wkvb], [p2])
                            kv3 = p2.t[:, 0:512].rearrange("p (h d) -> p h d", h=4)
                            cp("dve", mkt.t[:, hh * 4:hh * 4 + 4, 0:64], kv3[:, :, 0:64], [p2], [mkt])
                            cp("dve", o2.t[:, hh * 256:hh * 256 + 256].rearrange("p (h d) -> p h d", h=4), kv3[:, :, 64:128], [p2], [o2])
                        dma("sp", VM[t0:t0 + 128, :], o2.t[:, 0:512], [o2], [])
                        o_ = hnr(S, [(mkt.t[:].rearrange("p h d -> p (h d)"), 0, 768)], [mkt], 8, 96, gv(V_MKN, 96), (64, 32, cosm, sinm, r_))
                        transp_store(S, o_, 8, 96, KM.rearrange("h d t -> d h t")[:, :, t0:t0 + 128])
                    run_interleaved(a1_tile, list(range(CFG['nA1'])))

                with fw.scope():
                    Wl = fw.sb("Wl", [128, 8, 3072], BF16)
                    for (dst0, c0, ncol) in ((0, 0, 1280), (1280, C_NQ, 1792)):
                        for s0 in range(0, ncol, 640):
                            sn = min(640, ncol - s0)
                            dma("pool", Wl.t[:, :, dst0 + s0:dst0 + s0 + sn],
                                w_in[:, c0 + s0:c0 + s0 + sn].rearrange("(j p) c -> p j c", p=128), [], [Wl])
                    L_WQ, L_WKV, L_DQ, L_NQ, L_NK, L_NV, L_MQA = 0, 512, 768, 1280, 1792, 2304, 2816

                    def a2_tile(i, S):
                        is_ctx = i >= 22
                        is_own = 3 <= i < 19
                        wantq = is_own or (is_ctx and want_ctx)
                        qs = (i - 3) if is_own else (16 + i - 22)
                        hT_, r_ = make_hT(S, local_src(i), rope_loc, i * 128, 1 if is_ctx else 0)
                        t0 = i * 128
                        q0 = qs * 128
                        cosh, sinh, cosm, sinm = r_.t[:, 0:32], r_.t[:, 32:64], r_.t[:, 64:80], r_.t[:, 80:96]
                        p_ = project(S, hT_, Wl, L_WKV, 256)
                        o_ = hnr(S, [(p_.t[:, 0:128], 0, 128)], [p_], 2, 64, gv(V_WKN, 64), (0, 64, cosh, sinh, r_))
                        transp_store(S, o_, 1, 128, KW[:, t0:t0 + 128].unsqueeze(1))
                        plain_store(S, p_.t[:, 128:256], [p_], 128, VW[t0:t0 + 128, :])
                        p_ = project(S, hT_, Wl, L_NK, 512)
                        o_ = hnr(S, [(p_.t[:, 0:512], 0, 512)], [p_], 8, 64, gv(V_NKN, 64), None)
                        transp_store(S, o_, 4, 128, KN.rearrange("(i q) t -> q i t", q=128)[:, :, t0:t0 + 128])
                        p_ = project(S, hT_, Wl, L_NV, 512)
                        plain_store(S, p_.t[:, 0:512], [p_], 512, VN[t0:t0 + 128, :])
                        if not wantq:
                            return
                        dma("sp", HTS[qs], hT_.t[:], [hT_], [])
                        for (lc, gain, dst, rope) in ((L_WQ, V_WQN, QW, True), (L_DQ, V_DQN, QD, True), (L_NQ, V_NQN, QN, False)):
                            p_ = project(S, hT_, Wl, lc, 512)
                            o_ = hnr(S, [(p_.t[:, 0:512], 0, 512)], [p_], 8, 64, gv(gain, 64), (0, 64, cosh, sinh, r_) if rope else None)
                            transp_store(S, o_, 4, 128, dst.rearrange("(i q) t -> q i t", q=128)[:, :, q0:q0 + 128])
                        p_ = project(S, hT_, Wl, L_MQA, 256)
                        o_ = hnr(S, [(p_.t[:, 0:256], 0, 256)], [p_], 1, 256, gv(V_MQAN, 256), None)
                        t_ = transp(S, o_, 2, 128)
                        pa_, pb_ = nextp(S), nextp(S)
                        for (pq, n0, nn) in ((pa_, 0, 512), (pb_, 512, 256)):
                            for j in range(2):
                                mm(pq.t[:, 0:nn], t_.t[:, j * 128:(j + 1) * 128], wqb.t[:, j, n0:n0 + nn], j == 0, j == 1, [t_, wqb], [pq])
                        o_ = hnr(S, [(pa_.t[:, 0:512], 0, 512), (pb_.t[:, 0:256], 512, 256)], [pa_, pb_], 8, 96, gv(V_MQN, 96), (64, 32, cosm, sinm, r_))
                        transp_store(S, o_, 8, 96, QM.rearrange("h d t -> d h t")[:, :, q0:q0 + 128])
                    run_interleaved(a2_tile, list(range(NT_LOC)) if CFG['nA2'] == NT_LOC else [0, 3, 22][:CFG['nA2']])

                with fw.scope():
                    Wg = [fw.sb("Wg%d" % i, [128, 8, 1024], BF16) for i in range(2)]
                    gt = [fw.sb("gt%d" % i, [128, 1024], BF16) for i in range(2)]
                    gp = [sets[0].pp[0], sets[0].pp[1], sets[1].pp[0], sets[1].pp[1]]
                    nq_tiles = NQT if want_ctx else 16
                    if CFG['nA2'] != NT_LOC:
                        nq_tiles = 0
                    HT = fw.sb("HT", [128, NQT, D], BF16)
                    for qs in range(nq_tiles):
                        dma("sp", HT.t[:, qs, :], HTS[qs], [], [HT])
                    n = 0
                    for gq in range(4):
                        W = Wg[gq % 2]
                        for s0 in (0, 512):
                            dma("pool", W.t[:, :, s0:s0 + 512],
                                w_in[:, C_G + gq * 1024 + s0:C_G + gq * 1024 + s0 + 512].rearrange("(j p) c -> p j c", p=128), [], [W])
                        for qs in range(nq_tiles):
                            g_ = gt[n % 2]
                            for hf in range(2):
                                p_ = gp[(2 * n + hf) % 4]
                                for j in range(8):
                                    mm(p_.t[:], HT.t[:, qs, j * 128:(j + 1) * 128], W.t[:, j, hf * 512:(hf + 1) * 512], j == 0, j == 7, [HT, W], [p_])
                                cp("act" if hf else "dve", g_.t[:, hf * 512:(hf + 1) * 512], p_.t[:], [p_], [g_])
                            n += 1
                            dma("sp", GATES[qs * 128:(qs + 1) * 128, gq * 1024:(gq + 1) * 1024], g_.t[:], [g_], [])

            if CFG['phases'] == 'all' or 'B' in CFG['phases']:
              with fw.scope():
                ES = fw.sb("ES", [128, 8], F32)
                act(ES.t[:], gv(V_SINK, 8), AF.Exp, [VEC], [ES])
                LM = fw.sb("LM", [128, 8], F32)
                lt = fw.sb("lt", [128, 128], F32)
                tt("dve", lt.t[:, 0:64], gv(V_LAM, 64), gv(V_LAM + 64, 64), ALU.mult, [VEC], [lt])
                tt("dve", lt.t[:, 64:128], gv(V_LAM + 128, 64), gv(V_LAM + 192, 64), ALU.mult, [VEC, lt], [lt])
                op("dve", lambda e: e.tensor_reduce(LM.t[:, 0:2], lt.t[:].rearrange("p (a d) -> p a d", a=2), AX.X, ALU.add), [lt], [LM])
                act(LM.t[:, 2:4], LM.t[:, 0:2], AF.Exp, [LM], [LM])
                tt("dve", LM.t[:, 4:5], LM.t[:, 2:3], LM.t[:, 3:4], ALU.subtract, [LM], [LM])
                ts("dve", LM.t[:, 5:6], LM.t[:, 4:5], -1.0, -lam_init, ALU.mult, ALU.add, [LM], [LM])
                NEGLAM = LM.t[:, 5:6]
                SG = fw.sb("SG", [128, 128], F32)
                ts("dve", SG.t[:], gv(V_SUBLN, 128), 1.0 - lam_init, None, ALU.mult, None, [VEC], [SG])

                acc = [fw.ps("acc%d" % i, [128, 512], F32) for i in range(4)]
                sps = [fw.ps("sps%d" % i, [128, 512], F32) for i in range(3)]
                pt = [fw.sb("pt%d" % i, [128, 512], BF16) for i in range(3)]
                fz = [fw.sb("fz%d" % i, [128, 8], F32) for i in range(4)]
                cs_ = [0]
                nqt = NQT if want_ctx else 16

                def attn(chunks, nacc, dv, scale, qn=512):
                    n = len(chunks)
                    LOOK = 2
                    live = {}
                    for it in range(n + LOOK):
                        if it < n:
                            ch = chunks[it]
                            k = cs_[0]
                            cs_[0] += 1
                            s_, p_ = sps[k % 3], pt[k % 3]
                            for (l_ap, r_ap, c0, ncl) in ch['mms']:
                                mm(s_.t[:, c0:c0 + ncl], l_ap, r_ap, True, True, ch['rb'], [s_])
                            act(p_.t[:, :qn], s_.t[:, :qn], AF.Exp, [s_], [p_], scale=scale)
                            for mi, (m_ap, m_buf) in enumerate(ch.get('masks', ())):
                                p3 = p_.t[:, :].rearrange("p (h q) -> p h q", h=4)
                                tt("pool" if mi % 2 == 0 else "dve", p3, p3, m_ap, ALU.mult, [p_, m_buf], [p_])
                            live[it] = p_
                        ci = it - LOOK
                        if ci >= 0:
                            ch = chunks[ci]
                            p_ = live.pop(ci)
                            for j in range(nacc):
                                mm(acc[j].t[:, 0:dv + 1], p_.t[:, j * 128:(j + 1) * 128], ch['v'][j], ci == 0, ci == n - 1,
                                   [p_] + ch['vb'], [acc[j]])

                def fin_simple(j, dst_ap, dst_buf, dv, sink_ap=None):
                    z_ = fz[j]
                    if sink_ap is not None:
                        tt("dve", z_.t[:, 0:1], acc[j].t[:, dv:dv + 1], sink_ap, ALU.add, [acc[j], ES], [z_])
                        op("dve", lambda e: e.reciprocal(z_.t[:, 1:2], z_.t[:, 0:1]), [z_], [z_])
                    else:
                        op("dve", lambda e: e.reciprocal(z_.t[:, 1:2], acc[j].t[:, dv:dv + 1]), [acc[j]], [z_])
                    ts("dve", dst_ap, acc[j].t[:, 0:dv], z_.t[:, 1:2], None, ALU.mult, None, [acc[j], z_], [dst_buf])

                Ysb = [fw.sb("Ysb%d" % i, [128, NQT, 512], BF16) for i in range(2)]

                def store_branch(n, ybuf):
                    dma("sp", Y[0:nqt * 128, n * 512:(n + 1) * 512].rearrange("(t p) c -> p t c", p=128), ybuf.t[:, 0:nqt, :], [ybuf], [])

                if 'nodense' not in CFG['phases']:
                  with fw.scope():
                    kb = [fw.sb("kb%d" % i, [96, NT_ALL * 128], BF16) for i in range(2)]
                    vb = [fw.sb("vb%d" % i, [128, NT_ALL, 129], BF16) for i in range(2)]
                    qb_ = [fw.sb("qb%d" % i, [96, NQT * 128], BF16) for i in range(2)]
                    D1 = fw.sb("D1", [128, NQT, 128], F32)
                    dtm = [fw.sb("dtm%d" % i, [128, 128], F32) for i in range(2)]
                    dsq = fw.sb("dsq", [128, 128], F32)
                    hcount = [0]

                    def dense_head(KTsrc, d, QTsrc, V1, dv, scale, fin):
                        hi = hcount[0]
                        hcount[0] += 1
                        KT, QT = kb[hi % 2], qb_[hi % 2]
                        dma("sp", KT.t[0:d, :], KTsrc, [], [KT])
                        dma("sp", QT.t[0:d, :], QTsrc, [], [QT])
                        for qblk in range(4):
                            chunks = [dict(mms=[(KT.t[0:d, c * 128:(c + 1) * 128], QT.t[0:d, qblk * 512:(qblk + 1) * 512], 0, 512)],
                                           rb=[KT, QT], v=[V1.t[:, c, 0:dv + 1]] * 4, vb=[V1]) for c in range(CFG.get('nkc', NT_ALL))]
                            attn(chunks, 4, dv, scale)
                            for j in range(4):
                                fin(j, qblk * 4 + j)
                        if want_ctx:
                            chunks = [dict(mms=[(KT.t[0:d, c * 128:(c + 1) * 128], QT.t[0:d, 2048:2304], 0, 256)],
                                           rb=[KT, QT], v=[V1.t[:, c, 0:dv + 1]] * 2, vb=[V1]) for c in range(2)]
                            attn(chunks, 2, dv, scale, qn=256)
                            for j in range(2):
                                fin(j, 16 + j)

                    Yd = Ysb[0]
                    for hd in range(4):
                        V1 = vb[hd % 2]
                        dma("sp", V1.t[:, :, 0:128], VD[:, hd * 128:(hd + 1) * 128].rearrange("(c p) d -> p c d", p=128), [], [V1])
                        op("pool", lambda e: e.memset(V1.t[:, :, 128:129], 1.0), [], [V1])
                        for i2 in range(2):
                            hs = 2 * hd + i2

                            def fin(j, qt, i2=i2, hd=hd):
                                if i2 == 0:
                                    fin_simple(j, D1.t[:, qt, :], D1, 128)
                                    return
                                t_ = dtm[j % 2]
                                z_ = fz[j]
                                fin_simple(j, t_.t[:], t_, 128)
                                stt("dve", t_.t[:], t_.t[:], NEGLAM, D1.t[:, qt, :], ALU.mult, ALU.add, [t_, LM, D1], [t_])
                                tt("pool", dsq.t[:], t_.t[:], t_.t[:], ALU.mult, [t_], [dsq])
                                op("dve", lambda e: e.tensor_reduce(z_.t[:, 2:3], dsq.t[:], AX.X, ALU.add), [dsq], [z_])
                                act(z_.t[:, 3:4], z_.t[:, 2:3], AF.Sqrt, [z_], [z_], bias=EPS, scale=1.0 / 128)
                                op("dve", lambda e: e.reciprocal(z_.t[:, 4:5], z_.t[:, 3:4]), [z_], [z_])
                                stt("dve", Yd.t[:, qt, hd * 128:(hd + 1) * 128], t_.t[:], z_.t[:, 4:5], SG.t[:], ALU.mult, ALU.mult,
                                    [t_, z_, SG], [Yd])
                            dense_head(KD[hs * 64:(hs + 1) * 64, :], 64, QD[hs * 64:(hs + 1) * 64, :], V1, 128, 0.125, fin)
                    store_branch(1, Yd)

                    Ym = Ysb[1]
                    for h in range(8):
                        V1 = vb[h % 2]
                        dma("sp", V1.t[:, :, 0:64], VM[:, h * 64:(h + 1) * 64].rearrange("(c p) d -> p c d", p=128), [], [V1])
                        op("pool", lambda e: e.memset(V1.t[:, :, 64:65], 1.0), [], [V1])

                        def fin(j, qt, h=h):
                            fin_simple(j, Ym.t[:, qt, h * 64:(h + 1) * 64], Ym, 64)
                        dense_head(KM[h], 96, QM[h], V1, 64, 96.0 ** -0.5, fin)
                    store_branch(3, Ym)

                with fw.scope():
                    Yw = Ysb[0]
                    WM = fw.sb("WM", [128, 4, 128], BF16)
                    dma("pool", WM.t[:], wmask.rearrange("m k q -> k m q"), [], [WM])
                    kw_ = [fw.sb("kw%d" % i, [64, NT_LOC * 128], BF16) for i in range(2)]
                    vw_ = [fw.sb("vw%d" % i, [128, NT_LOC, 65], BF16) for i in range(2)]
                    qw_ = [fw.sb("qw%d" % i, [64, NQT, 4, 128], BF16) for i in range(2)]
                    for g in range(2):
                        KT, V1, QT = kw_[g], vw_[g], qw_[g]
                        dma("sp", KT.t[:], KW[g * 64:(g + 1) * 64, :], [], [KT])
                        dma("sp", V1.t[:, :, 0:64], VW[:, g * 64:(g + 1) * 64].rearrange("(c p) d -> p c d", p=128), [], [V1])
                        op("pool", lambda e: e.memset(V1.t[:, :, 64:65], 1.0), [], [V1])
                        for hq in range(4):
                            dma("sp", QT.t[:, :, hq, :], QW[(g * 4 + hq) * 64:(g * 4 + hq + 1) * 64, :].rearrange("d (t q) -> d t q", q=128), [], [QT])
                        for qt in range(nqt):
                            rhs = QT.t[0:64, qt, :, :].rearrange("p h q -> p (h q)")
                            if qt < 16:
                                sl = [(qt + 2, WM.t[:, 0 if qt == 0 else 1, :]), (qt + 3, None), (qt + 4, WM.t[:, 3 if qt == 15 else 2, :]), (22, None), (23, None)]
                            else:
                                sl = [(22, None), (23, None)]
                            chunks = []
                            for (slot, m) in sl:
                                ch = dict(mms=[(KT.t[0:64, slot * 128:(slot + 1) * 128], rhs, 0, 512)], rb=[KT, QT],
                                          v=[V1.t[:, slot, 0:65]] * 4, vb=[V1])
                                if m is not None:
                                    ch['masks'] = [(m.unsqueeze(1).to_broadcast([128, 4, 128]), WM)]
                                chunks.append(ch)
                            attn(chunks, 4, 64, 0.125)
                            for j in range(4):
                                h = g * 4 + j
                                fin_simple(j, Yw.t[:, qt, h * 64:(h + 1) * 64], Yw, 64, sink_ap=ES.t[:, h:h + 1])
                    store_branch(0, Yw)

                with fw.scope():
                    Yn = Ysb[1]
                    NVm = fw.sb("NVm", [128, 5, 7, 128], BF16)
                    dma("pool", NVm.t[:], natv.rearrange("a c k q -> k a c q"), [], [NVm])
                    ebf = fw.sb("ebf", [128, 7, 4, 128], F32)
                    EB = [fw.sb("EB%d" % i, [128, 7, 4, 128], BF16) for i in range(2)]
                    kn_ = [fw.sb("kn%d" % i, [64, 4, NT_LOC * 128], BF16) for i in range(2)]
                    vn_ = [fw.sb("vn%d" % i, [128, NT_LOC, 4, 65], BF16) for i in range(2)]
                    qn_ = [fw.sb("qn%d" % i, [64, NQT, 4, 128], BF16) for i in range(2)]
                    for hh in range(2):
                        KT, V1, QT, EBh = kn_[hh], vn_[hh], qn_[hh], EB[hh]
                        for c in range(7):
                            dma("sp", ebf.t[:, c, :, :], natb[c, :, hh * 4:(hh + 1) * 4, :], [], [ebf])
                        act(EBh.t[:].rearrange("p c h q -> p (c h q)"), ebf.t[:].rearrange("p c h q -> p (c h q)"), AF.Exp, [ebf], [EBh])
                        for j in range(4):
                            h = hh * 4 + j
                            dma("sp", KT.t[:, j, :], KN[h * 64:(h + 1) * 64, :], [], [KT])
                            dma("sp", V1.t[:, :, j, 0:64], VN[:, h * 64:(h + 1) * 64].rearrange("(c p) d -> p c d", p=128), [], [V1])
                            dma("sp", QT.t[:, :, j, :], QN[h * 64:(h + 1) * 64, :].rearrange("d (t q) -> d t q", q=128), [], [QT])
                        op("pool", lambda e: e.memset(V1.t[:, :, :, 64:65], 1.0), [], [V1])
                        for qt in range(nqt):
                            chunks = []
                            if qt < 16:
                                cls = {0: 0, 1: 1, 14: 3, 15: 4}.get(qt, 2)
                                lst = [(qt + c + 3, c + 3) for c in range(-3, 4)] + [(22, None), (23, None)]
                            else:
                                lst = [(22, None), (23, None)]
                            for (slot, ci) in lst:
                                ch = dict(mms=[(KT.t[0:64, j, slot * 128:(slot + 1) * 128], QT.t[0:64, qt, j, :], j * 128, 128) for j in range(4)],
                                          rb=[KT, QT], v=[V1.t[:, slot, j, 0:65] for j in range(4)], vb=[V1])
                                if ci is not None:
                                    ch['masks'] = [(EBh.t[:, ci, :, :], EBh),
                                                   (NVm.t[:, cls, ci, :].unsqueeze(1).to_broadcast([128, 4, 128]), NVm)]
                                chunks.append(ch)
                            attn(chunks, 4, 64, 0.125)
                            for j in range(4):
                                h = hh * 4 + j
                                fin_simple(j, Yn.t[:, qt, h * 64:(h + 1) * 64], Yn, 64)
                    store_branch(2, Yn)
            if CFG['phases'] == 'all' or 'C' in CFG['phases']:
              with fw.scope():
                nqt = NQT if want_ctx else 16
                H2T = fw.sb("H2T", [128, 8, NQT * 128], BF16)
                WT = fw.sb("WT", [128, NQT, 16], F32)
                xslot = lambda qt: (3 + qt) if qt < 16 else (22 + qt - 16)
                with fw.scope():
                    G1 = [fw.sb("G1_%d" % w, [128, D], F32) for w in range(2)]
                    A2 = [fw.sb("A2_%d" % w, [128, D], F32) for w in range(2)]
                    SH2 = [fw.sb("SH2_%d" % w, [128, D], F32) for w in range(2)]
                    for w in range(2):
                        load_mod(G1[w], w, M_G1)
                        load_mod(A2[w], w, M_A2)
                        load_mod(SH2[w], w, M_SH2)
                    wbr = fw.sb("wbr", [128, 16, D], BF16)
                    wo = fw.sb("wo", [128, 8, D], BF16)
                    rw = fw.sb("rw", [128, 8, 16], F32)
                    for q4 in range(4):
                        dma("pool", wbr.t[:, q4 * 4:(q4 + 1) * 4, :], w_branch[q4 * 512:(q4 + 1) * 512, :].rearrange("(j p) c -> p j c", p=128), [], [wbr])
                    for q2 in range(2):
                        dma("pool", wo.t[:, q2 * 4:(q2 + 1) * 4, :], w_out[q2 * 512:(q2 + 1) * 512, :].rearrange("(j p) c -> p j c", p=128), [], [wo])
                    dma("sp", rw.t[:], router_w.rearrange("(j p) e -> p j e", p=128), [], [rw])
                    yt = [fw.sb("yt%d" % i, [128, 2048], BF16) for i in range(2)]
                    gt_ = [fw.sb("gtc%d" % i, [128, 4096], BF16) for i in range(2)]
                    sg = fw.sb("sg", [128, 4096], BF16)
                    YT = fw.sb("YT", [128, 16, 128], BF16)
                    macc = fw.sb("macc", [128, D], F32)
                    mtmp = fw.sb("mtmp", [128, 512], F32)
                    mbf = fw.sb("mbf", [128, D], BF16)
                    mT = fw.sb("mT", [128, 8, 128], BF16)
                    xin = [fw.sb("xin%d" % i, [128, D], F32) for i in range(2)]
                    xn = [fw.sb("xn%d" % i, [128, D], F32) for i in range(2)]
                    junk2 = fw.sb("junk2", [128, D], F32)
                    h2 = fw.sb("h2", [128, D], F32)
                    h2tf = fw.sb("h2tf", [128, 8, 128], F32)
                    s2 = [fw.sb("s2_%d" % i, [128, 8], F32) for i in range(2)]
                    rr = [fw.sb("rr%d" % i, [128, 160], F32) for i in range(2)]
                    ptc = [fw.ps("ptc%d" % i, [128, D], BF16) for i in range(2)]
                    pm = [fw.ps("pm%d" % i, [128, 512], F32) for i in range(3)]
                    pf = [fw.ps("pf%d" % i, [128, 512], F32) for i in range(2)]
                    pr = fw.ps("pr", [128, 16], F32)
                    pmc = [0]
                    for qt in range(nqt):
                        which = 1 if qt >= 16 else 0
                        y_, g_, x_, xn_, s_, r_ = yt[qt % 2], gt_[qt % 2], xin[qt % 2], xn[qt % 2], s2[qt % 2], rr[qt % 2]
                        dma("sp", y_.t[:], Y[qt * 128:(qt + 1) * 128, :], [], [y_])
                        dma("sp", g_.t[:], GATES[qt * 128:(qt + 1) * 128, :], [], [g_])
                        dma("sp", x_.t[:], local_src(xslot(qt)), [], [x_])
                        act(sg.t[:], g_.t[:], AF.Sigmoid, [g_], [sg])
                        for hf in range(2):
                            p_ = ptc[hf]
                            for i in range(8):
                                tr(p_.t[:, i * 128:(i + 1) * 128], y_.t[:, (hf * 8 + i) * 128:(hf * 8 + i + 1) * 128], identb.t[:], [y_, identb], [p_])
                            cp("dve" if hf else "act", YT.t[:, hf * 8:(hf + 1) * 8, :].rearrange("p a b -> p (a b)"), p_.t[:], [p_], [YT])
                        for n in range(4):
                            for half in range(2):
                                p_ = pm[pmc[0] % 3]
                                pmc[0] += 1
                                for k in range(4):
                                    mm(p_.t[:], YT.t[:, n * 4 + k, :], wbr.t[:, n * 4 + k, half * 512:(half + 1) * 512], k == 0, k == 3, [YT, wbr], [p_])
                                sgs = sg.t[:, n * 1024 + half * 512:n * 1024 + (half + 1) * 512]
                                if n == 0:
                                    tt("dve", macc.t[:, half * 512:(half + 1) * 512], p_.t[:], sgs, ALU.mult, [p_, sg], [macc])
                                else:
                                    tt("dve", mtmp.t[:], p_.t[:], sgs, ALU.mult, [p_, sg], [mtmp])
                                    tt("pool", macc.t[:, half * 512:(half + 1) * 512], macc.t[:, half * 512:(half + 1) * 512], mtmp.t[:], ALU.add, [macc, mtmp], [macc])
                        cp("act", mbf.t[:], macc.t[:], [macc], [mbf])
                        p_ = ptc[0]
                        for i in range(8):
                            tr(p_.t[:, i * 128:(i + 1) * 128], mbf.t[:, i * 128:(i + 1) * 128], identb.t[:], [mbf, identb], [p_])
                        cp("dve", mT.t[:].rearrange("p a b -> p (a b)"), p_.t[:], [p_], [mT])
                        for half in range(2):
                            p_ = pm[pmc[0] % 3]
                            pmc[0] += 1
                            for j in range(8):
                                mm(p_.t[:], mT.t[:, j, :], wo.t[:, j, half * 512:(half + 1) * 512], j == 0, j == 7, [mT, wo], [p_])
                            hs_ = slice(half * 512, (half + 1) * 512)
                            tt("dve", mtmp.t[:], p_.t[:], G1[which].t[:, hs_], ALU.mult, [p_, G1[which]], [mtmp])
                            tt("pool", xn_.t[:, hs_], mtmp.t[:], x_.t[:, hs_], ALU.add, [mtmp, x_], [xn_])
                        dma("sp", XN[qt * 128:(qt + 1) * 128, :], xn_.t[:], [xn_], [])
                        act(junk2.t[:], xn_.t[:], AF.Square, [xn_], [junk2, s_], accum_out=s_.t[:, 0:1])
                        act(s_.t[:, 1:2], s_.t[:, 0:1], AF.Sqrt, [s_], [s_], bias=EPS, scale=1.0 / D)
                        op("dve", lambda e: e.reciprocal(s_.t[:, 2:3], s_.t[:, 1:2]), [s_], [s_])
                        stt("dve", h2.t[:], xn_.t[:], s_.t[:, 2:3], A2[which].t[:], ALU.mult, ALU.mult, [xn_, s_, A2[which]], [h2])
                        tt("pool", h2.t[:], h2.t[:], SH2[which].t[:], ALU.add, [h2, SH2[which]], [h2])
                        for hf in range(2):
                            p_ = pf[hf]
                            for i in range(4):
                                j = hf * 4 + i
                                tr(p_.t[:, i * 128:(i + 1) * 128], h2.t[:, j * 128:(j + 1) * 128], identf.t[:], [h2, identf], [p_])
                            cp("dve" if hf else "act", h2tf.t[:, hf * 4:(hf + 1) * 4, :].rearrange("p a b -> p (a b)"), p_.t[:], [p_], [h2tf])
                        cp("pool", H2T.t[:, :, qt * 128:(qt + 1) * 128], h2tf.t[:], [h2tf], [H2T])
                        for j in range(8):
                            mm(pr.t[:], h2tf.t[:, j, :], rw.t[:, j, :], j == 0, j == 7, [h2tf, rw], [pr])
                        R = lambda a, b: r_.t[:, a:b]
                        v4 = lambda ap: ap.rearrange("p (g e) -> p g e", g=4)
                        act(R(0, 16), pr.t[:], AF.Sigmoid, [pr], [r_])
                        tt("dve", R(16, 32), R(0, 16), gv(V_RB, 16), ALU.add, [r_, VEC], [r_])
                        op("dve", lambda e: e.tensor_reduce(R(32, 36), v4(R(16, 32)), AX.X, ALU.max), [r_], [r_])
                        tt("dve", v4(R(48, 64)), v4(R(16, 32)), R(32, 36).unsqueeze(2).to_broadcast([128, 4, 4]), ALU.is_equal, [r_], [r_])
                        stt("dve", R(64, 80), R(48, 64), -1e9, R(16, 32), ALU.mult, ALU.add, [r_], [r_])
                        op("dve", lambda e: e.tensor_reduce(R(36, 40), v4(R(64, 80)), AX.X, ALU.max), [r_], [r_])
                        tt("dve", R(40, 44), R(32, 36), R(36, 40), ALU.add, [r_], [r_])
                        op("dve", lambda e: e.tensor_reduce(R(44, 45), R(40, 44), AX.X, ALU.max), [r_], [r_])
                        ts("dve", R(80, 84), R(40, 44), R(44, 45), None, ALU.is_equal, None, [r_], [r_])
                        tt("dve", v4(R(96, 112)), v4(R(16, 32)), R(36, 40).unsqueeze(2).to_broadcast([128, 4, 4]), ALU.is_ge, [r_], [r_])
                        tt("dve", v4(R(96, 112)), v4(R(96, 112)), R(80, 84).unsqueeze(2).to_broadcast([128, 4, 4]), ALU.mult, [r_], [r_])
                        tt("dve", R(112, 128), R(96, 112), R(0, 16), ALU.mult, [r_], [r_])
                        op("dve", lambda e: e.tensor_reduce(R(128, 129), R(112, 128), AX.X, ALU.add), [r_], [r_])
                        op("dve", lambda e: e.reciprocal(R(129, 130), R(128, 129)), [r_], [r_])
                        ts("dve", WT.t[:, qt, :], R(112, 128), R(129, 130), None, ALU.mult, None, [r_], [WT])
                    if False:
                        dbg_wt = dscr("dbg_wt", [128, NQT * 16], F32)
                        dma("sp", dbg_wt, WT.t[:].rearrange("p a b -> p (a b)"), [WT], [])
                with fw.scope():
                    Ft = [fw.sb("F%d" % i, [128, D], F32) for i in range(nqt)]
                    for f_ in Ft:
                        op("pool", lambda e: e.memset(f_.t[:], 0.0), [], [f_])
                    w1 = [fw.sb("w1_%d" % i, [128, 8, 512], BF16) for i in range(2)]
                    w3 = [fw.sb("w3_%d" % i, [128, 8, 512], BF16) for i in range(2)]
                    w2 = [fw.sb("w2_%d" % i, [128, 4, D], BF16) for i in range(2)]
                    GT = [fw.sb("GT%d" % i, [128, 4, 512], BF16) for i in range(2)]
                    sl = [fw.sb("sl%d" % i, [128, 512], F32) for i in range(2)]
                    pa = [fw.ps("pa%d" % i, [128, 512], F32) for i in range(2)]
                    pb = [fw.ps("pb%d" % i, [128, 512], F32) for i in range(2)]
                    py = [fw.ps("py%d" % i, [128, 512], F32) for i in range(3)]
                    blocks = [(i * 512, 512) for i in range(4)] + ([(2048, 256)] if want_ctx else [])
                    c1, c2 = [0], [0]
                    for e_ in range(CFG.get('nexp', 16)):
                        a1, a3, a2 = w1[e_ % 2], w3[e_ % 2], w2[e_ % 2]
                        dma("pool", a1.t[:], moe_w1[e_].rearrange("(j p) c -> p j c", p=128), [], [a1])
                        dma("pool", a3.t[:], moe_w3[e_].rearrange("(j p) c -> p j c", p=128), [], [a3])
                        dma("pool", a2.t[:], moe_w2[e_].rearrange("(j p) c -> p j c", p=128), [], [a2])
                        for bi_, (t0, nb) in enumerate(blocks):
                            G_ = GT[(e_ * 5 + bi_) % 2]
                            for m in range(4):
                                pa_, pb_, sl_ = pa[c1[0] % 2], pb[c1[0] % 2], sl[c1[0] % 2]
                                c1[0] += 1
                                for k in range(8):
                                    mm(pa_.t[:, :nb], a1.t[:, k, m * 128:(m + 1) * 128], H2T.t[:, k, t0:t0 + nb], k == 0, k == 7, [a1, H2T], [pa_])
                                for k in range(8):
                                    mm(pb_.t[:, :nb], a3.t[:, k, m * 128:(m + 1) * 128], H2T.t[:, k, t0:t0 + nb], k == 0, k == 7, [a3, H2T], [pb_])
                                act(sl_.t[:, :nb], pa_.t[:, :nb], AF.Silu, [pa_], [sl_])
                                tt("dve", G_.t[:, m, :nb], sl_.t[:, :nb], pb_.t[:, :nb], ALU.mult, [sl_, pb_], [G_])
                            for j in range(nb // 128):
                                qt = t0 // 128 + j
                                for half in range(2):
                                    py_ = py[c2[0] % 3]
                                    c2[0] += 1
                                    for m in range(4):
                                        mm(py_.t[:], G_.t[:, m, j * 128:(j + 1) * 128], a2.t[:, m, half * 512:(half + 1) * 512], m == 0, m == 3, [G_, a2], [py_])
                                    fs = Ft[qt].t[:, half * 512:(half + 1) * 512]
                                    stt("dve", fs, py_.t[:], WT.t[:, qt, e_:e_ + 1], fs, ALU.mult, ALU.add, [py_, WT, Ft[qt]], [Ft[qt]])
                    G2 = [fw.sb("G2_%d" % w, [128, D], F32) for w in range(2)]
                    for w in range(2):
                        load_mod(G2[w], w, M_G2)
                    xo = [fw.sb("xo%d" % i, [128, D], F32) for i in range(2)]
                    for qt in range(nqt):
                        which = 1 if qt >= 16 else 0
                        x_ = xo[qt % 2]
                        dma("sp", x_.t[:], XN[qt * 128:(qt + 1) * 128, :], [], [x_])
                        tt("dve", Ft[qt].t[:], Ft[qt].t[:], G2[which].t[:], ALU.mult, [Ft[qt], G2[which]], [Ft[qt]])
                        tt("pool", x_.t[:], x_.t[:], Ft[qt].t[:], ALU.add, [x_, Ft[qt]], [x_])
                        if lidx == 1:
                            dma("sp", x_out[qt * 128:(qt + 1) * 128, :], x_.t[:], [x_], [], out=True)
                        elif qt < 16:
                            dma("sp", X1[(seg * 16 + qt) * 128:(seg * 16 + qt + 1) * 128, :], x_.t[:], [x_], [])
                        else:
                            dma("sp", XC1[(qt - 16) * 128:(qt - 15) * 128, :], x_.t[:], [x_], [])

        for lidx in layers:
            for k_, seg in enumerate(segs0 if lidx == 0 else (0,)):
                layer_pass(lidx, seg, lidx == 0 and seg == 0, k_ == 0)
        fw.finish()
    return nc


def _rope_tables():
    t = np.arange(SEQ)
    rows = (t // 64).astype(np.float32)
    cols = (t % 64).astype(np.float32)
    out = []
    for dim in (64, 32):
        quarter = dim // 4
        inv = np.exp(-np.log(10000.0) * np.arange(quarter, dtype=np.float32) / quarter).astype(np.float32)
        ang = np.concatenate([rows[:, None] * inv, cols[:, None] * inv], axis=-1).astype(np.float32)
        out += [np.cos(ang).astype(np.float32), np.sin(ang).astype(np.float32)]
    tab = np.concatenate(out, axis=1)
    ident = np.zeros((1, 96), np.float32)
    ident[0, 0:32] = 1.0
    ident[0, 64:80] = 1.0
    return tab, ident


def _nat_consts(s):
    col = np.arange(64)
    cs = np.clip(col - 8, 0, 48)
    col_ok = (col[None, :] >= cs[:, None]) & (col[None, :] < cs[:, None] + 16)
    reps = (0, 1, 7, 14, 15)
    V = np.zeros((5, 7, 128, 128), np.float32)
    for ci, tl in enumerate(reps):
        t = 16 * s + tl
        for c in range(-3, 4):
            u = t + c
            if not (0 <= u < 64):
                continue
            for kr in range(2):
                for qr in range(2):
                    krow, qrow = 2 * u + kr, 2 * t + qr
                    st = min(max(qrow - 4, 0), 120)
                    if st <= krow < st + 8:
                        V[ci, c + 3, kr * 64:(kr + 1) * 64, qr * 64:(qr + 1) * 64] = col_ok.T
    return V


def _nat_bias_index():
    k = np.arange(128)
    q = np.arange(128)
    kr, kc = k // 64, k % 64
    qr, qc = q // 64, q % 64
    dcol = np.clip(kc[:, None] - qc[None, :] + 15, 0, 30)
    idx_r = np.zeros((7, 128, 128), np.int64)
    for c in range(-3, 4):
        idx_r[c + 3] = np.clip(2 * c + kr[:, None] - qr[None, :] + 7, 0, 14)
    return idx_r, np.broadcast_to(dcol, (7, 128, 128))


def prep_fused(inp):
    f32 = lambda a: np.ascontiguousarray(a, dtype=np.float32)
    tab, rid = _rope_tables()
    x = np.asarray(inp['x'], dtype=np.float32)
    xc = np.asarray(inp['ctx'], dtype=np.float32)
    idx_r, idx_c = _nat_bias_index()
    tri_prev = (np.arange(128)[:, None] >= np.arange(128)[None, :]).astype(np.float32)
    tri_next = (np.arange(128)[:, None] <= np.arange(128)[None, :]).astype(np.float32)
    common = dict(ident=np.eye(128, dtype=np.float32), router_w=f32(inp['router_w']))
    for l in range(2):
        vecs = [inp['win_q_norm'][l], inp['win_k_norm'][l], inp['dif_q_norm'][l], inp['dif_k_norm'][l],
                inp['nat_q_norm'][l], inp['nat_k_norm'][l], inp['mla_q_norm'][l], inp['mla_k_norm'][l],
                inp['mla_q_a_norm'][l], inp['mla_kv_a_norm'][l], inp['dif_subln'][l], inp['win_sink'][l],
                inp['dif_lambda'][l].reshape(-1), inp['router_b'], inp['ada_b'][l], inp['norm1_g'][l], inp['norm2_g'][l]]
        vec = f32(np.concatenate([np.asarray(v).reshape(-1) for v in vecs])[None, :])
        assert vec.shape[1] == NVEC
        rpb = np.asarray(inp['nat_rpb'][l])
        common.update({
            'vec_%d' % l: vec, 'ada_w_%d' % l: f32(inp['ada_w'][l]), 'w_in_%d' % l: f32(inp['w_in'][l]),
            'wq_b_%d' % l: f32(inp['mla_wq_b'][l]), 'wkv_b_%d' % l: f32(inp['mla_wkv_b'][l]),
            'w_branch_%d' % l: f32(np.asarray(inp['w_branch'][l]).reshape(2048, D)), 'w_out_%d' % l: f32(inp['w_out'][l]),
            'moe_w1_%d' % l: f32(inp['moe_w1'][l]), 'moe_w3_%d' % l: f32(inp['moe_w3'][l]), 'moe_w2_%d' % l: f32(inp['moe_w2'][l]),
            'natb_%d' % l: f32(np.transpose(rpb[:, idx_r, idx_c], (1, 2, 0, 3)))})
    maps = []
    zeros_h = np.zeros((384, D), np.float32)
    rid384 = np.repeat(rid, 384, 0)
    rc = np.repeat(rid, NCTX, 0)
    for core in range(8):
        b, s = core // 4, core % 4
        sigma = [s] + [g for g in range(4) if g != s]
        m = dict(common)
        m['xall'] = f32(np.concatenate([xc[b]] + [x[b, 2048 * g:2048 * (g + 1)] for g in sigma], 0))
        m['rope_all'] = f32(np.concatenate([rc] + [tab[2048 * g:2048 * (g + 1)] for g in sigma], 0))
        for j, g in enumerate(sigma):
            o0 = 2048 * g
            hb = x[b, o0 - 384:o0] if g > 0 else zeros_h
            ha = x[b, o0 + 2048:o0 + 2048 + 384] if g < 3 else zeros_h
            rb = tab[o0 - 384:o0] if g > 0 else rid384
            ra = tab[o0 + 2048:o0 + 2048 + 384] if g < 3 else rid384
            m['xloc%d' % j] = f32(np.concatenate([hb, x[b, o0:o0 + 2048], ha, xc[b]], 0))
            m['rope_loc%d' % j] = f32(np.concatenate([rb, tab[o0:o0 + 2048], ra, rc], 0))
            m['wmask%d' % j] = f32(np.stack([tri_prev if g > 0 else np.zeros_like(tri_prev), tri_prev, tri_next,
                                             tri_next if g < 3 else np.zeros_like(tri_next)], 0))
            m['natv%d' % j] = f32(_nat_consts(g))
        hs = np.zeros((128, 8), np.float32)
        for r in (1, 2, 3):
            if sigma[r] == s - 1:
                hs[:, r] = 1.0
            if sigma[r] == s + 1:
                hs[:, 4 + r] = 1.0
        m['hsel'] = hs
        m['cvec'] = f32(np.stack([inp['c'][b], inp['c_ctx']], 0).reshape(2, 8, 128).transpose(2, 0, 1).reshape(128, 16))
        maps.append(m)
    return maps


def kernel(**inputs):
    inp = {k: np.asarray(v) for k, v in inputs.items()}
    nc = build_fused()
    maps = prep_fused(inp)
    res = run_bass_kernel_spmd(nc, maps, core_ids=list(range(8)))
    out = np.empty((2, SEQ, D), np.float32)
    for core in range(8):
        b, s = core // 4, core % 4
        out[b, 2048 * s:2048 * (s + 1)] = np.asarray(res.results[core]['x_out'])
    return out
```

```python
import numpy as np
import ml_dtypes
import concourse.bass as bass
import concourse.mybir as mybir
from concourse.bass_utils import run_bass_kernel_spmd

F32 = mybir.dt.float32
BF16 = mybir.dt.bfloat16
I32 = mybir.dt.int32
AF = mybir.ActivationFunctionType
ALU = mybir.AluOpType
AX = mybir.AxisListType


class Buf:
    __slots__ = ("t", "w", "r", "name")

    def __init__(self, t, name=""):
        self.t = t
        self.w = None
        self.r = {}
        self.name = name


class _Eng:
    def __init__(self, name, eng, sem):
        self.name, self.eng, self.sem = name, eng, sem
        self.count = 0
        self.seen = {}
        self.dq = []
        self.dn = 0


class FW:
    NDMA = 6

    def __init__(self, nc):
        self.nc = nc
        self.stack = None
        self.E = {}
        self.out_tokens = []

    def __enter__(self):
        from contextlib import ExitStack
        self.stack = ExitStack()
        self.stack.__enter__()
        nc = self.nc
        for name, eng in (("pe", nc.tensor), ("act", nc.scalar), ("dve", nc.vector),
                          ("pool", nc.gpsimd), ("sp", nc.sync)):
            sem = self.stack.enter_context(nc.semaphore("s_" + name))
            self.E[name] = _Eng(name, eng, sem)
        for q in ("sp", "act", "pool"):
            for i in range(self.NDMA):
                self.E[q].dq.append(self.stack.enter_context(nc.semaphore("d_%s%d" % (q, i))))
        return self

    def __exit__(self, *a):
        return self.stack.__exit__(*a)

    nalloc = 0

    def sb(self, name, shape, dt):
        self.nalloc += 1
        name = "%s_u%d" % (name, self.nalloc)
        return Buf(self.stack.enter_context(self.nc.sbuf_tensor(name, list(shape), dt)), name)

    def ps(self, name, shape, dt=F32):
        self.nalloc += 1
        name = "%s_u%d" % (name, self.nalloc)
        return Buf(self.stack.enter_context(self.nc.psum_tensor(name, list(shape), dt)), name)

    def scope(self):
        fw = self

        class _S:
            def __enter__(s):
                from contextlib import ExitStack
                s.prev = fw.stack
                fw.stack = ExitStack()
                fw.stack.__enter__()
                return s

            def __exit__(s, *a):
                fw.barrier()
                r = fw.stack.__exit__(*a)
                fw.stack = s.prev
                return r
        return _S()

    def _wait(self, E, tok):
        sem, val = tok
        if E.name == "pe" and sem is E.sem:
            return
        key = id(sem)
        if E.seen.get(key, 0) >= val:
            return
        E.eng.wait_ge(sem, val)
        E.seen[key] = val

    def _deps(self, E, reads, writes):
        toks = []
        for b in reads:
            if b.w is not None:
                toks.append(b.w)
        for b in writes:
            if b.w is not None:
                toks.append(b.w)
            toks.extend(b.r.values())
        for tok in toks:
            self._wait(E, tok)

    def _mark(self, tok, reads, writes):
        key = id(tok[0])
        for b in reads:
            old = b.r.get(key)
            if old is None or old[1] < tok[1]:
                b.r[key] = tok
        for b in writes:
            b.w = tok
            b.r = {}

    nops = 0
    maxops = 10 ** 9
    rec = None

    def op(self, en, fn, reads=(), writes=()):
        if self.rec is not None:
            self.rec.append((0, en, fn, tuple(reads), tuple(writes)))
            return
        self.nops += 1
        if self.nops > self.maxops:
            return
        E = self.E[en]
        self._deps(E, reads, writes)
        ins = fn(E.eng)
        E.count += 1
        ins.then_inc(E.sem, 1)
        self._mark((E.sem, E.count), reads, writes)

    def dma(self, q, out_ap, in_ap, reads=(), writes=(), out=False):
        if self.rec is not None:
            self.rec.append((1, q, out_ap, in_ap, tuple(reads), tuple(writes), out))
            return
        self.nops += 1
        if self.nops > self.maxops:
            return
        E = self.E[q]
        self._deps(E, reads, writes)
        k = E.dn % self.NDMA
        rnd = E.dn // self.NDMA
        sem = E.dq[k]
        if rnd > 0:
            self._wait(E, (sem, 16 * rnd))
        E.eng.dma_start(out=out_ap, in_=in_ap).then_inc(sem, 16)
        E.dn += 1
        tok = (sem, 16 * (rnd + 1))
        self._mark(tok, reads, writes)
        if out:
            self.out_tokens.append(tok)
        return tok

    def emit_interleaved(self, streams):
        n = max(len(st) for st in streams)
        for i in range(n):
            for st in streams:
                if i < len(st):
                    r = st[i]
                    if r[0] == 0:
                        self.op(r[1], r[2], r[3], r[4])
                    else:
                        self.dma(r[1], r[2], r[3], r[4], r[5], r[6])

    def barrier(self):
        toks = []
        for E in self.E.values():
            if E.count:
                toks.append((E.sem, E.count))
            for k, sem in enumerate(E.dq):
                n = (E.dn - k + self.NDMA - 1) // self.NDMA
                if n > 0:
                    toks.append((sem, 16 * n))
        for E in self.E.values():
            for tok in toks:
                sem, val = tok
                key = id(sem)
                if E.seen.get(key, 0) >= val:
                    continue
                E.eng.wait_ge(sem, val)
                E.seen[key] = val

    def finish(self):
        self.barrier()


D = 1024
SEQ = 8192
NCTX = 256
NT_ALL = 66
NT_LOC = 24
NQT = 18
EPS = 1e-6
C_WQ, C_WK, C_WV, C_DQ, C_DK, C_DV, C_NQ, C_NK, C_NV, C_MQA, C_MKVA, C_G = (
    0, 512, 640, 768, 1280, 1792, 2304, 2816, 3328, 3840, 4096, 4256)
V_WQN, V_WKN, V_DQN, V_DKN, V_NQN, V_NKN = 0, 64, 128, 192, 256, 320
V_MQN, V_MKN, V_MQAN, V_MKVAN, V_SUBLN, V_SINK, V_LAM, V_RB = 384, 480, 576, 832, 960, 1088, 1096, 1352
V_ADAB, V_N1, V_N2 = 1368, 1368 + 6144, 1368 + 6144 + 1024
NVEC = V_N2 + 1024
NSMALL = 1368


def bcast_rows(ap2d, row, c0, n, parts=128):
    t = ap2d.tensor
    cols = ap2d.shape[1]
    return bass.AP(t, ap2d.offset + row * cols + c0, [[0, parts], [1, n]])


CFG = {'phases': 'all', 'nA1': NT_ALL, 'nA2': NT_LOC}


def build_fused(debug=False, layers=(0, 1), segs0=(0, 1, 2, 3)):
    nc = bass.Bass("TRN2", target_bir_lowering=False)

    def din(name, shape, dt=F32):
        return nc.dram_tensor(name, list(shape), dt, kind="ExternalInput").ap()

    def dscr(name, shape, dt=BF16, out=False):
        kind = "ExternalOutput" if (out or debug) else "Internal"
        return nc.dram_tensor(name, list(shape), dt, kind=kind).ap()

    xall = din("xall", [NT_ALL * 128, D])
    rope_all = din("rope_all", [NT_ALL * 128, 96])
    xloc_s = [din("xloc%d" % j, [NT_LOC * 128, D]) for j in range(4)]
    rope_loc_s = [din("rope_loc%d" % j, [NT_LOC * 128, 96]) for j in range(4)]
    wmask_s = [din("wmask%d" % j, [4, 128, 128]) for j in range(4)]
    natv_s = [din("natv%d" % j, [5, 7, 128, 128]) for j in range(4)]
    hsel_in = din("hsel", [128, 8])
    cvec = din("cvec", [128, 16])
    ident_in = din("ident", [128, 128])
    router_w = din("router_w", [D, 16])
    LW = []
    for l in range(2):
        LW.append(dict(
            vec=din("vec_%d" % l, [1, NVEC]), ada_w=din("ada_w_%d" % l, [D, 6 * D]), w_in=din("w_in_%d" % l, [D, 8352]),
            wq_b=din("wq_b_%d" % l, [256, 768]), wkv_b=din("wkv_b_%d" % l, [128, 1024]),
            w_branch=din("w_branch_%d" % l, [2048, D]), w_out=din("w_out_%d" % l, [D, D]),
            moe_w1=din("moe_w1_%d" % l, [16, D, 512]), moe_w3=din("moe_w3_%d" % l, [16, D, 512]),
            moe_w2=din("moe_w2_%d" % l, [16, 512, D]), natb=din("natb_%d" % l, [7, 128, 8, 128])))

    x_out = dscr("x_out", [2048, D], F32, out=True)
    X1 = dscr("X1", [SEQ, D], F32)
    XC1 = dscr("XC1", [NCTX, D], F32)

    KD = dscr("KD", [512, NT_ALL * 128])
    VD = dscr("VD", [NT_ALL * 128, 512])
    KM = dscr("KM", [8, 96, NT_ALL * 128])
    VM = dscr("VM", [NT_ALL * 128, 512])
    KW = dscr("KW", [128, NT_LOC * 128])
    VW = dscr("VW", [NT_LOC * 128, 128])
    KN = dscr("KN", [512, NT_LOC * 128])
    VN = dscr("VN", [NT_LOC * 128, 512])
    QW = dscr("QW", [512, NQT * 128])
    QD = dscr("QD", [512, NQT * 128])
    QN = dscr("QN", [512, NQT * 128])
    QM = dscr("QM", [8, 96, NQT * 128])
    GATES = dscr("GATES", [NQT * 128, 4096])
    Y = dscr("Y", [NQT * 128, 2048])
    XN = dscr("XN", [NQT * 128, D], F32)
    MODS = dscr("MODS", [2, 128, 6 * D], F32)
    HTS = dscr("HTS", [NQT, 128, D])

    fw = FW(nc)
    with fw:
        op, dma = fw.op, fw.dma

        def mm(out, lhsT, rhs, start, stop, reads, writes):
            op("pe", lambda e: e.matmul(out, lhsT, rhs, start=start, stop=stop), reads, writes)

        def tr(out, in_, idn, reads, writes):
            op("pe", lambda e: e.transpose(out, in_, idn), reads, writes)

        def tt(en, out, a, b, alu, reads, writes):
            op(en, lambda e: e.tensor_tensor(out, a, b, alu), reads, writes)

        def ts(en, out, a, s1, s2, o0, o1, reads, writes):
            if s2 is None:
                op(en, lambda e: e.tensor_scalar(out, a, s1, None, o0), reads, writes)
            else:
                op(en, lambda e: e.tensor_scalar(out, a, s1, s2, o0, o1), reads, writes)

        def stt(en, out, a, s, b, o0, o1, reads, writes):
            op(en, lambda e: e.scalar_tensor_tensor(out, a, s, b, o0, o1), reads, writes)

        def act(out, in_, func, reads, writes, **kw):
            op("act", lambda e: e.activation(out, in_, func, **kw), reads, writes)

        def cp(en, out, in_, reads, writes):
            if en == "act":
                act(out, in_, AF.Copy, reads, writes)
            else:
                op(en, lambda e: e.tensor_copy(out, in_), reads, writes)

        identb = fw.sb("identb", [128, 128], BF16)
        identf = fw.sb("identf", [128, 128], F32)
        VEC = fw.sb("VEC", [128, NSMALL], F32)
        dma("pool", identb.t[:], ident_in, [], [identb])
        dma("sp", identf.t[:], ident_in, [], [identf])
        gv = lambda off, n: VEC.t[:, off:off + n]

        HSEL = fw.sb("HSEL", [128, 8], F32)
        dma("sp", HSEL.t[:], hsel_in, [], [HSEL])

        def layer_pass(lidx, seg, want_ctx, first):
            lam_init = 0.8 - 0.6 * float(np.exp(-0.3 * lidx))
            W_ = LW[lidx]
            vec, ada_w, w_in, wq_b, wkv_b = W_["vec"], W_["ada_w"], W_["w_in"], W_["wq_b"], W_["wkv_b"]
            w_branch, w_out, moe_w1, moe_w3, moe_w2, natb = W_["w_branch"], W_["w_out"], W_["moe_w1"], W_["moe_w3"], W_["moe_w2"], W_["natb"]
            rope_loc, wmask, natv = rope_loc_s[seg], wmask_s[seg], natv_s[seg]
            if first:
                dma("sp", VEC.t[:], bcast_rows(vec, 0, 0, NSMALL), [], [VEC])

            def dense_src(i):
                if lidx == 0:
                    return xall[i * 128:(i + 1) * 128, :]
                return XC1[i * 128:(i + 1) * 128, :] if i < 2 else X1[(i - 2) * 128:(i - 1) * 128, :]

            def local_src(i):
                if lidx == 0:
                    return xloc_s[seg][i * 128:(i + 1) * 128, :]
                if 3 <= i < 19:
                    return X1[(i - 3) * 128:(i - 2) * 128, :]
                if i >= 22:
                    return XC1[(i - 22) * 128:(i - 21) * 128, :]
                if i < 3:
                    return [(X1[(r * 16 + 13 + i) * 128:(r * 16 + 14 + i) * 128, :], HSEL.t[:, r:r + 1]) for r in (1, 2, 3)]
                return [(X1[(r * 16 + i - 19) * 128:(r * 16 + i - 18) * 128, :], HSEL.t[:, 4 + r:5 + r]) for r in (1, 2, 3)]

            if first:
              with fw.scope():
                cs = fw.sb("cs", [128, 2, 8], F32)
                CB = fw.sb("CB", [128, 2, 8, 128], F32)
                NG = fw.sb("NG", [128, 2, D], F32)
                dma("sp", cs.t[:].rearrange("p w j -> p (w j)"), cvec, [], [cs])
                dma("sp", NG.t[:, 0, :], bcast_rows(vec, 0, V_N1, D), [], [NG])
                dma("sp", NG.t[:, 1, :], bcast_rows(vec, 0, V_N2, D), [], [NG])
                act(cs.t[:], cs.t[:], AF.Silu, [cs], [cs])
                cp("dve", CB.t[:], cs.t[:].unsqueeze(3).to_broadcast([128, 2, 8, 128]), [cs], [CB])
                aw = [fw.sb("aw%d" % i, [128, 8, 512], F32) for i in range(2)]
                ab = [fw.sb("ab%d" % i, [128, 512], F32) for i in range(2)]
                mps = [fw.ps("mps%d" % i, [128, 512], F32) for i in range(2)]
                mo = [fw.sb("mo%d" % i, [128, 512], F32) for i in range(2)]
                n = 0
                for blk in range(12):
                    a_, b_ = aw[blk % 2], ab[blk % 2]
                    dma("sp", a_.t[:], ada_w[:, blk * 512:(blk + 1) * 512].rearrange("(j p) c -> p j c", p=128), [], [a_])
                    dma("sp", b_.t[:], bcast_rows(vec, 0, V_ADAB + blk * 512, 512), [], [b_])
                    chunk = blk // 2
                    half = blk % 2
                    for w in range(2):
                        p_, o_ = mps[n % 2], mo[n % 2]
                        n += 1
                        for j in range(8):
                            mm(p_.t[:], CB.t[:, w, j, :], a_.t[:, j, :], j == 0, j == 7, [CB, a_], [p_])
                        tt("dve", o_.t[:], p_.t[:], b_.t[:], ALU.add, [p_, b_], [o_])
                        if chunk in (1, 4):
                            g = NG.t[:, 0 if chunk == 1 else 1, half * 512:(half + 1) * 512]
                            stt("dve", o_.t[:], o_.t[:], 1.0, g, ALU.add, ALU.mult, [o_, NG], [o_])
                        dma("sp", MODS[w, :, blk * 512:(blk + 1) * 512], o_.t[:], [o_], [])
            M_SH1, M_A1, M_G1, M_SH2, M_A2, M_G2 = [i * D for i in range(6)]

            def load_mod(buf, which, off):
                dma("sp", buf.t[:], MODS[which, :, off:off + D], [], [buf])

            with fw.scope():
                A1 = [fw.sb("A1_%d" % w, [128, D], F32) for w in range(2)]
                SH1 = [fw.sb("SH1_%d" % w, [128, D], F32) for w in range(2)]
                for w in range(2):
                    load_mod(A1[w], w, M_A1)
                    load_mod(SH1[w], w, M_SH1)
                wkvb = fw.sb("wkvb", [128, 1024], BF16)
                wqb = fw.sb("wqb", [128, 2, 768], BF16)
                dma("pool", wkvb.t[:], wkv_b, [], [wkvb])
                dma("pool", wqb.t[:], wq_b.rearrange("(j p) c -> p j c", p=128), [], [wqb])
                junk = fw.sb("junk", [128, D], F32)

                class _Set:
                    pass

                def mkset(k):
                    S = _Set()
                    S.xt = fw.sb("xt", [128, D], F32)
                    S.rp = fw.sb("rp", [128, 96], F32)
                    S.st = fw.sb("st", [128, 16], F32)
                    S.hb = fw.sb("hb", [128, D], BF16)
                    S.hT = fw.sb("hT", [128, D], BF16)
                    S.ptr = fw.ps("ptr", [128, D], BF16)
                    S.pp = [fw.ps("pp", [128, 512], F32) for _ in range(3)]
                    S.xs = fw.sb("xs", [128, 1024], F32)
                    S.sq = fw.sb("sq", [128, 1024], F32)
                    S.ss = fw.sb("ss", [128, 32], F32)
                    S.yb = fw.sb("yb", [128, 1024], F32)
                    S.rt = fw.sb("rt", [128, 4, 8 * 48], F32)
                    S.ob = [fw.sb("ob", [128, 1024], BF16) for _ in range(2)]
                    S.oT = [fw.sb("oT", [128, 1024], BF16) for _ in range(2)]
                    S.mkt = fw.sb("mkt", [128, 8, 96], F32)
                    S.cnt = {"g": 0, "o": 0, "t": 0, "p": 0}
                    return S
                sets = [mkset(k) for k in range(2)]

                def hnr(S, src_aps, src_bufs, H, Dh, gain, rope):
                    g = S.cnt["g"]
                    S.cnt["g"] += 1
                    N = H * Dh
                    x_, s_, y_, r_, sq = S.xs, S.ss, S.yb, S.rt, S.sq
                    so = (g % 2) * 16
                    o_ = S.ob[S.cnt["o"] % 2]
                    S.cnt["o"] += 1
                    v3 = lambda ap: ap.rearrange("p (h d) -> p h d", h=H)
                    for (sap, c0_, nc_) in src_aps:
                        cp("act", x_.t[:, c0_:c0_ + nc_], sap, src_bufs, [x_])
                    tt("pool", sq.t[:, :N], x_.t[:, :N], x_.t[:, :N], ALU.mult, [x_], [sq])
                    op("dve", lambda e: e.tensor_reduce(s_.t[:, so:so + H], v3(sq.t[:, :N]), AX.X, ALU.add), [sq], [s_])
                    act(s_.t[:, so + 8:so + 8 + H], s_.t[:, so:so + H], AF.Sqrt, [s_], [s_], bias=EPS, scale=1.0 / Dh)
                    op("dve", lambda e: e.reciprocal(s_.t[:, so:so + H], s_.t[:, so + 8:so + 8 + H]), [s_], [s_])
                    tt("dve", v3(y_.t[:, :N]), v3(x_.t[:, :N]), s_.t[:, so:so + H].unsqueeze(2).to_broadcast([128, H, Dh]),
                       ALU.mult, [x_, s_], [y_])
                    gb = gain.unsqueeze(1).to_broadcast([128, H, Dh])
                    if rope is None:
                        tt("pool", v3(o_.t[:, :N]), v3(y_.t[:, :N]), gb, ALU.mult, [y_, VEC], [o_])
                        return o_
                    r0, n, cos_ap, sin_ap, rbuf = rope
                    hlf = n // 2
                    tt("pool", v3(y_.t[:, :N]), v3(y_.t[:, :N]), gb, ALU.mult, [y_, VEC], [y_])
                    y3 = v3(y_.t[:, :N])
                    o3 = v3(o_.t[:, :N])
                    x1, x2 = y3[:, :, r0:r0 + hlf], y3[:, :, r0 + hlf:r0 + n]
                    cb = cos_ap.unsqueeze(1).to_broadcast([128, H, hlf])
                    sb_ = sin_ap.unsqueeze(1).to_broadcast([128, H, hlf])
                    t = [r_.t[:, i, :H * hlf].rearrange("p (h d) -> p h d", h=H) for i in range(4)]
                    tt("dve", t[0], x1, cb, ALU.mult, [y_, rbuf], [r_])
                    tt("pool", t[1], x2, sb_, ALU.mult, [y_, rbuf], [r_])
                    tt("dve", t[2], x1, sb_, ALU.mult, [y_, rbuf], [r_])
                    tt("pool", t[3], x2, cb, ALU.mult, [y_, rbuf], [r_])
                    tt("dve", o3[:, :, r0:r0 + hlf], t[0], t[1], ALU.subtract, [r_], [o_])
                    tt("pool", o3[:, :, r0 + hlf:r0 + n], t[2], t[3], ALU.add, [r_], [o_])
                    if r0 > 0:
                        cp("act", o3[:, :, 0:r0], y3[:, :, 0:r0], [y_], [o_])
                    return o_

                def transp(S, o_, nblk, bw):
                    k = S.cnt["t"]
                    S.cnt["t"] += 1
                    p_, t_ = S.ptr, S.oT[k % 2]
                    for i in range(nblk):
                        tr(p_.t[0:bw, i * 128:(i + 1) * 128], o_.t[:, i * bw:(i + 1) * bw], identb.t[:], [o_, identb], [p_])
                    cp("dve" if k % 2 else "act", t_.t[0:bw, 0:nblk * 128], p_.t[0:bw, 0:nblk * 128], [p_], [t_])
                    return t_

                def transp_store(S, o_, nblk, bw, dst_ap):
                    t_ = transp(S, o_, nblk, bw)
                    dma("sp", dst_ap, t_.t[0:bw, 0:nblk * 128].rearrange("p (i t) -> p i t", i=nblk), [t_], [])

                def plain_store(S, src_ap, src_bufs, N, dst_ap):
                    o_ = S.ob[S.cnt["o"] % 2]
                    S.cnt["o"] += 1
                    cp("act", o_.t[:, :N], src_ap, src_bufs, [o_])
                    dma("sp", dst_ap, o_.t[:, :N], [o_], [])

                def make_hT(S, xsrc, rsrc, row0, which):
                    x_, r_, s_, hT_, h1 = S.xt, S.rp, S.st, S.hT, S.yb
                    if isinstance(xsrc, list):
                        for ci_, (cap, wap) in enumerate(xsrc):
                            dma("sp", S.sq.t[:], cap, [], [S.sq])
                            if ci_ == 0:
                                ts("dve", x_.t[:], S.sq.t[:], wap, None, ALU.mult, None, [S.sq, HSEL], [x_])
                            else:
                                stt("dve", x_.t[:], S.sq.t[:], wap, x_.t[:], ALU.mult, ALU.add, [S.sq, HSEL, x_], [x_])
                    else:
                        dma("sp", x_.t[:], xsrc, [], [x_])
                    dma("sp", r_.t[:], rsrc[row0:row0 + 128, :], [], [r_])
                    act(junk.t[:], x_.t[:], AF.Square, [x_], [junk, s_], accum_out=s_.t[:, 0:1])
                    act(s_.t[:, 1:2], s_.t[:, 0:1], AF.Sqrt, [s_], [s_], bias=EPS, scale=1.0 / D)
                    op("dve", lambda e: e.reciprocal(s_.t[:, 2:3], s_.t[:, 1:2]), [s_], [s_])
                    stt("dve", h1.t[:], x_.t[:], s_.t[:, 2:3], A1[which].t[:], ALU.mult, ALU.mult, [x_, s_, A1[which]], [h1])
                    tt("pool", S.hb.t[:], h1.t[:], SH1[which].t[:], ALU.add, [h1, SH1[which]], [S.hb])
                    p_ = S.ptr
                    for j in range(8):
                        tr(p_.t[:, j * 128:(j + 1) * 128], S.hb.t[:, j * 128:(j + 1) * 128], identb.t[:], [S.hb, identb], [p_])
                    cp("dve", hT_.t[:], p_.t[:], [p_], [hT_])
                    return hT_, r_

                def nextp(S):
                    S.cnt["p"] += 1
                    return S.pp[S.cnt["p"] % 3]

                def project(S, hT_, W, c0, ncol):
                    pbuf = nextp(S)
                    for j in range(8):
                        mm(pbuf.t[:, 0:ncol], hT_.t[:, j * 128:(j + 1) * 128], W.t[:, j, c0:c0 + ncol], j == 0, j == 7, [hT_, W], [pbuf])
                    return pbuf

                def run_interleaved(tile_fn, tiles):
                    for a in range(0, len(tiles), 2):
                        streams = []
                        for k, i in enumerate(tiles[a:a + 2]):
                            fw.rec = []
                            tile_fn(i, sets[k])
                            streams.append(fw.rec)
                            fw.rec = None
                        fw.emit_interleaved(streams)

                if first:
                  with fw.scope():
                    Wd = fw.sb("Wd", [128, 8, 1184], BF16)
                    dma("pool", Wd.t[:, :, 0:1024], w_in[:, C_DK:C_DK + 1024].rearrange("(j p) c -> p j c", p=128), [], [Wd])
                    dma("pool", Wd.t[:, :, 1024:1184], w_in[:, C_MKVA:C_MKVA + 160].rearrange("(j p) c -> p j c", p=128), [], [Wd])

                    def a1_tile(i, S):
                        which = 1 if i < 2 else 0
                        hT_, r_ = make_hT(S, dense_src(i), rope_all, i * 128, which)
                        t0 = i * 128
                        cosh, sinh, cosm, sinm = r_.t[:, 0:32], r_.t[:, 32:64], r_.t[:, 64:80], r_.t[:, 80:96]
                        mkt = S.mkt
                        p_ = project(S, hT_, Wd, 0, 512)
                        o_ = hnr(S, [(p_.t[:, 0:512], 0, 512)], [p_], 8, 64, gv(V_DKN, 64), (0, 64, cosh, sinh, r_))
                        transp_store(S, o_, 4, 128, KD.rearrange("(i q) t -> q i t", q=128)[:, :, t0:t0 + 128])
                        p_ = project(S, hT_, Wd, 512, 512)
                        plain_store(S, p_.t[:, 0:512], [p_], 512, VD[t0:t0 + 128, :])
                        p_ = project(S, hT_, Wd, 1024, 160)
                        cp("act", mkt.t[:, :, 64:96], p_.t[:, 128:160].unsqueeze(1).to_broadcast([128, 8, 32]), [p_], [mkt])
                        o_ = hnr(S, [(p_.t[:, 0:128], 0, 128)], [p_], 1, 128, gv(V_MKVAN, 128), None)
                        t_ = transp(S, o_, 1, 128)
                        o2 = S.ob[S.cnt["o"] % 2]
                        S.cnt["o"] += 1
                        for hh in range(2):
                            p2 = nextp(S)
                            mm(p2.t[:, 0:512], t_.t[:, 0:128], wkvb.t[:, hh * 512:(hh + 1) * 512], True, True, [t_, wkvb], [p2])
                            kv3 = p2.t[:, 0:512].rearrange("p (h d) -> p h d", h=4)
                            cp("dve", mkt.t[:, hh * 4:hh * 4 + 4, 0:64], kv3[:, :, 0:64], [p2], [mkt])
                            cp("dve", o2.t[:, hh * 256:hh * 256 + 256].rearrange("p (h d) -> p h d", h=4), kv3[:, :, 64:128], [p2], [o2])
                        dma("sp", VM[t0:t0 + 128, :], o2.t[:, 0:512], [o2], [])
                        o_ = hnr(S, [(mkt.t[:].rearrange("p h d -> p (h d)"), 0, 768)], [mkt], 8, 96, gv(V_MKN, 96), (64, 32, cosm, sinm, r_))
                        transp_store(S, o_, 8, 96, KM.rearrange("h d t -> d h t")[:, :, t0:t0 + 128])
                    run_interleaved(a1_tile, list(range(CFG['nA1'])))

                with fw.scope():
                    Wl = fw.sb("Wl", [128, 8, 3072], BF16)
                    for (dst0, c0, ncol) in ((0, 0, 1280), (1280, C_NQ, 1792)):
                        for s0 in range(0, ncol, 640):
                            sn = min(640, ncol - s0)
                            dma("pool", Wl.t[:, :, dst0 + s0:dst0 + s0 + sn],
                                w_in[:, c0 + s0:c0 + s0 + sn].rearrange("(j p) c -> p j c", p=128), [], [Wl])
                    L_WQ, L_WKV, L_DQ, L_NQ, L_NK, L_NV, L_MQA = 0, 512, 768, 1280, 1792, 2304, 2816

                    def a2_tile(i, S):
                        is_ctx = i >= 22
                        is_own = 3 <= i < 19
                        wantq = is_own or (is_ctx and want_ctx)
                        qs = (i - 3) if is_own else (16 + i - 22)
                        hT_, r_ = make_hT(S, local_src(i), rope_loc, i * 128, 1 if is_ctx else 0)
                        t0 = i * 128
                        q0 = qs * 128
                        cosh, sinh, cosm, sinm = r_.t[:, 0:32], r_.t[:, 32:64], r_.t[:, 64:80], r_.t[:, 80:96]
                        p_ = project(S, hT_, Wl, L_WKV, 256)
                        o_ = hnr(S, [(p_.t[:, 0:128], 0, 128)], [p_], 2, 64, gv(V_WKN, 64), (0, 64, cosh, sinh, r_))
                        transp_store(S, o_, 1, 128, KW[:, t0:t0 + 128].unsqueeze(1))
                        plain_store(S, p_.t[:, 128:256], [p_], 128, VW[t0:t0 + 128, :])
                        p_ = project(S, hT_, Wl, L_NK, 512)
                        o_ = hnr(S, [(p_.t[:, 0:512], 0, 512)], [p_], 8, 64, gv(V_NKN, 64), None)
                        transp_store(S, o_, 4, 128, KN.rearrange("(i q) t -> q i t", q=128)[:, :, t0:t0 + 128])
                        p_ = project(S, hT_, Wl, L_NV, 512)
                        plain_store(S, p_.t[:, 0:512], [p_], 512, VN[t0:t0 + 128, :])
                        if not wantq:
                            return
                        dma("sp", HTS[qs], hT_.t[:], [hT_], [])
                        for (lc, gain, dst, rope) in ((L_WQ, V_WQN, QW, True), (L_DQ, V_DQN, QD, True), (L_NQ, V_NQN, QN, False)):
                            p_ = project(S, hT_, Wl, lc, 512)
                            o_ = hnr(S, [(p_.t[:, 0:512], 0, 512)], [p_], 8, 64, gv(gain, 64), (0, 64, cosh, sinh, r_) if rope else None)
                            transp_store(S, o_, 4, 128, dst.rearrange("(i q) t -> q i t", q=128)[:, :, q0:q0 + 128])
                        p_ = project(S, hT_, Wl, L_MQA, 256)
                        o_ = hnr(S, [(p_.t[:, 0:256], 0, 256)], [p_], 1, 256, gv(V_MQAN, 256), None)
                        t_ = transp(S, o_, 2, 128)
                        pa_, pb_ = nextp(S), nextp(S)
                        for (pq, n0, nn) in ((pa_, 0, 512), (pb_, 512, 256)):
                            for j in range(2):
                                mm(pq.t[:, 0:nn], t_.t[:, j * 128:(j + 1) * 128], wqb.t[:, j, n0:n0 + nn], j == 0, j == 1, [t_, wqb], [pq])
                        o_ = hnr(S, [(pa_.t[:, 0:512], 0, 512), (pb_.t[:, 0:256], 512, 256)], [pa_, pb_], 8, 96, gv(V_MQN, 96), (64, 32, cosm, sinm, r_))
                        transp_store(S, o_, 8, 96, QM.rearrange("h d t -> d h t")[:, :, q0:q0 + 128])
                    run_interleaved(a2_tile, list(range(NT_LOC)) if CFG['nA2'] == NT_LOC else [0, 3, 22][:CFG['nA2']])

                with fw.scope():
                    Wg = [fw.sb("Wg%d" % i, [128, 8, 1024], BF16) for i in range(2)]
                    gt = [fw.sb("gt%d" % i, [128, 1024], BF16) for i in range(2)]
                    gp = [sets[0].pp[0], sets[0].pp[1], sets[1].pp[0], sets[1].pp[1]]
                    nq_tiles = NQT if want_ctx else 16
                    if CFG['nA2'] != NT_LOC:
                        nq_tiles = 0
                    HT = fw.sb("HT", [128, NQT, D], BF16)
                    for qs in range(nq_tiles):
                        dma("sp", HT.t[:, qs, :], HTS[qs], [], [HT])
                    n = 0
                    for gq in range(4):
                        W = Wg[gq % 2]
                        for s0 in (0, 512):
                            dma("pool", W.t[:, :, s0:s0 + 512],
                                w_in[:, C_G + gq * 1024 + s0:C_G + gq * 1024 + s0 + 512].rearrange("(j p) c -> p j c", p=128), [], [W])
                        for qs in range(nq_tiles):
                            g_ = gt[n % 2]
                            for hf in range(2):
                                p_ = gp[(2 * n + hf) % 4]
                                for j in range(8):
                                    mm(p_.t[:], HT.t[:, qs, j * 128:(j + 1) * 128], W.t[:, j, hf * 512:(hf + 1) * 512], j == 0, j == 7, [HT, W], [p_])
                                cp("act" if hf else "dve", g_.t[:, hf * 512:(hf + 1) * 512], p_.t[:], [p_], [g_])
                            n += 1
                            dma("sp", GATES[qs * 128:(qs + 1) * 128, gq * 1024:(gq + 1) * 1024], g_.t[:], [g_], [])

            if CFG['phases'] == 'all' or 'B' in CFG['phases']:
              with fw.scope():
                ES = fw.sb("ES", [128, 8], F32)
                act(ES.t[:], gv(V_SINK, 8), AF.Exp, [VEC], [ES])
                LM = fw.sb("LM", [128, 8], F32)
                lt = fw.sb("lt", [128, 128], F32)
                tt("dve", lt.t[:, 0:64], gv(V_LAM, 64), gv(V_LAM + 64, 64), ALU.mult, [VEC], [lt])
                tt("dve", lt.t[:, 64:128], gv(V_LAM + 128, 64), gv(V_LAM + 192, 64), ALU.mult, [VEC, lt], [lt])
                op("dve", lambda e: e.tensor_reduce(LM.t[:, 0:2], lt.t[:].rearrange("p (a d) -> p a d", a=2), AX.X, ALU.add), [lt], [LM])
                act(LM.t[:, 2:4], LM.t[:, 0:2], AF.Exp, [LM], [LM])
                tt("dve", LM.t[:, 4:5], LM.t[:, 2:3], LM.t[:, 3:4], ALU.subtract, [LM], [LM])
                ts("dve", LM.t[:, 5:6], LM.t[:, 4:5], -1.0, -lam_init, ALU.mult, ALU.add, [LM], [LM])
                NEGLAM = LM.t[:, 5:6]
                SG = fw.sb("SG", [128, 128], F32)
                ts("dve", SG.t[:], gv(V_SUBLN, 128), 1.0 - lam_init, None, ALU.mult, None, [VEC], [SG])

                acc = [fw.ps("acc%d" % i, [128, 512], F32) for i in range(4)]
                sps = [fw.ps("sps%d" % i, [128, 512], F32) for i in range(4)]
                pt = [fw.sb("pt%d" % i, [128, 512], BF16) for i in range(4)]
                fz = [fw.sb("fz%d" % i, [128, 8], F32) for i in range(4)]
                cs_ = [0]
                nqt = NQT if want_ctx else 16

                def attn(chunks, nacc, dv, scale, qn=512):
                    n = len(chunks)
                    LOOK = 3
                    live = {}
                    for it in range(n + LOOK):
                        if it < n:
                            ch = chunks[it]
                            k = cs_[0]
                            cs_[0] += 1
                            s_, p_ = sps[k % 4], pt[k % 4]
                            for (l_ap, r_ap, c0, ncl) in ch['mms']:
                                mm(s_.t[:, c0:c0 + ncl], l_ap, r_ap, True, True, ch['rb'], [s_])
                            act(p_.t[:, :qn], s_.t[:, :qn], AF.Exp, [s_], [p_], scale=scale)
                            for mi, (m_ap, m_buf) in enumerate(ch.get('masks', ())):
                                p3 = p_.t[:, :].rearrange("p (h q) -> p h q", h=4)
                                tt("pool" if mi % 2 == 0 else "dve", p3, p3, m_ap, ALU.mult, [p_, m_buf], [p_])
                            live[it] = p_
                        ci = it - LOOK
                        if ci >= 0:
                            ch = chunks[ci]
                            p_ = live.pop(ci)
                            for j in range(nacc):
                                mm(acc[j].t[:, 0:dv + 1], p_.t[:, j * 128:(j + 1) * 128], ch['v'][j], ci == 0, ci == n - 1,
                                   [p_] + ch['vb'], [acc[j]])

                def fin_simple(j, dst_ap, dst_buf, dv, sink_ap=None):
                    z_ = fz[j]
                    if sink_ap is not None:
                        tt("dve", z_.t[:, 0:1], acc[j].t[:, dv:dv + 1], sink_ap, ALU.add, [acc[j], ES], [z_])
                        op("dve", lambda e: e.reciprocal(z_.t[:, 1:2], z_.t[:, 0:1]), [z_], [z_])
                    else:
                        op("dve", lambda e: e.reciprocal(z_.t[:, 1:2], acc[j].t[:, dv:dv + 1]), [acc[j]], [z_])
                    ts("dve", dst_ap, acc[j].t[:, 0:dv], z_.t[:, 1:2], None, ALU.mult, None, [acc[j], z_], [dst_buf])

                Ysb = [fw.sb("Ysb%d" % i, [128, NQT, 512], BF16) for i in range(2)]

                def store_branch(n, ybuf):
                    dma("sp", Y[0:nqt * 128, n * 512:(n + 1) * 512].rearrange("(t p) c -> p t c", p=128), ybuf.t[:, 0:nqt, :], [ybuf], [])

                if 'nodense' not in CFG['phases']:
                  with fw.scope():
                    kb = [fw.sb("kb%d" % i, [96, NT_ALL * 128], BF16) for i in range(2)]
                    vb = [fw.sb("vb%d" % i, [128, NT_ALL, 129], BF16) for i in range(2)]
                    qb_ = [fw.sb("qb%d" % i, [96, NQT * 128], BF16) for i in range(2)]
                    D1 = fw.sb("D1", [128, NQT, 128], F32)
                    dtm = [fw.sb("dtm%d" % i, [128, 128], F32) for i in range(2)]
                    dsq = fw.sb("dsq", [128, 128], F32)
                    hcount = [0]

                    def dense_head(KTsrc, d, QTsrc, V1, dv, scale, fin):
                        hi = hcount[0]
                        hcount[0] += 1
                        KT, QT = kb[hi % 2], qb_[hi % 2]
                        dma("sp", KT.t[0:d, :], KTsrc, [], [KT])
                        dma("sp", QT.t[0:d, :], QTsrc, [], [QT])
                        for qblk in range(4):
                            chunks = [dict(mms=[(KT.t[0:d, c * 128:(c + 1) * 128], QT.t[0:d, qblk * 512:(qblk + 1) * 512], 0, 512)],
                                           rb=[KT, QT], v=[V1.t[:, c, 0:dv + 1]] * 4, vb=[V1]) for c in range(CFG.get('nkc', NT_ALL))]
                            attn(chunks, 4, dv, scale)
                            for j in range(4):
                                fin(j, qblk * 4 + j)
                        if want_ctx:
                            chunks = [dict(mms=[(KT.t[0:d, c * 128:(c + 1) * 128], QT.t[0:d, 2048:2304], 0, 256)],
                                           rb=[KT, QT], v=[V1.t[:, c, 0:dv + 1]] * 2, vb=[V1]) for c in range(2)]
                            attn(chunks, 2, dv, scale, qn=256)
                            for j in range(2):
                                fin(j, 16 + j)

                    Yd = Ysb[0]
                    for hd in range(4):
                        V1 = vb[hd % 2]
                        dma("sp", V1.t[:, :, 0:128], VD[:, hd * 128:(hd + 1) * 128].rearrange("(c p) d -> p c d", p=128), [], [V1])
                        op("pool", lambda e: e.memset(V1.t[:, :, 128:129], 1.0), [], [V1])
                        for i2 in range(2):
                            hs = 2 * hd + i2

                            def fin(j, qt, i2=i2, hd=hd):
                                if i2 == 0:
                                    fin_simple(j, D1.t[:, qt, :], D1, 128)
                                    return
                                t_ = dtm[j % 2]
                                z_ = fz[j]
                                fin_simple(j, t_.t[:], t_, 128)
                                stt("dve", t_.t[:], t_.t[:], NEGLAM, D1.t[:, qt, :], ALU.mult, ALU.add, [t_, LM, D1], [t_])
                                tt("pool", dsq.t[:], t_.t[:], t_.t[:], ALU.mult, [t_], [dsq])
                                op("dve", lambda e: e.tensor_reduce(z_.t[:, 2:3], dsq.t[:], AX.X, ALU.add), [dsq], [z_])
                                act(z_.t[:, 3:4], z_.t[:, 2:3], AF.Sqrt, [z_], [z_], bias=EPS, scale=1.0 / 128)
                                op("dve", lambda e: e.reciprocal(z_.t[:, 4:5], z_.t[:, 3:4]), [z_], [z_])
                                stt("dve", Yd.t[:, qt, hd * 128:(hd + 1) * 128], t_.t[:], z_.t[:, 4:5], SG.t[:], ALU.mult, ALU.mult,
                                    [t_, z_, SG], [Yd])
                            dense_head(KD[hs * 64:(hs + 1) * 64, :], 64, QD[hs * 64:(hs + 1) * 64, :], V1, 128, 0.125, fin)
                    store_branch(1, Yd)

                    Ym = Ysb[1]
                    for h in range(8):
                        V1 = vb[h % 2]
                        dma("sp", V1.t[:, :, 0:64], VM[:, h * 64:(h + 1) * 64].rearrange("(c p) d -> p c d", p=128), [], [V1])
                        op("pool", lambda e: e.memset(V1.t[:, :, 64:65], 1.0), [], [V1])

                        def fin(j, qt, h=h):
                            fin_simple(j, Ym.t[:, qt, h * 64:(h + 1) * 64], Ym, 64)
                        dense_head(KM[h], 96, QM[h], V1, 64, 96.0 ** -0.5, fin)
                    store_branch(3, Ym)

                with fw.scope():
                    Yw = Ysb[0]
                    WM = fw.sb("WM", [128, 4, 128], BF16)
                    dma("pool", WM.t[:], wmask.rearrange("m k q -> k m q"), [], [WM])
                    kw_ = [fw.sb("kw%d" % i, [64, NT_LOC * 128], BF16) for i in range(2)]
                    vw_ = [fw.sb("vw%d" % i, [128, NT_LOC, 65], BF16) for i in range(2)]
                    qw_ = [fw.sb("qw%d" % i, [64, NQT, 4, 128], BF16) for i in range(2)]
                    for g in range(2):
                        KT, V1, QT = kw_[g], vw_[g], qw_[g]
                        dma("sp", KT.t[:], KW[g * 64:(g + 1) * 64, :], [], [KT])
                        dma("sp", V1.t[:, :, 0:64], VW[:, g * 64:(g + 1) * 64].rearrange("(c p) d -> p c d", p=128), [], [V1])
                        op("pool", lambda e: e.memset(V1.t[:, :, 64:65], 1.0), [], [V1])
                        for hq in range(4):
                            dma("sp", QT.t[:, :, hq, :], QW[(g * 4 + hq) * 64:(g * 4 + hq + 1) * 64, :].rearrange("d (t q) -> d t q", q=128), [], [QT])
                        for qt in range(nqt):
                            rhs = QT.t[0:64, qt, :, :].rearrange("p h q -> p (h q)")
                            if qt < 16:
                                sl = [(qt + 2, WM.t[:, 0 if qt == 0 else 1, :]), (qt + 3, None), (qt + 4, WM.t[:, 3 if qt == 15 else 2, :]), (22, None), (23, None)]
                            else:
                                sl = [(22, None), (23, None)]
                            chunks = []
                            for (slot, m) in sl:
                                ch = dict(mms=[(KT.t[0:64, slot * 128:(slot + 1) * 128], rhs, 0, 512)], rb=[KT, QT],
                                          v=[V1.t[:, slot, 0:65]] * 4, vb=[V1])
                                if m is not None:
                                    ch['masks'] = [(m.unsqueeze(1).to_broadcast([128, 4, 128]), WM)]
                                chunks.append(ch)
                            attn(chunks, 4, 64, 0.125)
                            for j in range(4):
                                h = g * 4 + j
                                fin_simple(j, Yw.t[:, qt, h * 64:(h + 1) * 64], Yw, 64, sink_ap=ES.t[:, h:h + 1])
                    store_branch(0, Yw)

                with fw.scope():
                    Yn = Ysb[1]
                    NVm = fw.sb("NVm", [128, 5, 7, 128], BF16)
                    dma("pool", NVm.t[:], natv.rearrange("a c k q -> k a c q"), [], [NVm])
                    ebf = fw.sb("ebf", [128, 7, 4, 128], F32)
                    EB = [fw.sb("EB%d" % i, [128, 7, 4, 128], BF16) for i in range(2)]
                    kn_ = [fw.sb("kn%d" % i, [64, 4, NT_LOC * 128], BF16) for i in range(2)]
                    vn_ = [fw.sb("vn%d" % i, [128, NT_LOC, 4, 65], BF16) for i in range(2)]
                    qn_ = [fw.sb("qn%d" % i, [64, NQT, 4, 128], BF16) for i in range(2)]
                    for hh in range(2):
                        KT, V1, QT, EBh = kn_[hh], vn_[hh], qn_[hh], EB[hh]
                        for c in range(7):
                            dma("sp", ebf.t[:, c, :, :], natb[c, :, hh * 4:(hh + 1) * 4, :], [], [ebf])
                        act(EBh.t[:].rearrange("p c h q -> p (c h q)"), ebf.t[:].rearrange("p c h q -> p (c h q)"), AF.Exp, [ebf], [EBh])
                        for j in range(4):
                            h = hh * 4 + j
                            dma("sp", KT.t[:, j, :], KN[h * 64:(h + 1) * 64, :], [], [KT])
                            dma("sp", V1.t[:, :, j, 0:64], VN[:, h * 64:(h + 1) * 64].rearrange("(c p) d -> p c d", p=128), [], [V1])
                            dma("sp", QT.t[:, :, j, :], QN[h * 64:(h + 1) * 64, :].rearrange("d (t q) -> d t q", q=128), [], [QT])
                        op("pool", lambda e: e.memset(V1.t[:, :, :, 64:65], 1.0), [], [V1])
                        for qt in range(nqt):
                            chunks = []
                            if qt < 16:
                                cls = {0: 0, 1: 1, 14: 3, 15: 4}.get(qt, 2)
                                lst = [(qt + c + 3, c + 3) for c in range(-3, 4)] + [(22, None), (23, None)]
                            else:
                                lst = [(22, None), (23, None)]
                            for (slot, ci) in lst:
                                ch = dict(mms=[(KT.t[0:64, j, slot * 128:(slot + 1) * 128], QT.t[0:64, qt, j, :], j * 128, 128) for j in range(4)],
                                          rb=[KT, QT], v=[V1.t[:, slot, j, 0:65] for j in range(4)], vb=[V1])
                                if ci is not None:
                                    ch['masks'] = [(EBh.t[:, ci, :, :], EBh),
                                                   (NVm.t[:, cls, ci, :].unsqueeze(1).to_broadcast([128, 4, 128]), NVm)]
                                chunks.append(ch)
                            attn(chunks, 4, 64, 0.125)
                            for j in range(4):
                                h = hh * 4 + j
                                fin_simple(j, Yn.t[:, qt, h * 64:(h + 1) * 64], Yn, 64)
                    store_branch(2, Yn)
            if CFG['phases'] == 'all' or 'C' in CFG['phases']:
              with fw.scope():
                nqt = NQT if want_ctx else 16
                H2T = fw.sb("H2T", [128, 8, NQT * 128], BF16)
                WT = fw.sb("WT", [128, NQT, 16], F32)
                xslot = lambda qt: (3 + qt) if qt < 16 else (22 + qt - 16)
                with fw.scope():
                    G1 = [fw.sb("G1_%d" % w, [128, D], F32) for w in range(2)]
                    A2 = [fw.sb("A2_%d" % w, [128, D], F32) for w in range(2)]
                    SH2 = [fw.sb("SH2_%d" % w, [128, D], F32) for w in range(2)]
                    for w in range(2):
                        load_mod(G1[w], w, M_G1)
                        load_mod(A2[w], w, M_A2)
                        load_mod(SH2[w], w, M_SH2)
                    wbr = fw.sb("wbr", [128, 16, D], BF16)
                    wo = fw.sb("wo", [128, 8, D], BF16)
                    rw = fw.sb("rw", [128, 8, 16], F32)
                    for q4 in range(4):
                        dma("pool", wbr.t[:, q4 * 4:(q4 + 1) * 4, :], w_branch[q4 * 512:(q4 + 1) * 512, :].rearrange("(j p) c -> p j c", p=128), [], [wbr])
                    for q2 in range(2):
                        dma("pool", wo.t[:, q2 * 4:(q2 + 1) * 4, :], w_out[q2 * 512:(q2 + 1) * 512, :].rearrange("(j p) c -> p j c", p=128), [], [wo])
                    dma("sp", rw.t[:], router_w.rearrange("(j p) e -> p j e", p=128), [], [rw])
                    yt = [fw.sb("yt%d" % i, [128, 2048], BF16) for i in range(2)]
                    gt_ = [fw.sb("gtc%d" % i, [128, 4096], BF16) for i in range(2)]
                    sg = fw.sb("sg", [128, 4096], BF16)
                    YT = fw.sb("YT", [128, 16, 128], BF16)
                    macc = fw.sb("macc", [128, D], F32)
                    mtmp = fw.sb("mtmp", [128, 512], F32)
                    mbf = fw.sb("mbf", [128, D], BF16)
                    mT = fw.sb("mT", [128, 8, 128], BF16)
                    xin = [fw.sb("xin%d" % i, [128, D], F32) for i in range(2)]
                    xn = [fw.sb("xn%d" % i, [128, D], F32) for i in range(2)]
                    junk2 = fw.sb("junk2", [128, D], F32)
                    h2 = fw.sb("h2", [128, D], F32)
                    h2tf = fw.sb("h2tf", [128, 8, 128], F32)
                    s2 = [fw.sb("s2_%d" % i, [128, 8], F32) for i in range(2)]
                    rr = [fw.sb("rr%d" % i, [128, 160], F32) for i in range(2)]
                    ptc = [fw.ps("ptc%d" % i, [128, D], BF16) for i in range(2)]
                    pm = [fw.ps("pm%d" % i, [128, 512], F32) for i in range(3)]
                    pf = [fw.ps("pf%d" % i, [128, 512], F32) for i in range(2)]
                    pr = fw.ps("pr", [128, 16], F32)
                    pmc = [0]
                    for qt in range(nqt):
                        which = 1 if qt >= 16 else 0
                        y_, g_, x_, xn_, s_, r_ = yt[qt % 2], gt_[qt % 2], xin[qt % 2], xn[qt % 2], s2[qt % 2], rr[qt % 2]
                        dma("sp", y_.t[:], Y[qt * 128:(qt + 1) * 128, :], [], [y_])
                        dma("sp", g_.t[:], GATES[qt * 128:(qt + 1) * 128, :], [], [g_])
                        dma("sp", x_.t[:], local_src(xslot(qt)), [], [x_])
                        act(sg.t[:], g_.t[:], AF.Sigmoid, [g_], [sg])
                        for hf in range(2):
                            p_ = ptc[hf]
                            for i in range(8):
                                tr(p_.t[:, i * 128:(i + 1) * 128], y_.t[:, (hf * 8 + i) * 128:(hf * 8 + i + 1) * 128], identb.t[:], [y_, identb], [p_])
                            cp("dve" if hf else "act", YT.t[:, hf * 8:(hf + 1) * 8, :].rearrange("p a b -> p (a b)"), p_.t[:], [p_], [YT])
                        for n in range(4):
                            for half in range(2):
                                p_ = pm[pmc[0] % 3]
                                pmc[0] += 1
                                for k in range(4):
                                    mm(p_.t[:], YT.t[:, n * 4 + k, :], wbr.t[:, n * 4 + k, half * 512:(half + 1) * 512], k == 0, k == 3, [YT, wbr], [p_])
                                sgs = sg.t[:, n * 1024 + half * 512:n * 1024 + (half + 1) * 512]
                                if n == 0:
                                    tt("dve", macc.t[:, half * 512:(half + 1) * 512], p_.t[:], sgs, ALU.mult, [p_, sg], [macc])
                                else:
                                    tt("dve", mtmp.t[:], p_.t[:], sgs, ALU.mult, [p_, sg], [mtmp])
                                    tt("pool", macc.t[:, half * 512:(half + 1) * 512], macc.t[:, half * 512:(half + 1) * 512], mtmp.t[:], ALU.add, [macc, mtmp], [macc])
                        cp("act", mbf.t[:], macc.t[:], [macc], [mbf])
                        p_ = ptc[0]
                        for i in range(8):
                            tr(p_.t[:, i * 128:(i + 1) * 128], mbf.t[:, i * 128:(i + 1) * 128], identb.t[:], [mbf, identb], [p_])
                        cp("dve", mT.t[:].rearrange("p a b -> p (a b)"), p_.t[:], [p_], [mT])
                        for half in range(2):
                            p_ = pm[pmc[0] % 3]
                            pmc[0] += 1
                            for j in range(8):
                                mm(p_.t[:], mT.t[:, j, :], wo.t[:, j, half * 512:(half + 1) * 512], j == 0, j == 7, [mT, wo], [p_])
                            hs_ = slice(half * 512, (half + 1) * 512)
                            tt("dve", mtmp.t[:], p_.t[:], G1[which].t[:, hs_], ALU.mult, [p_, G1[which]], [mtmp])
                            tt("pool", xn_.t[:, hs_], mtmp.t[:], x_.t[:, hs_], ALU.add, [mtmp, x_], [xn_])
                        dma("sp", XN[qt * 128:(qt + 1) * 128, :], xn_.t[:], [xn_], [])
                        act(junk2.t[:], xn_.t[:], AF.Square, [xn_], [junk2, s_], accum_out=s_.t[:, 0:1])
                        act(s_.t[:, 1:2], s_.t[:, 0:1], AF.Sqrt, [s_], [s_], bias=EPS, scale=1.0 / D)
                        op("dve", lambda e: e.reciprocal(s_.t[:, 2:3], s_.t[:, 1:2]), [s_], [s_])
                        stt("dve", h2.t[:], xn_.t[:], s_.t[:, 2:3], A2[which].t[:], ALU.mult, ALU.mult, [xn_, s_, A2[which]], [h2])
                        tt("pool", h2.t[:], h2.t[:], SH2[which].t[:], ALU.add, [h2, SH2[which]], [h2])
                        for hf in range(2):
                            p_ = pf[hf]
                            for i in range(4):
                                j = hf * 4 + i
                                tr(p_.t[:, i * 128:(i + 1) * 128], h2.t[:, j * 128:(j + 1) * 128], identf.t[:], [h2, identf], [p_])
                            cp("dve" if hf else "act", h2tf.t[:, hf * 4:(hf + 1) * 4, :].rearrange("p a b -> p (a b)"), p_.t[:], [p_], [h2tf])
                        cp("pool", H2T.t[:, :, qt * 128:(qt + 1) * 128], h2tf.t[:], [h2tf], [H2T])
                        for j in range(8):
                            mm(pr.t[:], h2tf.t[:, j, :], rw.t[:, j, :], j == 0, j == 7, [h2tf, rw], [pr])
                        R = lambda a, b: r_.t[:, a:b]
                        v4 = lambda ap: ap.rearrange("p (g e) -> p g e", g=4)
                        act(R(0, 16), pr.t[:], AF.Sigmoid, [pr], [r_])
                        tt("dve", R(16, 32), R(0, 16), gv(V_RB, 16), ALU.add, [r_, VEC], [r_])
                        op("dve", lambda e: e.tensor_reduce(R(32, 36), v4(R(16, 32)), AX.X, ALU.max), [r_], [r_])
                        tt("dve", v4(R(48, 64)), v4(R(16, 32)), R(32, 36).unsqueeze(2).to_broadcast([128, 4, 4]), ALU.is_equal, [r_], [r_])
                        stt("dve", R(64, 80), R(48, 64), -1e9, R(16, 32), ALU.mult, ALU.add, [r_], [r_])
                        op("dve", lambda e: e.tensor_reduce(R(36, 40), v4(R(64, 80)), AX.X, ALU.max), [r_], [r_])
                        tt("dve", R(40, 44), R(32, 36), R(36, 40), ALU.add, [r_], [r_])
                        op("dve", lambda e: e.tensor_reduce(R(44, 45), R(40, 44), AX.X, ALU.max), [r_], [r_])
                        ts("dve", R(80, 84), R(40, 44), R(44, 45), None, ALU.is_equal, None, [r_], [r_])
                        tt("dve", v4(R(96, 112)), v4(R(16, 32)), R(36, 40).unsqueeze(2).to_broadcast([128, 4, 4]), ALU.is_ge, [r_], [r_])
                        tt("dve", v4(R(96, 112)), v4(R(96, 112)), R(80, 84).unsqueeze(2).to_broadcast([128, 4, 4]), ALU.mult, [r_], [r_])
                        tt("dve", R(112, 128), R(96, 112), R(0, 16), ALU.mult, [r_], [r_])
                        op("dve", lambda e: e.tensor_reduce(R(128, 129), R(112, 128), AX.X, ALU.add), [r_], [r_])
                        op("dve", lambda e: e.reciprocal(R(129, 130), R(128, 129)), [r_], [r_])
                        ts("dve", WT.t[:, qt, :], R(112, 128), R(129, 130), None, ALU.mult, None, [r_], [WT])
                    if False:
                        dbg_wt = dscr("dbg_wt", [128, NQT * 16], F32)
                        dma("sp", dbg_wt, WT.t[:].rearrange("p a b -> p (a b)"), [WT], [])
                with fw.scope():
                    Ft = [fw.sb("F%d" % i, [128, D], F32) for i in range(nqt)]
                    for f_ in Ft:
                        op("pool", lambda e: e.memset(f_.t[:], 0.0), [], [f_])
                    w1 = [fw.sb("w1_%d" % i, [128, 8, 512], BF16) for i in range(2)]
                    w3 = [fw.sb("w3_%d" % i, [128, 8, 512], BF16) for i in range(2)]
                    w2 = [fw.sb("w2_%d" % i, [128, 4, D], BF16) for i in range(2)]
                    GT = [fw.sb("GT%d" % i, [128, 4, 512], BF16) for i in range(2)]
                    sl = [fw.sb("sl%d" % i, [128, 512], F32) for i in range(2)]
                    pa = [fw.ps("pa%d" % i, [128, 512], F32) for i in range(2)]
                    pb = [fw.ps("pb%d" % i, [128, 512], F32) for i in range(2)]
                    py = [fw.ps("py%d" % i, [128, 512], F32) for i in range(3)]
                    blocks = [(i * 512, 512) for i in range(4)] + ([(2048, 256)] if want_ctx else [])
                    c1, c2 = [0], [0]
                    for e_ in range(CFG.get('nexp', 16)):
                        a1, a3, a2 = w1[e_ % 2], w3[e_ % 2], w2[e_ % 2]
                        dma("pool", a1.t[:], moe_w1[e_].rearrange("(j p) c -> p j c", p=128), [], [a1])
                        dma("pool", a3.t[:], moe_w3[e_].rearrange("(j p) c -> p j c", p=128), [], [a3])
                        dma("pool", a2.t[:], moe_w2[e_].rearrange("(j p) c -> p j c", p=128), [], [a2])
                        for bi_, (t0, nb) in enumerate(blocks):
                            G_ = GT[(e_ * 5 + bi_) % 2]
                            for m in range(4):
                                pa_, pb_, sl_ = pa[c1[0] % 2], pb[c1[0] % 2], sl[c1[0] % 2]
                                c1[0] += 1
                                for k in range(8):
                                    mm(pa_.t[:, :nb], a1.t[:, k, m * 128:(m + 1) * 128], H2T.t[:, k, t0:t0 + nb], k == 0, k == 7, [a1, H2T], [pa_])
                                for k in range(8):
                                    mm(pb_.t[:, :nb], a3.t[:, k, m * 128:(m + 1) * 128], H2T.t[:, k, t0:t0 + nb], k == 0, k == 7, [a3, H2T], [pb_])
                                act(sl_.t[:, :nb], pa_.t[:, :nb], AF.Silu, [pa_], [sl_])
                                tt("dve", G_.t[:, m, :nb], sl_.t[:, :nb], pb_.t[:, :nb], ALU.mult, [sl_, pb_], [G_])
                            for j in range(nb // 128):
                                qt = t0 // 128 + j
                                for half in range(2):
                                    py_ = py[c2[0] % 3]
                                    c2[0] += 1
                                    for m in range(4):
                                        mm(py_.t[:], G_.t[:, m, j * 128:(j + 1) * 128], a2.t[:, m, half * 512:(half + 1) * 512], m == 0, m == 3, [G_, a2], [py_])
                                    fs = Ft[qt].t[:, half * 512:(half + 1) * 512]
                                    stt("dve", fs, py_.t[:], WT.t[:, qt, e_:e_ + 1], fs, ALU.mult, ALU.add, [py_, WT, Ft[qt]], [Ft[qt]])
                    G2 = [fw.sb("G2_%d" % w, [128, D], F32) for w in range(2)]
                    for w in range(2):
                        load_mod(G2[w], w, M_G2)
                    xo = [fw.sb("xo%d" % i, [128, D], F32) for i in range(2)]
                    for qt in range(nqt):
                        which = 1 if qt >= 16 else 0
                        x_ = xo[qt % 2]
                        dma("sp", x_.t[:], XN[qt * 128:(qt + 1) * 128, :], [], [x_])
                        tt("dve", Ft[qt].t[:], Ft[qt].t[:], G2[which].t[:], ALU.mult, [Ft[qt], G2[which]], [Ft[qt]])
                        tt("pool", x_.t[:], x_.t[:], Ft[qt].t[:], ALU.add, [x_, Ft[qt]], [x_])
                        if lidx == 1:
                            dma("sp", x_out[qt * 128:(qt + 1) * 128, :], x_.t[:], [x_], [], out=True)
                        elif qt < 16:
                            dma("sp", X1[(seg * 16 + qt) * 128:(seg * 16 + qt + 1) * 128, :], x_.t[:], [x_], [])
                        else:
                            dma("sp", XC1[(qt - 16) * 128:(qt - 15) * 128, :], x_.t[:], [x_], [])

        for lidx in layers:
            for k_, seg in enumerate(segs0 if lidx == 0 else (0,)):
                layer_pass(lidx, seg, lidx == 0 and seg == 0, k_ == 0)
        fw.finish()
    return nc


def _rope_tables():
    t = np.arange(SEQ)
    rows = (t // 64).astype(np.float32)
    cols = (t % 64).astype(np.float32)
    out = []
    for dim in (64, 32):
        quarter = dim // 4
        inv = np.exp(-np.log(10000.0) * np.arange(quarter, dtype=np.float32) / quarter).astype(np.float32)
        ang = np.concatenate([rows[:, None] * inv, cols[:, None] * inv], axis=-1).astype(np.float32)
        out += [np.cos(ang).astype(np.float32), np.sin(ang).astype(np.float32)]
    tab = np.concatenate(out, axis=1)
    ident = np.zeros((1, 96), np.float32)
    ident[0, 0:32] = 1.0
    ident[0, 64:80] = 1.0
    return tab, ident


def _nat_consts(s):
    col = np.arange(64)
    cs = np.clip(col - 8, 0, 48)
    col_ok = (col[None, :] >= cs[:, None]) & (col[None, :] < cs[:, None] + 16)
    reps = (0, 1, 7, 14, 15)
    V = np.zeros((5, 7, 128, 128), np.float32)
    for ci, tl in enumerate(reps):
        t = 16 * s + tl
        for c in range(-3, 4):
            u = t + c
            if not (0 <= u < 64):
                continue
            for kr in range(2):
                for qr in range(2):
                    krow, qrow = 2 * u + kr, 2 * t + qr
                    st = min(max(qrow - 4, 0), 120)
                    if st <= krow < st + 8:
                        V[ci, c + 3, kr * 64:(kr + 1) * 64, qr * 64:(qr + 1) * 64] = col_ok.T
    return V


def _nat_bias_index():
    k = np.arange(128)
    q = np.arange(128)
    kr, kc = k // 64, k % 64
    qr, qc = q // 64, q % 64
    dcol = np.clip(kc[:, None] - qc[None, :] + 15, 0, 30)
    idx_r = np.zeros((7, 128, 128), np.int64)
    for c in range(-3, 4):
        idx_r[c + 3] = np.clip(2 * c + kr[:, None] - qr[None, :] + 7, 0, 14)
    return idx_r, np.broadcast_to(dcol, (7, 128, 128))


def prep_fused(inp):
    f32 = lambda a: np.ascontiguousarray(a, dtype=np.float32)
    tab, rid = _rope_tables()
    x = np.asarray(inp['x'], dtype=np.float32)
    xc = np.asarray(inp['ctx'], dtype=np.float32)
    idx_r, idx_c = _nat_bias_index()
    tri_prev = (np.arange(128)[:, None] >= np.arange(128)[None, :]).astype(np.float32)
    tri_next = (np.arange(128)[:, None] <= np.arange(128)[None, :]).astype(np.float32)
    common = dict(ident=np.eye(128, dtype=np.float32), router_w=f32(inp['router_w']))
    for l in range(2):
        vecs = [inp['win_q_norm'][l], inp['win_k_norm'][l], inp['dif_q_norm'][l], inp['dif_k_norm'][l],
                inp['nat_q_norm'][l], inp['nat_k_norm'][l], inp['mla_q_norm'][l], inp['mla_k_norm'][l],
                inp['mla_q_a_norm'][l], inp['mla_kv_a_norm'][l], inp['dif_subln'][l], inp['win_sink'][l],
                inp['dif_lambda'][l].reshape(-1), inp['router_b'], inp['ada_b'][l], inp['norm1_g'][l], inp['norm2_g'][l]]
        vec = f32(np.concatenate([np.asarray(v).reshape(-1) for v in vecs])[None, :])
        assert vec.shape[1] == NVEC
        rpb = np.asarray(inp['nat_rpb'][l])
        common.update({
            'vec_%d' % l: vec, 'ada_w_%d' % l: f32(inp['ada_w'][l]), 'w_in_%d' % l: f32(inp['w_in'][l]),
            'wq_b_%d' % l: f32(inp['mla_wq_b'][l]), 'wkv_b_%d' % l: f32(inp['mla_wkv_b'][l]),
            'w_branch_%d' % l: f32(np.asarray(inp['w_branch'][l]).reshape(2048, D)), 'w_out_%d' % l: f32(inp['w_out'][l]),
            'moe_w1_%d' % l: f32(inp['moe_w1'][l]), 'moe_w3_%d' % l: f32(inp['moe_w3'][l]), 'moe_w2_%d' % l: f32(inp['moe_w2'][l]),
            'natb_%d' % l: f32(np.transpose(rpb[:, idx_r, idx_c], (1, 2, 0, 3)))})
    maps = []
    zeros_h = np.zeros((384, D), np.float32)
    rid384 = np.repeat(rid, 384, 0)
    rc = np.repeat(rid, NCTX, 0)
    for core in range(8):
        b, s = core // 4, core % 4
        sigma = [s] + [g for g in range(4) if g != s]
        m = dict(common)
        m['xall'] = f32(np.concatenate([xc[b]] + [x[b, 2048 * g:2048 * (g + 1)] for g in sigma], 0))
        m['rope_all'] = f32(np.concatenate([rc] + [tab[2048 * g:2048 * (g + 1)] for g in sigma], 0))
        for j, g in enumerate(sigma):
            o0 = 2048 * g
            hb = x[b, o0 - 384:o0] if g > 0 else zeros_h
            ha = x[b, o0 + 2048:o0 + 2048 + 384] if g < 3 else zeros_h
            rb = tab[o0 - 384:o0] if g > 0 else rid384
            ra = tab[o0 + 2048:o0 + 2048 + 384] if g < 3 else rid384
            m['xloc%d' % j] = f32(np.concatenate([hb, x[b, o0:o0 + 2048], ha, xc[b]], 0))
            m['rope_loc%d' % j] = f32(np.concatenate([rb, tab[o0:o0 + 2048], ra, rc], 0))
            m['wmask%d' % j] = f32(np.stack([tri_prev if g > 0 else np.zeros_like(tri_prev), tri_prev, tri_next,
                                             tri_next if g < 3 else np.zeros_like(tri_next)], 0))
            m['natv%d' % j] = f32(_nat_consts(g))
        hs = np.zeros((128, 8), np.float32)
        for r in (1, 2, 3):
            if sigma[r] == s - 1:
                hs[:, r] = 1.0
            if sigma[r] == s + 1:
                hs[:, 4 + r] = 1.0
        m['hsel'] = hs
        m['cvec'] = f32(np.stack([inp['c'][b], inp['c_ctx']], 0).reshape(2, 8, 128).transpose(2, 0, 1).reshape(128, 16))
        maps.append(m)
    return maps


def kernel(**inputs):
    inp = {k: np.asarray(v) for k, v in inputs.items()}
    nc = build_fused()
    maps = prep_fused(inp)
    res = run_bass_kernel_spmd(nc, maps, core_ids=list(range(8)))
    out = np.empty((2, SEQ, D), np.float32)
    for core in range(8):
        b, s = core // 4, core % 4
        out[b, 2048 * s:2048 * (s + 1)] = np.asarray(res.results[core]['x_out'])
    return out
```

```python
import numpy as np
import ml_dtypes
import concourse.bass as bass
import concourse.mybir as mybir
from concourse.bass_utils import run_bass_kernel_spmd

F32 = mybir.dt.float32
BF16 = mybir.dt.bfloat16
I32 = mybir.dt.int32
AF = mybir.ActivationFunctionType
ALU = mybir.AluOpType
AX = mybir.AxisListType


class Buf:
    __slots__ = ("t", "w", "r", "name")

    def __init__(self, t, name=""):
        self.t = t
        self.w = None
        self.r = {}
        self.name = name


class _Eng:
    def __init__(self, name, eng, sem):
        self.name, self.eng, self.sem = name, eng, sem
        self.count = 0
        self.seen = {}
        self.dq = []
        self.dn = 0


class FW:
    NDMA = 6

    def __init__(self, nc):
        self.nc = nc
        self.stack = None
        self.E = {}
        self.out_tokens = []

    def __enter__(self):
        from contextlib import ExitStack
        self.stack = ExitStack()
        self.stack.__enter__()
        nc = self.nc
        for name, eng in (("pe", nc.tensor), ("act", nc.scalar), ("dve", nc.vector),
                          ("pool", nc.gpsimd), ("sp", nc.sync)):
            sem = self.stack.enter_context(nc.semaphore("s_" + name))
            self.E[name] = _Eng(name, eng, sem)
        for q in ("sp", "act", "pool"):
            for i in range(self.NDMA):
                self.E[q].dq.append(self.stack.enter_context(nc.semaphore("d_%s%d" % (q, i))))
        return self

    def __exit__(self, *a):
        return self.stack.__exit__(*a)

    nalloc = 0

    def sb(self, name, shape, dt):
        self.nalloc += 1
        name = "%s_u%d" % (name, self.nalloc)
        return Buf(self.stack.enter_context(self.nc.sbuf_tensor(name, list(shape), dt)), name)

    def ps(self, name, shape, dt=F32):
        self.nalloc += 1
        name = "%s_u%d" % (name, self.nalloc)
        return Buf(self.stack.enter_context(self.nc.psum_tensor(name, list(shape), dt)), name)

    def scope(self):
        fw = self

        class _S:
            def __enter__(s):
                from contextlib import ExitStack
                s.prev = fw.stack
                fw.stack = ExitStack()
                fw.stack.__enter__()
                return s

            def __exit__(s, *a):
                fw.barrier()
                r = fw.stack.__exit__(*a)
                fw.stack = s.prev
                return r
        return _S()

    def _wait(self, E, tok):
        sem, val = tok
        if E.name == "pe" and sem is E.sem:
            return
        key = id(sem)
        if E.seen.get(key, 0) >= val:
            return
        E.eng.wait_ge(sem, val)
        E.seen[key] = val

    def _deps(self, E, reads, writes):
        toks = []
        for b in reads:
            if b.w is not None:
                toks.append(b.w)
        for b in writes:
            if b.w is not None:
                toks.append(b.w)
            toks.extend(b.r.values())
        for tok in toks:
            self._wait(E, tok)

    def _mark(self, tok, reads, writes):
        key = id(tok[0])
        for b in reads:
            old = b.r.get(key)
            if old is None or old[1] < tok[1]:
                b.r[key] = tok
        for b in writes:
            b.w = tok
            b.r = {}

    nops = 0
    maxops = 10 ** 9
    rec = None

    def op(self, en, fn, reads=(), writes=()):
        if self.rec is not None:
            self.rec.append((0, en, fn, tuple(reads), tuple(writes)))
            return
        self.nops += 1
        if self.nops > self.maxops:
            return
        E = self.E[en]
        self._deps(E, reads, writes)
        ins = fn(E.eng)
        E.count += 1
        ins.then_inc(E.sem, 1)
        self._mark((E.sem, E.count), reads, writes)

    def dma(self, q, out_ap, in_ap, reads=(), writes=(), out=False):
        if self.rec is not None:
            self.rec.append((1, q, out_ap, in_ap, tuple(reads), tuple(writes), out))
            return
        self.nops += 1
        if self.nops > self.maxops:
            return
        E = self.E[q]
        self._deps(E, reads, writes)
        k = E.dn % self.NDMA
        rnd = E.dn // self.NDMA
        sem = E.dq[k]
        if rnd > 0:
            self._wait(E, (sem, 16 * rnd))
        E.eng.dma_start(out=out_ap, in_=in_ap).then_inc(sem, 16)
        E.dn += 1
        tok = (sem, 16 * (rnd + 1))
        self._mark(tok, reads, writes)
        if out:
            self.out_tokens.append(tok)
        return tok

    def emit_interleaved(self, streams):
        n = max(len(st) for st in streams)
        for i in range(n):
            for st in streams:
                if i < len(st):
                    r = st[i]
                    if r[0] == 0:
                        self.op(r[1], r[2], r[3], r[4])
                    else:
                        self.dma(r[1], r[2], r[3], r[4], r[5], r[6])

    def barrier(self):
        toks = []
        for E in self.E.values():
            if E.count:
                toks.append((E.sem, E.count))
            for k, sem in enumerate(E.dq):
                n = (E.dn - k + self.NDMA - 1) // self.NDMA
                if n > 0:
                    toks.append((sem, 16 * n))
        for E in self.E.values():
            for tok in toks:
                sem, val = tok
                key = id(sem)
                if E.seen.get(key, 0) >= val:
                    continue
                E.eng.wait_ge(sem, val)
                E.seen[key] = val

    def finish(self):
        self.barrier()


D = 1024
SEQ = 8192
NCTX = 256
NT_ALL = 66
NT_LOC = 24
NQT = 18
EPS = 1e-6
C_WQ, C_WK, C_WV, C_DQ, C_DK, C_DV, C_NQ, C_NK, C_NV, C_MQA, C_MKVA, C_G = (
    0, 512, 640, 768, 1280, 1792, 2304, 2816, 3328, 3840, 4096, 4256)
V_WQN, V_WKN, V_DQN, V_DKN, V_NQN, V_NKN = 0, 64, 128, 192, 256, 320
V_MQN, V_MKN, V_MQAN, V_MKVAN, V_SUBLN, V_SINK, V_LAM, V_RB = 384, 480, 576, 832, 960, 1088, 1096, 1352
V_ADAB, V_N1, V_N2 = 1368, 1368 + 6144, 1368 + 6144 + 1024
NVEC = V_N2 + 1024
NSMALL = 1368


def bcast_rows(ap2d, row, c0, n, parts=128):
    t = ap2d.tensor
    cols = ap2d.shape[1]
    return bass.AP(t, ap2d.offset + row * cols + c0, [[0, parts], [1, n]])


CFG = {'phases': 'all', 'nA1': NT_ALL, 'nA2': NT_LOC}


def build_fused(debug=False, layers=(0, 1), segs0=(0, 1, 2, 3)):
    nc = bass.Bass("TRN2", target_bir_lowering=False)

    def din(name, shape, dt=F32):
        return nc.dram_tensor(name, list(shape), dt, kind="ExternalInput").ap()

    def dscr(name, shape, dt=BF16, out=False):
        kind = "ExternalOutput" if (out or debug) else "Internal"
        return nc.dram_tensor(name, list(shape), dt, kind=kind).ap()

    xall = din("xall", [NT_ALL * 128, D])
    rope_all = din("rope_all", [NT_ALL * 128, 96])
    xloc_s = [din("xloc%d" % j, [NT_LOC * 128, D]) for j in range(4)]
    rope_loc_s = [din("rope_loc%d" % j, [NT_LOC * 128, 96]) for j in range(4)]
    wmask_s = [din("wmask%d" % j, [4, 128, 128]) for j in range(4)]
    natv_s = [din("natv%d" % j, [5, 7, 128, 128]) for j in range(4)]
    hsel_in = din("hsel", [128, 8])
    cvec = din("cvec", [128, 16])
    ident_in = din("ident", [128, 128])
    router_w = din("router_w", [D, 16])
    LW = []
    for l in range(2):
        LW.append(dict(
            vec=din("vec_%d" % l, [1, NVEC]), ada_w=din("ada_w_%d" % l, [D, 6 * D]), w_in=din("w_in_%d" % l, [D, 8352]),
            wq_b=din("wq_b_%d" % l, [256, 768]), wkv_b=din("wkv_b_%d" % l, [128, 1024]),
            w_branch=din("w_branch_%d" % l, [2048, D]), w_out=din("w_out_%d" % l, [D, D]),
            moe_w1=din("moe_w1_%d" % l, [16, D, 512]), moe_w3=din("moe_w3_%d" % l, [16, D, 512]),
            moe_w2=din("moe_w2_%d" % l, [16, 512, D]), natb=din("natb_%d" % l, [7, 128, 8, 128])))

    x_out = dscr("x_out", [2048, D], F32, out=True)
    X1 = dscr("X1", [SEQ, D], F32)
    XC1 = dscr("XC1", [NCTX, D], F32)

    KD = dscr("KD", [512, NT_ALL * 128])
    VD = dscr("VD", [NT_ALL * 128, 512])
    KM = dscr("KM", [8, 96, NT_ALL * 128])
    VM = dscr("VM", [NT_ALL * 128, 512])
    KW = dscr("KW", [128, NT_LOC * 128])
    VW = dscr("VW", [NT_LOC * 128, 128])
    KN = dscr("KN", [512, NT_LOC * 128])
    VN = dscr("VN", [NT_LOC * 128, 512])
    QW = dscr("QW", [512, NQT * 128])
    QD = dscr("QD", [512, NQT * 128])
    QN = dscr("QN", [512, NQT * 128])
    QM = dscr("QM", [8, 96, NQT * 128])
    GATES = dscr("GATES", [NQT * 128, 4096])
    Y = dscr("Y", [NQT * 128, 2048])
    XN = dscr("XN", [NQT * 128, D], F32)
    MODS = dscr("MODS", [2, 128, 6 * D], F32)
    HTS = dscr("HTS", [NQT, 128, D])

    fw = FW(nc)
    with fw:
        op, dma = fw.op, fw.dma

        def mm(out, lhsT, rhs, start, stop, reads, writes):
            op("pe", lambda e: e.matmul(out, lhsT, rhs, start=start, stop=stop), reads, writes)

        def tr(out, in_, idn, reads, writes):
            op("pe", lambda e: e.transpose(out, in_, idn), reads, writes)

        def tt(en, out, a, b, alu, reads, writes):
            op(en, lambda e: e.tensor_tensor(out, a, b, alu), reads, writes)

        def ts(en, out, a, s1, s2, o0, o1, reads, writes):
            if s2 is None:
                op(en, lambda e: e.tensor_scalar(out, a, s1, None, o0), reads, writes)
            else:
                op(en, lambda e: e.tensor_scalar(out, a, s1, s2, o0, o1), reads, writes)

        def stt(en, out, a, s, b, o0, o1, reads, writes):
            op(en, lambda e: e.scalar_tensor_tensor(out, a, s, b, o0, o1), reads, writes)

        def act(out, in_, func, reads, writes, **kw):
            op("act", lambda e: e.activation(out, in_, func, **kw), reads, writes)

        def cp(en, out, in_, reads, writes):
            if en == "act":
                act(out, in_, AF.Copy, reads, writes)
            else:
                op(en, lambda e: e.tensor_copy(out, in_), reads, writes)

        identb = fw.sb("identb", [128, 128], BF16)
        identf = fw.sb("identf", [128, 128], F32)
        VEC = fw.sb("VEC", [128, NSMALL], F32)
        dma("pool", identb.t[:], ident_in, [], [identb])
        dma("sp", identf.t[:], ident_in, [], [identf])
        gv = lambda off, n: VEC.t[:, off:off + n]

        HSEL = fw.sb("HSEL", [128, 8], F32)
        dma("sp", HSEL.t[:], hsel_in, [], [HSEL])

        def layer_pass(lidx, seg, want_ctx, first):
            lam_init = 0.8 - 0.6 * float(np.exp(-0.3 * lidx))
            W_ = LW[lidx]
            vec, ada_w, w_in, wq_b, wkv_b = W_["vec"], W_["ada_w"], W_["w_in"], W_["wq_b"], W_["wkv_b"]
            w_branch, w_out, moe_w1, moe_w3, moe_w2, natb = W_["w_branch"], W_["w_out"], W_["moe_w1"], W_["moe_w3"], W_["moe_w2"], W_["natb"]
            rope_loc, wmask, natv = rope_loc_s[seg], wmask_s[seg], natv_s[seg]
            if first:
                dma("sp", VEC.t[:], bcast_rows(vec, 0, 0, NSMALL), [], [VEC])

            def dense_src(i):
                if lidx == 0:
                    return xall[i * 128:(i + 1) * 128, :]
                return XC1[i * 128:(i + 1) * 128, :] if i < 2 else X1[(i - 2) * 128:(i - 1) * 128, :]

            def local_src(i):
                if lidx == 0:
                    return xloc_s[seg][i * 128:(i + 1) * 128, :]
                if 3 <= i < 19:
                    return X1[(i - 3) * 128:(i - 2) * 128, :]
                if i >= 22:
                    return XC1[(i - 22) * 128:(i - 21) * 128, :]
                if i < 3:
                    return [(X1[(r * 16 + 13 + i) * 128:(r * 16 + 14 + i) * 128, :], HSEL.t[:, r:r + 1]) for r in (1, 2, 3)]
                return [(X1[(r * 16 + i - 19) * 128:(r * 16 + i - 18) * 128, :], HSEL.t[:, 4 + r:5 + r]) for r in (1, 2, 3)]

            if first:
              with fw.scope():
                cs = fw.sb("cs", [128, 2, 8], F32)
                CB = fw.sb("CB", [128, 2, 8, 128], F32)
                NG = fw.sb("NG", [128, 2, D], F32)
                dma("sp", cs.t[:].rearrange("p w j -> p (w j)"), cvec, [], [cs])
                dma("sp", NG.t[:, 0, :], bcast_rows(vec, 0, V_N1, D), [], [NG])
                dma("sp", NG.t[:, 1, :], bcast_rows(vec, 0, V_N2, D), [], [NG])
                act(cs.t[:], cs.t[:], AF.Silu, [cs], [cs])
                cp("dve", CB.t[:], cs.t[:].unsqueeze(3).to_broadcast([128, 2, 8, 128]), [cs], [CB])
                aw = [fw.sb("aw%d" % i, [128, 8, 512], F32) for i in range(2)]
                ab = [fw.sb("ab%d" % i, [128, 512], F32) for i in range(2)]
                mps = [fw.ps("mps%d" % i, [128, 512], F32) for i in range(2)]
                mo = [fw.sb("mo%d" % i, [128, 512], F32) for i in range(2)]
                n = 0
                for blk in range(12):
                    a_, b_ = aw[blk % 2], ab[blk % 2]
                    dma("sp", a_.t[:], ada_w[:, blk * 512:(blk + 1) * 512].rearrange("(j p) c -> p j c", p=128), [], [a_])
                    dma("sp", b_.t[:], bcast_rows(vec, 0, V_ADAB + blk * 512, 512), [], [b_])
                    chunk = blk // 2
                    half = blk % 2
                    for w in range(2):
                        p_, o_ = mps[n % 2], mo[n % 2]
                        n += 1
                        for j in range(8):
                            mm(p_.t[:], CB.t[:, w, j, :], a_.t[:, j, :], j == 0, j == 7, [CB, a_], [p_])
                        tt("dve", o_.t[:], p_.t[:], b_.t[:], ALU.add, [p_, b_], [o_])
                        if chunk in (1, 4):
                            g = NG.t[:, 0 if chunk == 1 else 1, half * 512:(half + 1) * 512]
                            stt("dve", o_.t[:], o_.t[:], 1.0, g, ALU.add, ALU.mult, [o_, NG], [o_])
                        dma("sp", MODS[w, :, blk * 512:(blk + 1) * 512], o_.t[:], [o_], [])
            M_SH1, M_A1, M_G1, M_SH2, M_A2, M_G2 = [i * D for i in range(6)]

            def load_mod(buf, which, off):
                dma("sp", buf.t[:], MODS[which, :, off:off + D], [], [buf])

            with fw.scope():
                A1 = [fw.sb("A1_%d" % w, [128, D], F32) for w in range(2)]
                SH1 = [fw.sb("SH1_%d" % w, [128, D], F32) for w in range(2)]
                for w in range(2):
                    load_mod(A1[w], w, M_A1)
                    load_mod(SH1[w], w, M_SH1)
                wkvb = fw.sb("wkvb", [128, 1024], BF16)
                wqb = fw.sb("wqb", [128, 2, 768], BF16)
                dma("pool", wkvb.t[:], wkv_b, [], [wkvb])
                dma("pool", wqb.t[:], wq_b.rearrange("(j p) c -> p j c", p=128), [], [wqb])
                junk = fw.sb("junk", [128, D], F32)

                class _Set:
                    pass

                def mkset(k):
                    S = _Set()
                    S.xt = fw.sb("xt", [128, D], F32)
                    S.rp = fw.sb("rp", [128, 96], F32)
                    S.st = fw.sb("st", [128, 16], F32)
                    S.hb = fw.sb("hb", [128, D], BF16)
                    S.hT = fw.sb("hT", [128, D], BF16)
                    S.ptr = fw.ps("ptr", [128, D], BF16)
                    S.pp = [fw.ps("pp", [128, 512], F32) for _ in range(3)]
                    S.xs = fw.sb("xs", [128, 1024], F32)
                    S.sq = fw.sb("sq", [128, 1024], F32)
                    S.ss = fw.sb("ss", [128, 32], F32)
                    S.yb = fw.sb("yb", [128, 1024], F32)
                    S.rt = fw.sb("rt", [128, 4, 8 * 48], F32)
                    S.ob = [fw.sb("ob", [128, 1024], BF16) for _ in range(2)]
                    S.oT = [fw.sb("oT", [128, 1024], BF16) for _ in range(2)]
                    S.mkt = fw.sb("mkt", [128, 8, 96], F32)
                    S.cnt = {"g": 0, "o": 0, "t": 0, "p": 0}
                    return S
                sets = [mkset(k) for k in range(2)]

                def hnr(S, src_aps, src_bufs, H, Dh, gain, rope):
                    g = S.cnt["g"]
                    S.cnt["g"] += 1
                    N = H * Dh
                    x_, s_, y_, r_, sq = S.xs, S.ss, S.yb, S.rt, S.sq
                    so = (g % 2) * 16
                    o_ = S.ob[S.cnt["o"] % 2]
                    S.cnt["o"] += 1
                    v3 = lambda ap: ap.rearrange("p (h d) -> p h d", h=H)
                    for (sap, c0_, nc_) in src_aps:
                        cp("act", x_.t[:, c0_:c0_ + nc_], sap, src_bufs, [x_])
                    tt("pool", sq.t[:, :N], x_.t[:, :N], x_.t[:, :N], ALU.mult, [x_], [sq])
                    op("dve", lambda e: e.tensor_reduce(s_.t[:, so:so + H], v3(sq.t[:, :N]), AX.X, ALU.add), [sq], [s_])
                    act(s_.t[:, so + 8:so + 8 + H], s_.t[:, so:so + H], AF.Sqrt, [s_], [s_], bias=EPS, scale=1.0 / Dh)
                    op("dve", lambda e: e.reciprocal(s_.t[:, so:so + H], s_.t[:, so + 8:so + 8 + H]), [s_], [s_])
                    tt("dve", v3(y_.t[:, :N]), v3(x_.t[:, :N]), s_.t[:, so:so + H].unsqueeze(2).to_broadcast([128, H, Dh]),
                       ALU.mult, [x_, s_], [y_])
                    gb = gain.unsqueeze(1).to_broadcast([128, H, Dh])
                    if rope is None:
                        tt("pool", v3(o_.t[:, :N]), v3(y_.t[:, :N]), gb, ALU.mult, [y_, VEC], [o_])
                        return o_
                    r0, n, cos_ap, sin_ap, rbuf = rope
                    hlf = n // 2
                    tt("pool", v3(y_.t[:, :N]), v3(y_.t[:, :N]), gb, ALU.mult, [y_, VEC], [y_])
                    y3 = v3(y_.t[:, :N])
                    o3 = v3(o_.t[:, :N])
                    x1, x2 = y3[:, :, r0:r0 + hlf], y3[:, :, r0 + hlf:r0 + n]
                    cb = cos_ap.unsqueeze(1).to_broadcast([128, H, hlf])
                    sb_ = sin_ap.unsqueeze(1).to_broadcast([128, H, hlf])
                    t = [r_.t[:, i, :H * hlf].rearrange("p (h d) -> p h d", h=H) for i in range(4)]
                    tt("dve", t[0], x1, cb, ALU.mult, [y_, rbuf], [r_])
                    tt("pool", t[1], x2, sb_, ALU.mult, [y_, rbuf], [r_])
                    tt("dve", t[2], x1, sb_, ALU.mult, [y_, rbuf], [r_])
                    tt("pool", t[3], x2, cb, ALU.mult, [y_, rbuf], [r_])
                    tt("dve", o3[:, :, r0:r0 + hlf], t[0], t[1], ALU.subtract, [r_], [o_])
                    tt("pool", o3[:, :, r0 + hlf:r0 + n], t[2], t[3], ALU.add, [r_], [o_])
                    if r0 > 0:
                        cp("act", o3[:, :, 0:r0], y3[:, :, 0:r0], [y_], [o_])
                    return o_

                def transp(S, o_, nblk, bw):
                    k = S.cnt["t"]
                    S.cnt["t"] += 1
                    p_, t_ = S.ptr, S.oT[k % 2]
                    for i in range(nblk):
                        tr(p_.t[0:bw, i * 128:(i + 1) * 128], o_.t[:, i * bw:(i + 1) * bw], identb.t[:], [o_, identb], [p_])
                    cp("dve" if k % 2 else "act", t_.t[0:bw, 0:nblk * 128], p_.t[0:bw, 0:nblk * 128], [p_], [t_])
                    return t_

                def transp_store(S, o_, nblk, bw, dst_ap):
                    t_ = transp(S, o_, nblk, bw)
                    dma("sp", dst_ap, t_.t[0:bw, 0:nblk * 128].rearrange("p (i t) -> p i t", i=nblk), [t_], [])

                def plain_store(S, src_ap, src_bufs, N, dst_ap):
                    o_ = S.ob[S.cnt["o"] % 2]
                    S.cnt["o"] += 1
                    cp("act", o_.t[:, :N], src_ap, src_bufs, [o_])
                    dma("sp", dst_ap, o_.t[:, :N], [o_], [])

                def make_hT(S, xsrc, rsrc, row0, which):
                    x_, r_, s_, hT_, h1 = S.xt, S.rp, S.st, S.hT, S.yb
                    if isinstance(xsrc, list):
                        for ci_, (cap, wap) in enumerate(xsrc):
                            dma("sp", S.sq.t[:], cap, [], [S.sq])
                            if ci_ == 0:
                                ts("dve", x_.t[:], S.sq.t[:], wap, None, ALU.mult, None, [S.sq, HSEL], [x_])
                            else:
                                stt("dve", x_.t[:], S.sq.t[:], wap, x_.t[:], ALU.mult, ALU.add, [S.sq, HSEL, x_], [x_])
                    else:
                        dma("sp", x_.t[:], xsrc, [], [x_])
                    dma("sp", r_.t[:], rsrc[row0:row0 + 128, :], [], [r_])
                    act(junk.t[:], x_.t[:], AF.Square, [x_], [junk, s_], accum_out=s_.t[:, 0:1])
                    act(s_.t[:, 1:2], s_.t[:, 0:1], AF.Sqrt, [s_], [s_], bias=EPS, scale=1.0 / D)
                    op("dve", lambda e: e.reciprocal(s_.t[:, 2:3], s_.t[:, 1:2]), [s_], [s_])
                    stt("dve", h1.t[:], x_.t[:], s_.t[:, 2:3], A1[which].t[:], ALU.mult, ALU.mult, [x_, s_, A1[which]], [h1])
                    tt("pool", S.hb.t[:], h1.t[:], SH1[which].t[:], ALU.add, [h1, SH1[which]], [S.hb])
                    p_ = S.ptr
                    for j in range(8):
                        tr(p_.t[:, j * 128:(j + 1) * 128], S.hb.t[:, j * 128:(j + 1) * 128], identb.t[:], [S.hb, identb], [p_])
                    cp("dve", hT_.t[:], p_.t[:], [p_], [hT_])
                    return hT_, r_

                def nextp(S):
                    S.cnt["p"] += 1
                    return S.pp[S.cnt["p"] % 3]

                def project(S, hT_, W, c0, ncol):
                    pbuf = nextp(S)
                    for j in range(8):
                        mm(pbuf.t[:, 0:ncol], hT_.t[:, j * 128:(j + 1) * 128], W.t[:, j, c0:c0 + ncol], j == 0, j == 7, [hT_, W], [pbuf])
                    return pbuf

                def run_interleaved(tile_fn, tiles):
                    for a in range(0, len(tiles), 2):
                        streams = []
                        for k, i in enumerate(tiles[a:a + 2]):
                            fw.rec = []
                            tile_fn(i, sets[k])
                            streams.append(fw.rec)
                            fw.rec = None
                        fw.emit_interleaved(streams)

                if first:
                  with fw.scope():
                    Wd = fw.sb("Wd", [128, 8, 1184], BF16)
                    dma("pool", Wd.t[:, :, 0:1024], w_in[:, C_DK:C_DK + 1024].rearrange("(j p) c -> p j c", p=128), [], [Wd])
                    dma("pool", Wd.t[:, :, 1024:1184], w_in[:, C_MKVA:C_MKVA + 160].rearrange("(j p) c -> p j c", p=128), [], [Wd])

                    def a1_tile(i, S):
                        which = 1 if i < 2 else 0
                        hT_, r_ = make_hT(S, dense_src(i), rope_all, i * 128, which)
                        t0 = i * 128
                        cosh, sinh, cosm, sinm = r_.t[:, 0:32], r_.t[:, 32:64], r_.t[:, 64:80], r_.t[:, 80:96]
                        mkt = S.mkt
                        p_ = project(S, hT_, Wd, 0, 512)
                        o_ = hnr(S, [(p_.t[:, 0:512], 0, 512)], [p_], 8, 64, gv(V_DKN, 64), (0, 64, cosh, sinh, r_))
                        transp_store(S, o_, 4, 128, KD.rearrange("(i q) t -> q i t", q=128)[:, :, t0:t0 + 128])
                        p_ = project(S, hT_, Wd, 512, 512)
                        plain_store(S, p_.t[:, 0:512], [p_], 512, VD[t0:t0 + 128, :])
                        p_ = project(S, hT_, Wd, 1024, 160)
                        cp("act", mkt.t[:, :, 64:96], p_.t[:, 128:160].unsqueeze(1).to_broadcast([128, 8, 32]), [p_], [mkt])
                        o_ = hnr(S, [(p_.t[:, 0:128], 0, 128)], [p_], 1, 128, gv(V_MKVAN, 128), None)
                        t_ = transp(S, o_, 1, 128)
                        o2 = S.ob[S.cnt["o"] % 2]
                        S.cnt["o"] += 1
                        for hh in range(2):
                            p2 = nextp(S)
                            mm(p2.t[:, 0:512], t_.t[:, 0:128], wkvb.t[:, hh * 512:(hh + 1) * 512], True, True, [t_, wkvb], [p2])
                            kv3 = p2.t[:, 0:512].rearrange("p (h d) -> p h d", h=4)
                            cp("dve", mkt.t[:, hh * 4:hh * 4 + 4, 0:64], kv3[:, :, 0:64], [p2], [mkt])
                            cp("dve", o2.t[:, hh * 256:hh * 256 + 256].rearrange("p (h d) -> p h d", h=4), kv3[:, :, 64:128], [p2], [o2])
                        dma("sp", VM[t0:t0 + 128, :], o2.t[:, 0:512], [o2], [])
                        o_ = hnr(S, [(mkt.t[:].rearrange("p h d -> p (h d)"), 0, 768)], [mkt], 8, 96, gv(V_MKN, 96), (64, 32, cosm, sinm, r_))
                        transp_store(S, o_, 8, 96, KM.rearrange("h d t -> d h t")[:, :, t0:t0 + 128])
                    run_interleaved(a1_tile, list(range(CFG['nA1'])))

                with fw.scope():
                    Wl = fw.sb("Wl", [128, 8, 3072], BF16)
                    for (dst0, c0, ncol) in ((0, 0, 1280), (1280, C_NQ, 1792)):
                        for s0 in range(0, ncol, 640):
                            sn = min(640, ncol - s0)
                            dma("pool", Wl.t[:, :, dst0 + s0:dst0 + s0 + sn],
                                w_in[:, c0 + s0:c0 + s0 + sn].rearrange("(j p) c -> p j c", p=128), [], [Wl])
                    L_WQ, L_WKV, L_DQ, L_NQ, L_NK, L_NV, L_MQA = 0, 512, 768, 1280, 1792, 2304, 2816

                    def a2_tile(i, S):
                        is_ctx = i >= 22
                        is_own = 3 <= i < 19
                        wantq = is_own or (is_ctx and want_ctx)
                        qs = (i - 3) if is_own else (16 + i - 22)
                        hT_, r_ = make_hT(S, local_src(i), rope_loc, i * 128, 1 if is_ctx else 0)
                        t0 = i * 128
                        q0 = qs * 128
                        cosh, sinh, cosm, sinm = r_.t[:, 0:32], r_.t[:, 32:64], r_.t[:, 64:80], r_.t[:, 80:96]
                        p_ = project(S, hT_, Wl, L_WKV, 256)
                        o_ = hnr(S, [(p_.t[:, 0:128], 0, 128)], [p_], 2, 64, gv(V_WKN, 64), (0, 64, cosh, sinh, r_))
                        transp_store(S, o_, 1, 128, KW[:, t0:t0 + 128].unsqueeze(1))
                        plain_store(S, p_.t[:, 128:256], [p_], 128, VW[t0:t0 + 128, :])
                        p_ = project(S, hT_, Wl, L_NK, 512)
                        o_ = hnr(S, [(p_.t[:, 0:512], 0, 512)], [p_], 8, 64, gv(V_NKN, 64), None)
                        transp_store(S, o_, 4, 128, KN.rearrange("(i q) t -> q i t", q=128)[:, :, t0:t0 + 128])
                        p_ = project(S, hT_, Wl, L_NV, 512)
                        plain_store(S, p_.t[:, 0:512], [p_], 512, VN[t0:t0 + 128, :])
                        if not wantq:
                            return
                        dma("sp", HTS[qs], hT_.t[:], [hT_], [])
                        for (lc, gain, dst, rope) in ((L_WQ, V_WQN, QW, True), (L_DQ, V_DQN, QD, True), (L_NQ, V_NQN, QN, False)):
                            p_ = project(S, hT_, Wl, lc, 512)
                            o_ = hnr(S, [(p_.t[:, 0:512], 0, 512)], [p_], 8, 64, gv(gain, 64), (0, 64, cosh, sinh, r_) if rope else None)
                            transp_store(S, o_, 4, 128, dst.rearrange("(i q) t -> q i t", q=128)[:, :, q0:q0 + 128])
                        p_ = project(S, hT_, Wl, L_MQA, 256)
                        o_ = hnr(S, [(p_.t[:, 0:256], 0, 256)], [p_], 1, 256, gv(V_MQAN, 256), None)
                        t_ = transp(S, o_, 2, 128)
                        pa_, pb_ = nextp(S), nextp(S)
                        for (pq, n0, nn) in ((pa_, 0, 512), (pb_, 512, 256)):
                            for j in range(2):
                                mm(pq.t[:, 0:nn], t_.t[:, j * 128:(j + 1) * 128], wqb.t[:, j, n0:n0 + nn], j == 0, j == 1, [t_, wqb], [pq])
                        o_ = hnr(S, [(pa_.t[:, 0:512], 0, 512), (pb_.t[:, 0:256], 512, 256)], [pa_, pb_], 8, 96, gv(V_MQN, 96), (64, 32, cosm, sinm, r_))
                        transp_store(S, o_, 8, 96, QM.rearrange("h d t -> d h t")[:, :, q0:q0 + 128])
                    run_interleaved(a2_tile, list(range(NT_LOC)) if CFG['nA2'] == NT_LOC else [0, 3, 22][:CFG['nA2']])

                with fw.scope():
                    Wg = [fw.sb("Wg%d" % i, [128, 8, 1024], BF16) for i in range(2)]
                    gt = [fw.sb("gt%d" % i, [128, 1024], BF16) for i in range(2)]
                    gp = [sets[0].pp[0], sets[0].pp[1], sets[1].pp[0], sets[1].pp[1]]
                    nq_tiles = NQT if want_ctx else 16
                    if CFG['nA2'] != NT_LOC:
                        nq_tiles = 0
                    HT = fw.sb("HT", [128, NQT, D], BF16)
                    for qs in range(nq_tiles):
                        dma("sp", HT.t[:, qs, :], HTS[qs], [], [HT])
                    n = 0
                    for gq in range(4):
                        W = Wg[gq % 2]
                        for s0 in (0, 512):
                            dma("pool", W.t[:, :, s0:s0 + 512],
                                w_in[:, C_G + gq * 1024 + s0:C_G + gq * 1024 + s0 + 512].rearrange("(j p) c -> p j c", p=128), [], [W])
                        for qs in range(nq_tiles):
                            g_ = gt[n % 2]
                            for hf in range(2):
                                p_ = gp[(2 * n + hf) % 4]
                                for j in range(8):
                                    mm(p_.t[:], HT.t[:, qs, j * 128:(j + 1) * 128], W.t[:, j, hf * 512:(hf + 1) * 512], j == 0, j == 7, [HT, W], [p_])
                                cp("act" if hf else "dve", g_.t[:, hf * 512:(hf + 1) * 512], p_.t[:], [p_], [g_])
                            n += 1
                            dma("sp", GATES[qs * 128:(qs + 1) * 128, gq * 1024:(gq + 1) * 1024], g_.t[:], [g_], [])

            if CFG['phases'] == 'all' or 'B' in CFG['phases']:
              with fw.scope():
                ES = fw.sb("ES", [128, 8], F32)
                act(ES.t[:], gv(V_SINK, 8), AF.Exp, [VEC], [ES])
                LM = fw.sb("LM", [128, 8], F32)
                lt = fw.sb("lt", [128, 128], F32)
                tt("dve", lt.t[:, 0:64], gv(V_LAM, 64), gv(V_LAM + 64, 64), ALU.mult, [VEC], [lt])
                tt("dve", lt.t[:, 64:128], gv(V_LAM + 128, 64), gv(V_LAM + 192, 64), ALU.mult, [VEC, lt], [lt])
                op("dve", lambda e: e.tensor_reduce(LM.t[:, 0:2], lt.t[:].rearrange("p (a d) -> p a d", a=2), AX.X, ALU.add), [lt], [LM])
                act(LM.t[:, 2:4], LM.t[:, 0:2], AF.Exp, [LM], [LM])
                tt("dve", LM.t[:, 4:5], LM.t[:, 2:3], LM.t[:, 3:4], ALU.subtract, [LM], [LM])
                ts("dve", LM.t[:, 5:6], LM.t[:, 4:5], -1.0, -lam_init, ALU.mult, ALU.add, [LM], [LM])
                NEGLAM = LM.t[:, 5:6]
                SG = fw.sb("SG", [128, 128], F32)
                ts("dve", SG.t[:], gv(V_SUBLN, 128), 1.0 - lam_init, None, ALU.mult, None, [VEC], [SG])

                acc = [fw.ps("acc%d" % i, [128, 512], F32) for i in range(4)]
                sps = [fw.ps("sps%d" % i, [128, 512], F32) for i in range(4)]
                pt = [fw.sb("pt%d" % i, [128, 512], BF16) for i in range(4)]
                fz = [fw.sb("fz%d" % i, [128, 8], F32) for i in range(4)]
                cs_ = [0]
                nqt = NQT if want_ctx else 16

                def attn(chunks, nacc, dv, scale, qn=512):
                    n = len(chunks)
                    GRP = 2
                    groups = [list(range(a, min(n, a + GRP))) for a in range(0, n, GRP)]
                    live = {}
                    for gi in range(len(groups) + 1):
                        if gi < len(groups):
                            for it in groups[gi]:
                                ch = chunks[it]
                                k = cs_[0]
                                cs_[0] += 1
                                s_, p_ = sps[k % 4], pt[k % 4]
                                for (l_ap, r_ap, c0, ncl) in ch['mms']:
                                    mm(s_.t[:, c0:c0 + ncl], l_ap, r_ap, True, True, ch['rb'], [s_])
                                live[it] = (s_, p_)
                            for it in groups[gi]:
                                ch = chunks[it]
                                s_, p_ = live[it]
                                act(p_.t[:, :qn], s_.t[:, :qn], AF.Exp, [s_], [p_], scale=scale)
                                for mi, (m_ap, m_buf) in enumerate(ch.get('masks', ())):
                                    p3 = p_.t[:, :].rearrange("p (h q) -> p h q", h=4)
                                    tt("pool" if mi % 2 == 0 else "dve", p3, p3, m_ap, ALU.mult, [p_, m_buf], [p_])
                        if gi >= 1:
                            for ci in groups[gi - 1]:
                                ch = chunks[ci]
                                s_, p_ = live.pop(ci)
                                for j in range(nacc):
                                    mm(acc[j].t[:, 0:dv + 1], p_.t[:, j * 128:(j + 1) * 128], ch['v'][j], ci == 0, ci == n - 1,
                                       [p_] + ch['vb'], [acc[j]])

                def fin_simple(j, dst_ap, dst_buf, dv, sink_ap=None):
                    z_ = fz[j]
                    if sink_ap is not None:
                        tt("dve", z_.t[:, 0:1], acc[j].t[:, dv:dv + 1], sink_ap, ALU.add, [acc[j], ES], [z_])
                        op("dve", lambda e: e.reciprocal(z_.t[:, 1:2], z_.t[:, 0:1]), [z_], [z_])
                    else:
                        op("dve", lambda e: e.reciprocal(z_.t[:, 1:2], acc[j].t[:, dv:dv + 1]), [acc[j]], [z_])
                    ts("dve", dst_ap, acc[j].t[:, 0:dv], z_.t[:, 1:2], None, ALU.mult, None, [acc[j], z_], [dst_buf])

                Ysb = [fw.sb("Ysb%d" % i, [128, NQT, 512], BF16) for i in range(2)]

                def store_branch(n, ybuf):
                    dma("sp", Y[0:nqt * 128, n * 512:(n + 1) * 512].rearrange("(t p) c -> p t c", p=128), ybuf.t[:, 0:nqt, :], [ybuf], [])

                if 'nodense' not in CFG['phases']:
                  with fw.scope():
                    kb = [fw.sb("kb%d" % i, [96, NT_ALL * 128], BF16) for i in range(2)]
                    vb = [fw.sb("vb%d" % i, [128, NT_ALL, 129], BF16) for i in range(2)]
                    qb_ = [fw.sb("qb%d" % i, [96, NQT * 128], BF16) for i in range(2)]
                    D1 = fw.sb("D1", [128, NQT, 128], F32)
                    dtm = [fw.sb("dtm%d" % i, [128, 128], F32) for i in range(2)]
                    dsq = fw.sb("dsq", [128, 128], F32)
                    hcount = [0]

                    def dense_head(KTsrc, d, QTsrc, V1, dv, scale, fin):
                        hi = hcount[0]
                        hcount[0] += 1
                        KT, QT = kb[hi % 2], qb_[hi % 2]
                        dma("sp", KT.t[0:d, :], KTsrc, [], [KT])
                        dma("sp", QT.t[0:d, :], QTsrc, [], [QT])
                        for qblk in range(4):
                            chunks = [dict(mms=[(KT.t[0:d, c * 128:(c + 1) * 128], QT.t[0:d, qblk * 512:(qblk + 1) * 512], 0, 512)],
                                           rb=[KT, QT], v=[V1.t[:, c, 0:dv + 1]] * 4, vb=[V1]) for c in range(CFG.get('nkc', NT_ALL))]
                            attn(chunks, 4, dv, scale)
                            for j in range(4):
                                fin(j, qblk * 4 + j)
                        if want_ctx:
                            chunks = [dict(mms=[(KT.t[0:d, c * 128:(c + 1) * 128], QT.t[0:d, 2048:2304], 0, 256)],
                                           rb=[KT, QT], v=[V1.t[:, c, 0:dv + 1]] * 2, vb=[V1]) for c in range(2)]
                            attn(chunks, 2, dv, scale, qn=256)
                            for j in range(2):
                                fin(j, 16 + j)

                    Yd = Ysb[0]
                    for hd in range(4):
                        V1 = vb[hd % 2]
                        dma("sp", V1.t[:, :, 0:128], VD[:, hd * 128:(hd + 1) * 128].rearrange("(c p) d -> p c d", p=128), [], [V1])
                        op("pool", lambda e: e.memset(V1.t[:, :, 128:129], 1.0), [], [V1])
                        for i2 in range(2):
                            hs = 2 * hd + i2

                            def fin(j, qt, i2=i2, hd=hd):
                                if i2 == 0:
                                    fin_simple(j, D1.t[:, qt, :], D1, 128)
                                    return
                                t_ = dtm[j % 2]
                                z_ = fz[j]
                                fin_simple(j, t_.t[:], t_, 128)
                                stt("dve", t_.t[:], t_.t[:], NEGLAM, D1.t[:, qt, :], ALU.mult, ALU.add, [t_, LM, D1], [t_])
                                tt("pool", dsq.t[:], t_.t[:], t_.t[:], ALU.mult, [t_], [dsq])
                                op("dve", lambda e: e.tensor_reduce(z_.t[:, 2:3], dsq.t[:], AX.X, ALU.add), [dsq], [z_])
                                act(z_.t[:, 3:4], z_.t[:, 2:3], AF.Sqrt, [z_], [z_], bias=EPS, scale=1.0 / 128)
                                op("dve", lambda e: e.reciprocal(z_.t[:, 4:5], z_.t[:, 3:4]), [z_], [z_])
                                stt("dve", Yd.t[:, qt, hd * 128:(hd + 1) * 128], t_.t[:], z_.t[:, 4:5], SG.t[:], ALU.mult, ALU.mult,
                                    [t_, z_, SG], [Yd])
                            dense_head(KD[hs * 64:(hs + 1) * 64, :], 64, QD[hs * 64:(hs + 1) * 64, :], V1, 128, 0.125, fin)
                    store_branch(1, Yd)

                    Ym = Ysb[1]
                    for h in range(8):
                        V1 = vb[h % 2]
                        dma("sp", V1.t[:, :, 0:64], VM[:, h * 64:(h + 1) * 64].rearrange("(c p) d -> p c d", p=128), [], [V1])
                        op("pool", lambda e: e.memset(V1.t[:, :, 64:65], 1.0), [], [V1])

                        def fin(j, qt, h=h):
                            fin_simple(j, Ym.t[:, qt, h * 64:(h + 1) * 64], Ym, 64)
                        dense_head(KM[h], 96, QM[h], V1, 64, 96.0 ** -0.5, fin)
                    store_branch(3, Ym)

                with fw.scope():
                    Yw = Ysb[0]
                    WM = fw.sb("WM", [128, 4, 128], BF16)
                    dma("pool", WM.t[:], wmask.rearrange("m k q -> k m q"), [], [WM])
                    kw_ = [fw.sb("kw%d" % i, [64, NT_LOC * 128], BF16) for i in range(2)]
                    vw_ = [fw.sb("vw%d" % i, [128, NT_LOC, 65], BF16) for i in range(2)]
                    qw_ = [fw.sb("qw%d" % i, [64, NQT, 4, 128], BF16) for i in range(2)]
                    for g in range(2):
                        KT, V1, QT = kw_[g], vw_[g], qw_[g]
                        dma("sp", KT.t[:], KW[g * 64:(g + 1) * 64, :], [], [KT])
                        dma("sp", V1.t[:, :, 0:64], VW[:, g * 64:(g + 1) * 64].rearrange("(c p) d -> p c d", p=128), [], [V1])
                        op("pool", lambda e: e.memset(V1.t[:, :, 64:65], 1.0), [], [V1])
                        for hq in range(4):
                            dma("sp", QT.t[:, :, hq, :], QW[(g * 4 + hq) * 64:(g * 4 + hq + 1) * 64, :].rearrange("d (t q) -> d t q", q=128), [], [QT])
                        for qt in range(nqt):
                            rhs = QT.t[0:64, qt, :, :].rearrange("p h q -> p (h q)")
                            if qt < 16:
                                sl = [(qt + 2, WM.t[:, 0 if qt == 0 else 1, :]), (qt + 3, None), (qt + 4, WM.t[:, 3 if qt == 15 else 2, :]), (22, None), (23, None)]
                            else:
                                sl = [(22, None), (23, None)]
                            chunks = []
                            for (slot, m) in sl:
                                ch = dict(mms=[(KT.t[0:64, slot * 128:(slot + 1) * 128], rhs, 0, 512)], rb=[KT, QT],
                                          v=[V1.t[:, slot, 0:65]] * 4, vb=[V1])
                                if m is not None:
                                    ch['masks'] = [(m.unsqueeze(1).to_broadcast([128, 4, 128]), WM)]
                                chunks.append(ch)
                            attn(chunks, 4, 64, 0.125)
                            for j in range(4):
                                h = g * 4 + j
                                fin_simple(j, Yw.t[:, qt, h * 64:(h + 1) * 64], Yw, 64, sink_ap=ES.t[:, h:h + 1])
                    store_branch(0, Yw)

                with fw.scope():
                    Yn = Ysb[1]
                    NVm = fw.sb("NVm", [128, 5, 7, 128], BF16)
                    dma("pool", NVm.t[:], natv.rearrange("a c k q -> k a c q"), [], [NVm])
                    ebf = fw.sb("ebf", [128, 7, 4, 128], F32)
                    EB = [fw.sb("EB%d" % i, [128, 7, 4, 128], BF16) for i in range(2)]
                    kn_ = [fw.sb("kn%d" % i, [64, 4, NT_LOC * 128], BF16) for i in range(2)]
                    vn_ = [fw.sb("vn%d" % i, [128, NT_LOC, 4, 65], BF16) for i in range(2)]
                    qn_ = [fw.sb("qn%d" % i, [64, NQT, 4, 128], BF16) for i in range(2)]
                    for hh in range(2):
                        KT, V1, QT, EBh = kn_[hh], vn_[hh], qn_[hh], EB[hh]
                        for c in range(7):
                            dma("sp", ebf.t[:, c, :, :], natb[c, :, hh * 4:(hh + 1) * 4, :], [], [ebf])
                        act(EBh.t[:].rearrange("p c h q -> p (c h q)"), ebf.t[:].rearrange("p c h q -> p (c h q)"), AF.Exp, [ebf], [EBh])
                        for j in range(4):
                            h = hh * 4 + j
                            dma("sp", KT.t[:, j, :], KN[h * 64:(h + 1) * 64, :], [], [KT])
                            dma("sp", V1.t[:, :, j, 0:64], VN[:, h * 64:(h + 1) * 64].rearrange("(c p) d -> p c d", p=128), [], [V1])
                            dma("sp", QT.t[:, :, j, :], QN[h * 64:(h + 1) * 64, :].rearrange("d (t q) -> d t q", q=128), [], [QT])
                        op("pool", lambda e: e.memset(V1.t[:, :, :, 64:65], 1.0), [], [V1])
                        for qt in range(nqt):
                            chunks = []
                            if qt < 16:
                                cls = {0: 0, 1: 1, 14: 3, 15: 4}.get(qt, 2)
                                lst = [(qt + c + 3, c + 3) for c in range(-3, 4)] + [(22, None), (23, None)]
                            else:
                                lst = [(22, None), (23, None)]
                            for (slot, ci) in lst:
                                ch = dict(mms=[(KT.t[0:64, j, slot * 128:(slot + 1) * 128], QT.t[0:64, qt, j, :], j * 128, 128) for j in range(4)],
                                          rb=[KT, QT], v=[V1.t[:, slot, j, 0:65] for j in range(4)], vb=[V1])
                                if ci is not None:
                                    ch['masks'] = [(EBh.t[:, ci, :, :], EBh),
                                                   (NVm.t[:, cls, ci, :].unsqueeze(1).to_broadcast([128, 4, 128]), NVm)]
                                chunks.append(ch)
                            attn(chunks, 4, 64, 0.125)
                            for j in range(4):
                                h = hh * 4 + j
                                fin_simple(j, Yn.t[:, qt, h * 64:(h + 1) * 64], Yn, 64)
                    store_branch(2, Yn)
            if CFG['phases'] == 'all' or 'C' in CFG['phases']:
              with fw.scope():
                nqt = NQT if want_ctx else 16
                H2T = fw.sb("H2T", [128, 8, NQT * 128], BF16)
                WT = fw.sb("WT", [128, NQT, 16], F32)
                xslot = lambda qt: (3 + qt) if qt < 16 else (22 + qt - 16)
                with fw.scope():
                    G1 = [fw.sb("G1_%d" % w, [128, D], F32) for w in range(2)]
                    A2 = [fw.sb("A2_%d" % w, [128, D], F32) for w in range(2)]
                    SH2 = [fw.sb("SH2_%d" % w, [128, D], F32) for w in range(2)]
                    for w in range(2):
                        load_mod(G1[w], w, M_G1)
                        load_mod(A2[w], w, M_A2)
                        load_mod(SH2[w], w, M_SH2)
                    wbr = fw.sb("wbr", [128, 16, D], BF16)
                    wo = fw.sb("wo", [128, 8, D], BF16)
                    rw = fw.sb("rw", [128, 8, 16], F32)
                    for q4 in range(4):
                        dma("pool", wbr.t[:, q4 * 4:(q4 + 1) * 4, :], w_branch[q4 * 512:(q4 + 1) * 512, :].rearrange("(j p) c -> p j c", p=128), [], [wbr])
                    for q2 in range(2):
                        dma("pool", wo.t[:, q2 * 4:(q2 + 1) * 4, :], w_out[q2 * 512:(q2 + 1) * 512, :].rearrange("(j p) c -> p j c", p=128), [], [wo])
                    dma("sp", rw.t[:], router_w.rearrange("(j p) e -> p j e", p=128), [], [rw])
                    yt = [fw.sb("yt%d" % i, [128, 2048], BF16) for i in range(2)]
                    gt_ = [fw.sb("gtc%d" % i, [128, 4096], BF16) for i in range(2)]
                    sg = fw.sb("sg", [128, 4096], BF16)
                    YT = fw.sb("YT", [128, 16, 128], BF16)
                    macc = fw.sb("macc", [128, D], F32)
                    mtmp = fw.sb("mtmp", [128, 512], F32)
                    mbf = fw.sb("mbf", [128, D], BF16)
                    mT = fw.sb("mT", [128, 8, 128], BF16)
                    xin = [fw.sb("xin%d" % i, [128, D], F32) for i in range(2)]
                    xn = [fw.sb("xn%d" % i, [128, D], F32) for i in range(2)]
                    junk2 = fw.sb("junk2", [128, D], F32)
                    h2 = fw.sb("h2", [128, D], F32)
                    h2tf = fw.sb("h2tf", [128, 8, 128], F32)
                    s2 = [fw.sb("s2_%d" % i, [128, 8], F32) for i in range(2)]
                    rr = [fw.sb("rr%d" % i, [128, 160], F32) for i in range(2)]
                    ptc = [fw.ps("ptc%d" % i, [128, D], BF16) for i in range(2)]
                    pm = [fw.ps("pm%d" % i, [128, 512], F32) for i in range(3)]
                    pf = [fw.ps("pf%d" % i, [128, 512], F32) for i in range(2)]
                    pr = fw.ps("pr", [128, 16], F32)
                    pmc = [0]
                    for qt in range(nqt):
                        which = 1 if qt >= 16 else 0
                        y_, g_, x_, xn_, s_, r_ = yt[qt % 2], gt_[qt % 2], xin[qt % 2], xn[qt % 2], s2[qt % 2], rr[qt % 2]
                        dma("sp", y_.t[:], Y[qt * 128:(qt + 1) * 128, :], [], [y_])
                        dma("sp", g_.t[:], GATES[qt * 128:(qt + 1) * 128, :], [], [g_])
                        dma("sp", x_.t[:], local_src(xslot(qt)), [], [x_])
                        act(sg.t[:], g_.t[:], AF.Sigmoid, [g_], [sg])
                        for hf in range(2):
                            p_ = ptc[hf]
                            for i in range(8):
                                tr(p_.t[:, i * 128:(i + 1) * 128], y_.t[:, (hf * 8 + i) * 128:(hf * 8 + i + 1) * 128], identb.t[:], [y_, identb], [p_])
                            cp("dve" if hf else "act", YT.t[:, hf * 8:(hf + 1) * 8, :].rearrange("p a b -> p (a b)"), p_.t[:], [p_], [YT])
                        for n in range(4):
                            for half in range(2):
                                p_ = pm[pmc[0] % 3]
                                pmc[0] += 1
                                for k in range(4):
                                    mm(p_.t[:], YT.t[:, n * 4 + k, :], wbr.t[:, n * 4 + k, half * 512:(half + 1) * 512], k == 0, k == 3, [YT, wbr], [p_])
                                sgs = sg.t[:, n * 1024 + half * 512:n * 1024 + (half + 1) * 512]
                                if n == 0:
                                    tt("dve", macc.t[:, half * 512:(half + 1) * 512], p_.t[:], sgs, ALU.mult, [p_, sg], [macc])
                                else:
                                    tt("dve", mtmp.t[:], p_.t[:], sgs, ALU.mult, [p_, sg], [mtmp])
                                    tt("pool", macc.t[:, half * 512:(half + 1) * 512], macc.t[:, half * 512:(half + 1) * 512], mtmp.t[:], ALU.add, [macc, mtmp], [macc])
                        cp("act", mbf.t[:], macc.t[:], [macc], [mbf])
                        p_ = ptc[0]
                        for i in range(8):
                            tr(p_.t[:, i * 128:(i + 1) * 128], mbf.t[:, i * 128:(i + 1) * 128], identb.t[:], [mbf, identb], [p_])
                        cp("dve", mT.t[:].rearrange("p a b -> p (a b)"), p_.t[:], [p_], [mT])
                        for half in range(2):
                            p_ = pm[pmc[0] % 3]
                            pmc[0] += 1
                            for j in range(8):
                                mm(p_.t[:], mT.t[:, j, :], wo.t[:, j, half * 512:(half + 1) * 512], j == 0, j == 7, [mT, wo], [p_])
                            hs_ = slice(half * 512, (half + 1) * 512)
                            tt("dve", mtmp.t[:], p_.t[:], G1[which].t[:, hs_], ALU.mult, [p_, G1[which]], [mtmp])
                            tt("pool", xn_.t[:, hs_], mtmp.t[:], x_.t[:, hs_], ALU.add, [mtmp, x_], [xn_])
                        dma("sp", XN[qt * 128:(qt + 1) * 128, :], xn_.t[:], [xn_], [])
                        act(junk2.t[:], xn_.t[:], AF.Square, [xn_], [junk2, s_], accum_out=s_.t[:, 0:1])
                        act(s_.t[:, 1:2], s_.t[:, 0:1], AF.Sqrt, [s_], [s_], bias=EPS, scale=1.0 / D)
                        op("dve", lambda e: e.reciprocal(s_.t[:, 2:3], s_.t[:, 1:2]), [s_], [s_])
                        stt("dve", h2.t[:], xn_.t[:], s_.t[:, 2:3], A2[which].t[:], ALU.mult, ALU.mult, [xn_, s_, A2[which]], [h2])
                        tt("pool", h2.t[:], h2.t[:], SH2[which].t[:], ALU.add, [h2, SH2[which]], [h2])
                        for hf in range(2):
                            p_ = pf[hf]
                            for i in range(4):
                                j = hf * 4 + i
                                tr(p_.t[:, i * 128:(i + 1) * 128], h2.t[:, j * 128:(j + 1) * 128], identf.t[:], [h2, identf], [p_])
                            cp("dve" if hf else "act", h2tf.t[:, hf * 4:(hf + 1) * 4, :].rearrange("p a b -> p (a b)"), p_.t[:], [p_], [h2tf])
                        cp("pool", H2T.t[:, :, qt * 128:(qt + 1) * 128], h2tf.t[:], [h2tf], [H2T])
                        for j in range(8):
                            mm(pr.t[:], h2tf.t[:, j, :], rw.t[:, j, :], j == 0, j == 7, [h2tf, rw], [pr])
                        R = lambda a, b: r_.t[:, a:b]
                        v4 = lambda ap: ap.rearrange("p (g e) -> p g e", g=4)
                        act(R(0, 16), pr.t[:], AF.Sigmoid, [pr], [r_])
                        tt("dve", R(16, 32), R(0, 16), gv(V_RB, 16), ALU.add, [r_, VEC], [r_])
                        op("dve", lambda e: e.tensor_reduce(R(32, 36), v4(R(16, 32)), AX.X, ALU.max), [r_], [r_])
                        tt("dve", v4(R(48, 64)), v4(R(16, 32)), R(32, 36).unsqueeze(2).to_broadcast([128, 4, 4]), ALU.is_equal, [r_], [r_])
                        stt("dve", R(64, 80), R(48, 64), -1e9, R(16, 32), ALU.mult, ALU.add, [r_], [r_])
                        op("dve", lambda e: e.tensor_reduce(R(36, 40), v4(R(64, 80)), AX.X, ALU.max), [r_], [r_])
                        tt("dve", R(40, 44), R(32, 36), R(36, 40), ALU.add, [r_], [r_])
                        op("dve", lambda e: e.tensor_reduce(R(44, 45), R(40, 44), AX.X, ALU.max), [r_], [r_])
                        ts("dve", R(80, 84), R(40, 44), R(44, 45), None, ALU.is_equal, None, [r_], [r_])
                        tt("dve", v4(R(96, 112)), v4(R(16, 32)), R(36, 40).unsqueeze(2).to_broadcast([128, 4, 4]), ALU.is_ge, [r_], [r_])
                        tt("dve", v4(R(96, 112)), v4(R(96, 112)), R(80, 84).unsqueeze(2).to_broadcast([128, 4, 4]), ALU.mult, [r_], [r_])
                        tt("dve", R(112, 128), R(96, 112), R(0, 16), ALU.mult, [r_], [r_])
                        op("dve", lambda e: e.tensor_reduce(R(128, 129), R(112, 128), AX.X, ALU.add), [r_], [r_])
                        op("dve", lambda e: e.reciprocal(R(129, 130), R(128, 129)), [r_], [r_])
                        ts("dve", WT.t[:, qt, :], R(112, 128), R(129, 130), None, ALU.mult, None, [r_], [WT])
                    if False:
                        dbg_wt = dscr("dbg_wt", [128, NQT * 16], F32)
                        dma("sp", dbg_wt, WT.t[:].rearrange("p a b -> p (a b)"), [WT], [])
                with fw.scope():
                    Ft = [fw.sb("F%d" % i, [128, D], F32) for i in range(nqt)]
                    for f_ in Ft:
                        op("pool", lambda e: e.memset(f_.t[:], 0.0), [], [f_])
                    w1 = [fw.sb("w1_%d" % i, [128, 8, 512], BF16) for i in range(2)]
                    w3 = [fw.sb("w3_%d" % i, [128, 8, 512], BF16) for i in range(2)]
                    w2 = [fw.sb("w2_%d" % i, [128, 4, D], BF16) for i in range(2)]
                    GT = [fw.sb("GT%d" % i, [128, 4, 512], BF16) for i in range(2)]
                    sl = [fw.sb("sl%d" % i, [128, 512], F32) for i in range(2)]
                    pa = [fw.ps("pa%d" % i, [128, 512], F32) for i in range(2)]
                    pb = [fw.ps("pb%d" % i, [128, 512], F32) for i in range(2)]
                    py = [fw.ps("py%d" % i, [128, 512], F32) for i in range(3)]
                    blocks = [(i * 512, 512) for i in range(4)] + ([(2048, 256)] if want_ctx else [])
                    c1, c2 = [0], [0]
                    for e_ in range(CFG.get('nexp', 16)):
                        a1, a3, a2 = w1[e_ % 2], w3[e_ % 2], w2[e_ % 2]
                        dma("pool", a1.t[:], moe_w1[e_].rearrange("(j p) c -> p j c", p=128), [], [a1])
                        dma("pool", a3.t[:], moe_w3[e_].rearrange("(j p) c -> p j c", p=128), [], [a3])
                        dma("pool", a2.t[:], moe_w2[e_].rearrange("(j p) c -> p j c", p=128), [], [a2])
                        for bi_, (t0, nb) in enumerate(blocks):
                            G_ = GT[(e_ * 5 + bi_) % 2]
                            for m in range(4):
                                pa_, pb_, sl_ = pa[c1[0] % 2], pb[c1[0] % 2], sl[c1[0] % 2]
                                c1[0] += 1
                                for k in range(8):
                                    mm(pa_.t[:, :nb], a1.t[:, k, m * 128:(m + 1) * 128], H2T.t[:, k, t0:t0 + nb], k == 0, k == 7, [a1, H2T], [pa_])
                                for k in range(8):
                                    mm(pb_.t[:, :nb], a3.t[:, k, m * 128:(m + 1) * 128], H2T.t[:, k, t0:t0 + nb], k == 0, k == 7, [a3, H2T], [pb_])
                                act(sl_.t[:, :nb], pa_.t[:, :nb], AF.Silu, [pa_], [sl_])
                                tt("dve", G_.t[:, m, :nb], sl_.t[:, :nb], pb_.t[:, :nb], ALU.mult, [sl_, pb_], [G_])
                            for j in range(nb // 128):
                                qt = t0 // 128 + j
                                for half in range(2):
                                    py_ = py[c2[0] % 3]
                                    c2[0] += 1
                                    for m in range(4):
                                        mm(py_.t[:], G_.t[:, m, j * 128:(j + 1) * 128], a2.t[:, m, half * 512:(half + 1) * 512], m == 0, m == 3, [G_, a2], [py_])
                                    fs = Ft[qt].t[:, half * 512:(half + 1) * 512]
                                    stt("dve", fs, py_.t[:], WT.t[:, qt, e_:e_ + 1], fs, ALU.mult, ALU.add, [py_, WT, Ft[qt]], [Ft[qt]])
                    G2 = [fw.sb("G2_%d" % w, [128, D], F32) for w in range(2)]
                    for w in range(2):
                        load_mod(G2[w], w, M_G2)
                    xo = [fw.sb("xo%d" % i, [128, D], F32) for i in range(2)]
                    for qt in range(nqt):
                        which = 1 if qt >= 16 else 0
                        x_ = xo[qt % 2]
                        dma("sp", x_.t[:], XN[qt * 128:(qt + 1) * 128, :], [], [x_])
                        tt("dve", Ft[qt].t[:], Ft[qt].t[:], G2[which].t[:], ALU.mult, [Ft[qt], G2[which]], [Ft[qt]])
                        tt("pool", x_.t[:], x_.t[:], Ft[qt].t[:], ALU.add, [x_, Ft[qt]], [x_])
                        if lidx == 1:
                            dma("sp", x_out[qt * 128:(qt + 1) * 128, :], x_.t[:], [x_], [], out=True)
                        elif qt < 16:
                            dma("sp", X1[(seg * 16 + qt) * 128:(seg * 16 + qt + 1) * 128, :], x_.t[:], [x_], [])
                        else:
                            dma("sp", XC1[(qt - 16) * 128:(qt - 15) * 128, :], x_.t[:], [x_], [])

        for lidx in layers:
            for k_, seg in enumerate(segs0 if lidx == 0 else (0,)):
                layer_pass(lidx, seg, lidx == 0 and seg == 0, k_ == 0)
        fw.finish()
    return nc


def _rope_tables():
    t = np.arange(SEQ)
    rows = (t // 64).astype(np.float32)
    cols = (t % 64).astype(np.float32)
    out = []
    for dim in (64, 32):
        quarter = dim // 4
        inv = np.exp(-np.log(10000.0) * np.arange(quarter, dtype=np.float32) / quarter).astype(np.float32)
        ang = np.concatenate([rows[:, None] * inv, cols[:, None] * inv], axis=-1).astype(np.float32)
        out += [np.cos(ang).astype(np.float32), np.sin(ang).astype(np.float32)]
    tab = np.concatenate(out, axis=1)
    ident = np.zeros((1, 96), np.float32)
    ident[0, 0:32] = 1.0
    ident[0, 64:80] = 1.0
    return tab, ident


def _nat_consts(s):
    col = np.arange(64)
    cs = np.clip(col - 8, 0, 48)
    col_ok = (col[None, :] >= cs[:, None]) & (col[None, :] < cs[:, None] + 16)
    reps = (0, 1, 7, 14, 15)
    V = np.zeros((5, 7, 128, 128), np.float32)
    for ci, tl in enumerate(reps):
        t = 16 * s + tl
        for c in range(-3, 4):
            u = t + c
            if not (0 <= u < 64):
                continue
            for kr in range(2):
                for qr in range(2):
                    krow, qrow = 2 * u + kr, 2 * t + qr
                    st = min(max(qrow - 4, 0), 120)
                    if st <= krow < st + 8:
                        V[ci, c + 3, kr * 64:(kr + 1) * 64, qr * 64:(qr + 1) * 64] = col_ok.T
    return V


def _nat_bias_index():
    k = np.arange(128)
    q = np.arange(128)
    kr, kc = k // 64, k % 64
    qr, qc = q // 64, q % 64
    dcol = np.clip(kc[:, None] - qc[None, :] + 15, 0, 30)
    idx_r = np.zeros((7, 128, 128), np.int64)
    for c in range(-3, 4):
        idx_r[c + 3] = np.clip(2 * c + kr[:, None] - qr[None, :] + 7, 0, 14)
    return idx_r, np.broadcast_to(dcol, (7, 128, 128))


def prep_fused(inp):
    f32 = lambda a: np.ascontiguousarray(a, dtype=np.float32)
    tab, rid = _rope_tables()
    x = np.asarray(inp['x'], dtype=np.float32)
    xc = np.asarray(inp['ctx'], dtype=np.float32)
    idx_r, idx_c = _nat_bias_index()
    tri_prev = (np.arange(128)[:, None] >= np.arange(128)[None, :]).astype(np.float32)
    tri_next = (np.arange(128)[:, None] <= np.arange(128)[None, :]).astype(np.float32)
    common = dict(ident=np.eye(128, dtype=np.float32), router_w=f32(inp['router_w']))
    for l in range(2):
        vecs = [inp['win_q_norm'][l], inp['win_k_norm'][l], inp['dif_q_norm'][l], inp['dif_k_norm'][l],
                inp['nat_q_norm'][l], inp['nat_k_norm'][l], inp['mla_q_norm'][l], inp['mla_k_norm'][l],
                inp['mla_q_a_norm'][l], inp['mla_kv_a_norm'][l], inp['dif_subln'][l], inp['win_sink'][l],
                inp['dif_lambda'][l].reshape(-1), inp['router_b'], inp['ada_b'][l], inp['norm1_g'][l], inp['norm2_g'][l]]
        vec = f32(np.concatenate([np.asarray(v).reshape(-1) for v in vecs])[None, :])
        assert vec.shape[1] == NVEC
        rpb = np.asarray(inp['nat_rpb'][l])
        common.update({
            'vec_%d' % l: vec, 'ada_w_%d' % l: f32(inp['ada_w'][l]), 'w_in_%d' % l: f32(inp['w_in'][l]),
            'wq_b_%d' % l: f32(inp['mla_wq_b'][l]), 'wkv_b_%d' % l: f32(inp['mla_wkv_b'][l]),
            'w_branch_%d' % l: f32(np.asarray(inp['w_branch'][l]).reshape(2048, D)), 'w_out_%d' % l: f32(inp['w_out'][l]),
            'moe_w1_%d' % l: f32(inp['moe_w1'][l]), 'moe_w3_%d' % l: f32(inp['moe_w3'][l]), 'moe_w2_%d' % l: f32(inp['moe_w2'][l]),
            'natb_%d' % l: f32(np.transpose(rpb[:, idx_r, idx_c], (1, 2, 0, 3)))})
    maps = []
    zeros_h = np.zeros((384, D), np.float32)
    rid384 = np.repeat(rid, 384, 0)
    rc = np.repeat(rid, NCTX, 0)
    for core in range(8):
        b, s = core // 4, core % 4
        sigma = [s] + [g for g in range(4) if g != s]
        m = dict(common)
        m['xall'] = f32(np.concatenate([xc[b]] + [x[b, 2048 * g:2048 * (g + 1)] for g in sigma], 0))
        m['rope_all'] = f32(np.concatenate([rc] + [tab[2048 * g:2048 * (g + 1)] for g in sigma], 0))
        for j, g in enumerate(sigma):
            o0 = 2048 * g
            hb = x[b, o0 - 384:o0] if g > 0 else zeros_h
            ha = x[b, o0 + 2048:o0 + 2048 + 384] if g < 3 else zeros_h
            rb = tab[o0 - 384:o0] if g > 0 else rid384
            ra = tab[o0 + 2048:o0 + 2048 + 384] if g < 3 else rid384
            m['xloc%d' % j] = f32(np.concatenate([hb, x[b, o0:o0 + 2048], ha, xc[b]], 0))
            m['rope_loc%d' % j] = f32(np.concatenate([rb, tab[o0:o0 + 2048], ra, rc], 0))
            m['wmask%d' % j] = f32(np.stack([tri_prev if g > 0 else np.zeros_like(tri_prev), tri_prev, tri_next,
                                             tri_next if g < 3 else np.zeros_like(tri_next)], 0))
            m['natv%d' % j] = f32(_nat_consts(g))
        hs = np.zeros((128, 8), np.float32)
        for r in (1, 2, 3):
            if sigma[r] == s - 1:
                hs[:, r] = 1.0
            if sigma[r] == s + 1:
                hs[:, 4 + r] = 1.0
        m['hsel'] = hs
        m['cvec'] = f32(np.stack([inp['c'][b], inp['c_ctx']], 0).reshape(2, 8, 128).transpose(2, 0, 1).reshape(128, 16))
        maps.append(m)
    return maps


def kernel(**inputs):
    inp = {k: np.asarray(v) for k, v in inputs.items()}
    nc = build_fused()
    maps = prep_fused(inp)
    res = run_bass_kernel_spmd(nc, maps, core_ids=list(range(8)))
    out = np.empty((2, SEQ, D), np.float32)
    for core in range(8):
        b, s = core // 4, core % 4
        out[b, 2048 * s:2048 * (s + 1)] = np.asarray(res.results[core]['x_out'])
    return out
```

```python
import numpy as np
import ml_dtypes
import concourse.bass as bass
import concourse.mybir as mybir
from concourse.bass_utils import run_bass_kernel_spmd

F32 = mybir.dt.float32
BF16 = mybir.dt.bfloat16
I32 = mybir.dt.int32
AF = mybir.ActivationFunctionType
ALU = mybir.AluOpType
AX = mybir.AxisListType


class Buf:
    __slots__ = ("t", "w", "r", "name")

    def __init__(self, t, name=""):
        self.t = t
        self.w = None
        self.r = {}
        self.name = name


class _Eng:
    def __init__(self, name, eng, sem):
        self.name, self.eng, self.sem = name, eng, sem
        self.count = 0
        self.seen = {}
        self.dq = []
        self.dn = 0


class FW:
    NDMA = 6

    def __init__(self, nc):
        self.nc = nc
        self.stack = None
        self.E = {}
        self.out_tokens = []

    def __enter__(self):
        from contextlib import ExitStack
        self.stack = ExitStack()
        self.stack.__enter__()
        nc = self.nc
        for name, eng in (("pe", nc.tensor), ("act", nc.scalar), ("dve", nc.vector),
                          ("pool", nc.gpsimd), ("sp", nc.sync)):
            sem = self.stack.enter_context(nc.semaphore("s_" + name))
            self.E[name] = _Eng(name, eng, sem)
        for q in ("sp", "act", "pool"):
            for i in range(self.NDMA):
                self.E[q].dq.append(self.stack.enter_context(nc.semaphore("d_%s%d" % (q, i))))
        return self

    def __exit__(self, *a):
        return self.stack.__exit__(*a)

    nalloc = 0

    def sb(self, name, shape, dt):
        self.nalloc += 1
        name = "%s_u%d" % (name, self.nalloc)
        return Buf(self.stack.enter_context(self.nc.sbuf_tensor(name, list(shape), dt)), name)

    def ps(self, name, shape, dt=F32):
        self.nalloc += 1
        name = "%s_u%d" % (name, self.nalloc)
        return Buf(self.stack.enter_context(self.nc.psum_tensor(name, list(shape), dt)), name)

    def scope(self):
        fw = self

        class _S:
            def __enter__(s):
                from contextlib import ExitStack
                s.prev = fw.stack
                fw.stack = ExitStack()
                fw.stack.__enter__()
                return s

            def __exit__(s, *a):
                fw.barrier()
                r = fw.stack.__exit__(*a)
                fw.stack = s.prev
                return r
        return _S()

    def _wait(self, E, tok):
        sem, val = tok
        if E.name == "pe" and sem is E.sem:
            return
        key = id(sem)
        if E.seen.get(key, 0) >= val:
            return
        E.eng.wait_ge(sem, val)
        E.seen[key] = val

    def _deps(self, E, reads, writes):
        toks = []
        for b in reads:
            if b.w is not None:
                toks.append(b.w)
        for b in writes:
            if b.w is not None:
                toks.append(b.w)
            toks.extend(b.r.values())
        for tok in toks:
            self._wait(E, tok)

    def _mark(self, tok, reads, writes):
        key = id(tok[0])
        for b in reads:
            old = b.r.get(key)
            if old is None or old[1] < tok[1]:
                b.r[key] = tok
        for b in writes:
            b.w = tok
            b.r = {}

    nops = 0
    maxops = 10 ** 9
    rec = None

    def op(self, en, fn, reads=(), writes=()):
        if self.rec is not None:
            self.rec.append((0, en, fn, tuple(reads), tuple(writes)))
            return
        self.nops += 1
        if self.nops > self.maxops:
            return
        E = self.E[en]
        self._deps(E, reads, writes)
        ins = fn(E.eng)
        E.count += 1
        ins.then_inc(E.sem, 1)
        self._mark((E.sem, E.count), reads, writes)

    def dma(self, q, out_ap, in_ap, reads=(), writes=(), out=False):
        if self.rec is not None:
            self.rec.append((1, q, out_ap, in_ap, tuple(reads), tuple(writes), out))
            return
        self.nops += 1
        if self.nops > self.maxops:
            return
        E = self.E[q]
        self._deps(E, reads, writes)
        k = E.dn % self.NDMA
        rnd = E.dn // self.NDMA
        sem = E.dq[k]
        if rnd > 0:
            self._wait(E, (sem, 16 * rnd))
        E.eng.dma_start(out=out_ap, in_=in_ap).then_inc(sem, 16)
        E.dn += 1
        tok = (sem, 16 * (rnd + 1))
        self._mark(tok, reads, writes)
        if out:
            self.out_tokens.append(tok)
        return tok

    def emit_interleaved(self, streams):
        n = max(len(st) for st in streams)
        for i in range(n):
            for st in streams:
                if i < len(st):
                    r = st[i]
                    if r[0] == 0:
                        self.op(r[1], r[2], r[3], r[4])
                    else:
                        self.dma(r[1], r[2], r[3], r[4], r[5], r[6])

    def barrier(self):
        toks = []
        for E in self.E.values():
            if E.count:
                toks.append((E.sem, E.count))
            for k, sem in enumerate(E.dq):
                n = (E.dn - k + self.NDMA - 1) // self.NDMA
                if n > 0:
                    toks.append((sem, 16 * n))
        for E in self.E.values():
            for tok in toks:
                sem, val = tok
                key = id(sem)
                if E.seen.get(key, 0) >= val:
                    continue
                E.eng.wait_ge(sem, val)
                E.seen[key] = val

    def finish(self):
        self.barrier()


D = 1024
SEQ = 8192
NCTX = 256
NT_ALL = 66
NT_LOC = 24
NQT = 18
EPS = 1e-6
C_WQ, C_WK, C_WV, C_DQ, C_DK, C_DV, C_NQ, C_NK, C_NV, C_MQA, C_MKVA, C_G = (
    0, 512, 640, 768, 1280, 1792, 2304, 2816, 3328, 3840, 4096, 4256)
V_WQN, V_WKN, V_DQN, V_DKN, V_NQN, V_NKN = 0, 64, 128, 192, 256, 320
V_MQN, V_MKN, V_MQAN, V_MKVAN, V_SUBLN, V_SINK, V_LAM, V_RB = 384, 480, 576, 832, 960, 1088, 1096, 1352
V_ADAB, V_N1, V_N2 = 1368, 1368 + 6144, 1368 + 6144 + 1024
NVEC = V_N2 + 1024
NSMALL = 1368


def bcast_rows(ap2d, row, c0, n, parts=128):
    t = ap2d.tensor
    cols = ap2d.shape[1]
    return bass.AP(t, ap2d.offset + row * cols + c0, [[0, parts], [1, n]])


CFG = {'phases': 'all', 'nA1': NT_ALL, 'nA2': NT_LOC}


def build_fused(debug=False, layers=(0, 1), segs0=(0, 1, 2, 3)):
    nc = bass.Bass("TRN2", target_bir_lowering=False)

    def din(name, shape, dt=F32):
        return nc.dram_tensor(name, list(shape), dt, kind="ExternalInput").ap()

    def dscr(name, shape, dt=BF16, out=False):
        kind = "ExternalOutput" if (out or debug) else "Internal"
        return nc.dram_tensor(name, list(shape), dt, kind=kind).ap()

    xall = din("xall", [NT_ALL * 128, D])
    rope_all = din("rope_all", [NT_ALL * 128, 96])
    xloc_s = [din("xloc%d" % j, [NT_LOC * 128, D]) for j in range(4)]
    rope_loc_s = [din("rope_loc%d" % j, [NT_LOC * 128, 96]) for j in range(4)]
    wmask_s = [din("wmask%d" % j, [4, 128, 128]) for j in range(4)]
    natv_s = [din("natv%d" % j, [5, 7, 128, 128]) for j in range(4)]
    hsel_in = din("hsel", [128, 8])
    cvec = din("cvec", [128, 16])
    ident_in = din("ident", [128, 128])
    router_w = din("router_w", [D, 16])
    LW = []
    for l in range(2):
        LW.append(dict(
            vec=din("vec_%d" % l, [1, NVEC]), ada_w=din("ada_w_%d" % l, [D, 6 * D]), w_in=din("w_in_%d" % l, [D, 8352]),
            wq_b=din("wq_b_%d" % l, [256, 768]), wkv_b=din("wkv_b_%d" % l, [128, 1024]),
            w_branch=din("w_branch_%d" % l, [2048, D]), w_out=din("w_out_%d" % l, [D, D]),
            moe_w1=din("moe_w1_%d" % l, [16, D, 512]), moe_w3=din("moe_w3_%d" % l, [16, D, 512]),
            moe_w2=din("moe_w2_%d" % l, [16, 512, D]), natb=din("natb_%d" % l, [7, 128, 8, 128])))

    x_out = dscr("x_out", [2048, D], F32, out=True)
    X1 = dscr("X1", [SEQ, D], F32)
    XC1 = dscr("XC1", [NCTX, D], F32)

    KD = dscr("KD", [512, NT_ALL * 128])
    VD = dscr("VD", [NT_ALL * 128, 512])
    KM = dscr("KM", [8, 96, NT_ALL * 128])
    VM = dscr("VM", [NT_ALL * 128, 512])
    KW = dscr("KW", [128, NT_LOC * 128])
    VW = dscr("VW", [NT_LOC * 128, 128])
    KN = dscr("KN", [512, NT_LOC * 128])
    VN = dscr("VN", [NT_LOC * 128, 512])
    QW = dscr("QW", [512, NQT * 128])
    QD = dscr("QD", [512, NQT * 128])
    QN = dscr("QN", [512, NQT * 128])
    QM = dscr("QM", [8, 96, NQT * 128])
    GATES = dscr("GATES", [NQT * 128, 4096])
    Y = dscr("Y", [NQT * 128, 2048])
    XN = dscr("XN", [NQT * 128, D], F32)
    MODS = dscr("MODS", [2, 128, 6 * D], F32)
    HTS = dscr("HTS", [NQT, 128, D])

    fw = FW(nc)
    with fw:
        op, dma = fw.op, fw.dma

        def mm(out, lhsT, rhs, start, stop, reads, writes):
            op("pe", lambda e: e.matmul(out, lhsT, rhs, start=start, stop=stop), reads, writes)

        def tr(out, in_, idn, reads, writes):
            op("pe", lambda e: e.transpose(out, in_, idn), reads, writes)

        def tt(en, out, a, b, alu, reads, writes):
            op(en, lambda e: e.tensor_tensor(out, a, b, alu), reads, writes)

        def ts(en, out, a, s1, s2, o0, o1, reads, writes):
            if s2 is None:
                op(en, lambda e: e.tensor_scalar(out, a, s1, None, o0), reads, writes)
            else:
                op(en, lambda e: e.tensor_scalar(out, a, s1, s2, o0, o1), reads, writes)

        def stt(en, out, a, s, b, o0, o1, reads, writes):
            op(en, lambda e: e.scalar_tensor_tensor(out, a, s, b, o0, o1), reads, writes)

        def act(out, in_, func, reads, writes, **kw):
            op("act", lambda e: e.activation(out, in_, func, **kw), reads, writes)

        def cp(en, out, in_, reads, writes):
            if en == "act":
                act(out, in_, AF.Copy, reads, writes)
            else:
                op(en, lambda e: e.tensor_copy(out, in_), reads, writes)

        identb = fw.sb("identb", [128, 128], BF16)
        identf = fw.sb("identf", [128, 128], F32)
        VEC = fw.sb("VEC", [128, NSMALL], F32)
        dma("pool", identb.t[:], ident_in, [], [identb])
        dma("sp", identf.t[:], ident_in, [], [identf])
        gv = lambda off, n: VEC.t[:, off:off + n]

        HSEL = fw.sb("HSEL", [128, 8], F32)
        dma("sp", HSEL.t[:], hsel_in, [], [HSEL])

        def layer_pass(lidx, seg, want_ctx, first):
            lam_init = 0.8 - 0.6 * float(np.exp(-0.3 * lidx))
            W_ = LW[lidx]
            vec, ada_w, w_in, wq_b, wkv_b = W_["vec"], W_["ada_w"], W_["w_in"], W_["wq_b"], W_["wkv_b"]
            w_branch, w_out, moe_w1, moe_w3, moe_w2, natb = W_["w_branch"], W_["w_out"], W_["moe_w1"], W_["moe_w3"], W_["moe_w2"], W_["natb"]
            rope_loc, wmask, natv = rope_loc_s[seg], wmask_s[seg], natv_s[seg]
            if first:
                dma("sp", VEC.t[:], bcast_rows(vec, 0, 0, NSMALL), [], [VEC])

            def dense_src(i):
                if lidx == 0:
                    return xall[i * 128:(i + 1) * 128, :]
                return XC1[i * 128:(i + 1) * 128, :] if i < 2 else X1[(i - 2) * 128:(i - 1) * 128, :]

            def local_src(i):
                if lidx == 0:
                    return xloc_s[seg][i * 128:(i + 1) * 128, :]
                if 3 <= i < 19:
                    return X1[(i - 3) * 128:(i - 2) * 128, :]
                if i >= 22:
                    return XC1[(i - 22) * 128:(i - 21) * 128, :]
                if i < 3:
                    return [(X1[(r * 16 + 13 + i) * 128:(r * 16 + 14 + i) * 128, :], HSEL.t[:, r:r + 1]) for r in (1, 2, 3)]
                return [(X1[(r * 16 + i - 19) * 128:(r * 16 + i - 18) * 128, :], HSEL.t[:, 4 + r:5 + r]) for r in (1, 2, 3)]

            if first:
              with fw.scope():
                cs = fw.sb("cs", [128, 2, 8], F32)
                CB = fw.sb("CB", [128, 2, 8, 128], F32)
                NG = fw.sb("NG", [128, 2, D], F32)
                dma("sp", cs.t[:].rearrange("p w j -> p (w j)"), cvec, [], [cs])
                dma("sp", NG.t[:, 0, :], bcast_rows(vec, 0, V_N1, D), [], [NG])
                dma("sp", NG.t[:, 1, :], bcast_rows(vec, 0, V_N2, D), [], [NG])
                act(cs.t[:], cs.t[:], AF.Silu, [cs], [cs])
                cp("dve", CB.t[:], cs.t[:].unsqueeze(3).to_broadcast([128, 2, 8, 128]), [cs], [CB])
                aw = [fw.sb("aw%d" % i, [128, 8, 512], F32) for i in range(2)]
                ab = [fw.sb("ab%d" % i, [128, 512], F32) for i in range(2)]
                mps = [fw.ps("mps%d" % i, [128, 512], F32) for i in range(2)]
                mo = [fw.sb("mo%d" % i, [128, 512], F32) for i in range(2)]
                n = 0
                for blk in range(12):
                    a_, b_ = aw[blk % 2], ab[blk % 2]
                    dma("sp", a_.t[:], ada_w[:, blk * 512:(blk + 1) * 512].rearrange("(j p) c -> p j c", p=128), [], [a_])
                    dma("sp", b_.t[:], bcast_rows(vec, 0, V_ADAB + blk * 512, 512), [], [b_])
                    chunk = blk // 2
                    half = blk % 2
                    for w in range(2):
                        p_, o_ = mps[n % 2], mo[n % 2]
                        n += 1
                        for j in range(8):
                            mm(p_.t[:], CB.t[:, w, j, :], a_.t[:, j, :], j == 0, j == 7, [CB, a_], [p_])
                        tt("dve", o_.t[:], p_.t[:], b_.t[:], ALU.add, [p_, b_], [o_])
                        if chunk in (1, 4):
                            g = NG.t[:, 0 if chunk == 1 else 1, half * 512:(half + 1) * 512]
                            stt("dve", o_.t[:], o_.t[:], 1.0, g, ALU.add, ALU.mult, [o_, NG], [o_])
                        dma("sp", MODS[w, :, blk * 512:(blk + 1) * 512], o_.t[:], [o_], [])
            M_SH1, M_A1, M_G1, M_SH2, M_A2, M_G2 = [i * D for i in range(6)]

            def load_mod(buf, which, off):
                dma("sp", buf.t[:], MODS[which, :, off:off + D], [], [buf])

            with fw.scope():
                A1 = [fw.sb("A1_%d" % w, [128, D], F32) for w in range(2)]
                SH1 = [fw.sb("SH1_%d" % w, [128, D], F32) for w in range(2)]
                for w in range(2):
                    load_mod(A1[w], w, M_A1)
                    load_mod(SH1[w], w, M_SH1)
                wkvb = fw.sb("wkvb", [128, 1024], BF16)
                wqb = fw.sb("wqb", [128, 2, 768], BF16)
                dma("pool", wkvb.t[:], wkv_b, [], [wkvb])
                dma("pool", wqb.t[:], wq_b.rearrange("(j p) c -> p j c", p=128), [], [wqb])
                junk = fw.sb("junk", [128, D], F32)

                class _Set:
                    pass

                def mkset(k):
                    S = _Set()
                    S.xt = fw.sb("xt", [128, D], F32)
                    S.rp = fw.sb("rp", [128, 96], F32)
                    S.st = fw.sb("st", [128, 16], F32)
                    S.hb = fw.sb("hb", [128, D], BF16)
                    S.hT = fw.sb("hT", [128, D], BF16)
                    S.ptr = fw.ps("ptr", [128, D], BF16)
                    S.pp = [fw.ps("pp", [128, 512], F32) for _ in range(3)]
                    S.xs = fw.sb("xs", [128, 1024], F32)
                    S.sq = fw.sb("sq", [128, 1024], F32)
                    S.ss = fw.sb("ss", [128, 32], F32)
                    S.yb = fw.sb("yb", [128, 1024], F32)
                    S.rt = fw.sb("rt", [128, 4, 8 * 48], F32)
                    S.ob = [fw.sb("ob", [128, 1024], BF16) for _ in range(2)]
                    S.oT = [fw.sb("oT", [128, 1024], BF16) for _ in range(2)]
                    S.mkt = fw.sb("mkt", [128, 8, 96], F32)
                    S.cnt = {"g": 0, "o": 0, "t": 0, "p": 0}
                    return S
                sets = [mkset(k) for k in range(2)]

                def hnr(S, src_aps, src_bufs, H, Dh, gain, rope):
                    g = S.cnt["g"]
                    S.cnt["g"] += 1
                    N = H * Dh
                    x_, s_, y_, r_, sq = S.xs, S.ss, S.yb, S.rt, S.sq
                    so = (g % 2) * 16
                    o_ = S.ob[S.cnt["o"] % 2]
                    S.cnt["o"] += 1
                    v3 = lambda ap: ap.rearrange("p (h d) -> p h d", h=H)
                    for (sap, c0_, nc_) in src_aps:
                        cp("act", x_.t[:, c0_:c0_ + nc_], sap, src_bufs, [x_])
                    tt("pool", sq.t[:, :N], x_.t[:, :N], x_.t[:, :N], ALU.mult, [x_], [sq])
                    op("dve", lambda e: e.tensor_reduce(s_.t[:, so:so + H], v3(sq.t[:, :N]), AX.X, ALU.add), [sq], [s_])
                    act(s_.t[:, so + 8:so + 8 + H], s_.t[:, so:so + H], AF.Sqrt, [s_], [s_], bias=EPS, scale=1.0 / Dh)
                    op("dve", lambda e: e.reciprocal(s_.t[:, so:so + H], s_.t[:, so + 8:so + 8 + H]), [s_], [s_])
                    tt("dve", v3(y_.t[:, :N]), v3(x_.t[:, :N]), s_.t[:, so:so + H].unsqueeze(2).to_broadcast([128, H, Dh]),
                       ALU.mult, [x_, s_], [y_])
                    gb = gain.unsqueeze(1).to_broadcast([128, H, Dh])
                    if rope is None:
                        tt("pool", v3(o_.t[:, :N]), v3(y_.t[:, :N]), gb, ALU.mult, [y_, VEC], [o_])
                        return o_
                    r0, n, cos_ap, sin_ap, rbuf = rope
                    hlf = n // 2
                    tt("pool", v3(y_.t[:, :N]), v3(y_.t[:, :N]), gb, ALU.mult, [y_, VEC], [y_])
                    y3 = v3(y_.t[:, :N])
                    o3 = v3(o_.t[:, :N])
                    x1, x2 = y3[:, :, r0:r0 + hlf], y3[:, :, r0 + hlf:r0 + n]
                    cb = cos_ap.unsqueeze(1).to_broadcast([128, H, hlf])
                    sb_ = sin_ap.unsqueeze(1).to_broadcast([128, H, hlf])
                    t = [r_.t[:, i, :H * hlf].rearrange("p (h d) -> p h d", h=H) for i in range(4)]
                    tt("dve", t[0], x1, cb, ALU.mult, [y_, rbuf], [r_])
                    tt("pool", t[1], x2, sb_, ALU.mult, [y_, rbuf], [r_])
                    tt("dve", t[2], x1, sb_, ALU.mult, [y_, rbuf], [r_])
                    tt("pool", t[3], x2, cb, ALU.mult, [y_, rbuf], [r_])
                    tt("dve", o3[:, :, r0:r0 + hlf], t[0], t[1], ALU.subtract, [r_], [o_])
                    tt("pool", o3[:, :, r0 + hlf:r0 + n], t[2], t[3], ALU.add, [r_], [o_])
                    if r0 > 0:
                        cp("act", o3[:, :, 0:r0], y3[:, :, 0:r0], [y_], [o_])
                    return o_

                def transp(S, o_, nblk, bw):
                    k = S.cnt["t"]
                    S.cnt["t"] += 1
                    p_, t_ = S.ptr, S.oT[k % 2]
                    for i in range(nblk):
                        tr(p_.t[0:bw, i * 128:(i + 1) * 128], o_.t[:, i * bw:(i + 1) * bw], identb.t[:], [o_, identb], [p_])
                    cp("dve" if k % 2 else "act", t_.t[0:bw, 0:nblk * 128], p_.t[0:bw, 0:nblk * 128], [p_], [t_])
                    return t_

                def transp_store(S, o_, nblk, bw, dst_ap):
                    t_ = transp(S, o_, nblk, bw)
                    dma("sp", dst_ap, t_.t[0:bw, 0:nblk * 128].rearrange("p (i t) -> p i t", i=nblk), [t_], [])

                def plain_store(S, src_ap, src_bufs, N, dst_ap):
                    o_ = S.ob[S.cnt["o"] % 2]
                    S.cnt["o"] += 1
                    cp("act", o_.t[:, :N], src_ap, src_bufs, [o_])
                    dma("sp", dst_ap, o_.t[:, :N], [o_], [])

                def make_hT(S, xsrc, rsrc, row0, which):
                    x_, r_, s_, hT_, h1 = S.xt, S.rp, S.st, S.hT, S.yb
                    if isinstance(xsrc, list):
                        for ci_, (cap, wap) in enumerate(xsrc):
                            dma("sp", S.sq.t[:], cap, [], [S.sq])
                            if ci_ == 0:
                                ts("dve", x_.t[:], S.sq.t[:], wap, None, ALU.mult, None, [S.sq, HSEL], [x_])
                            else:
                                stt("dve", x_.t[:], S.sq.t[:], wap, x_.t[:], ALU.mult, ALU.add, [S.sq, HSEL, x_], [x_])
                    else:
                        dma("sp", x_.t[:], xsrc, [], [x_])
                    dma("sp", r_.t[:], rsrc[row0:row0 + 128, :], [], [r_])
                    act(junk.t[:], x_.t[:], AF.Square, [x_], [junk, s_], accum_out=s_.t[:, 0:1])
                    act(s_.t[:, 1:2], s_.t[:, 0:1], AF.Sqrt, [s_], [s_], bias=EPS, scale=1.0 / D)
                    op("dve", lambda e: e.reciprocal(s_.t[:, 2:3], s_.t[:, 1:2]), [s_], [s_])
                    stt("dve", h1.t[:], x_.t[:], s_.t[:, 2:3], A1[which].t[:], ALU.mult, ALU.mult, [x_, s_, A1[which]], [h1])
                    tt("pool", S.hb.t[:], h1.t[:], SH1[which].t[:], ALU.add, [h1, SH1[which]], [S.hb])
                    p_ = S.ptr
                    for j in range(8):
                        tr(p_.t[:, j * 128:(j + 1) * 128], S.hb.t[:, j * 128:(j + 1) * 128], identb.t[:], [S.hb, identb], [p_])
                    cp("dve", hT_.t[:], p_.t[:], [p_], [hT_])
                    return hT_, r_

                def nextp(S):
                    S.cnt["p"] += 1
                    return S.pp[S.cnt["p"] % 3]

                def project(S, hT_, W, c0, ncol):
                    pbuf = nextp(S)
                    for j in range(8):
                        mm(pbuf.t[:, 0:ncol], hT_.t[:, j * 128:(j + 1) * 128], W.t[:, j, c0:c0 + ncol], j == 0, j == 7, [hT_, W], [pbuf])
                    return pbuf

                def run_interleaved(tile_fn, tiles):
                    for a in range(0, len(tiles), 2):
                        streams = []
                        for k, i in enumerate(tiles[a:a + 2]):
                            fw.rec = []
                            tile_fn(i, sets[k])
                            streams.append(fw.rec)
                            fw.rec = None
                        fw.emit_interleaved(streams)

                if first:
                  with fw.scope():
                    Wd = fw.sb("Wd", [128, 8, 1184], BF16)
                    dma("pool", Wd.t[:, :, 0:1024], w_in[:, C_DK:C_DK + 1024].rearrange("(j p) c -> p j c", p=128), [], [Wd])
                    dma("pool", Wd.t[:, :, 1024:1184], w_in[:, C_MKVA:C_MKVA + 160].rearrange("(j p) c -> p j c", p=128), [], [Wd])

                    def a1_tile(i, S):
                        which = 1 if i < 2 else 0
                        hT_, r_ = make_hT(S, dense_src(i), rope_all, i * 128, which)
                        t0 = i * 128
                        cosh, sinh, cosm, sinm = r_.t[:, 0:32], r_.t[:, 32:64], r_.t[:, 64:80], r_.t[:, 80:96]
                        mkt = S.mkt
                        p_ = project(S, hT_, Wd, 0, 512)
                        o_ = hnr(S, [(p_.t[:, 0:512], 0, 512)], [p_], 8, 64, gv(V_DKN, 64), (0, 64, cosh, sinh, r_))
                        transp_store(S, o_, 4, 128, KD.rearrange("(i q) t -> q i t", q=128)[:, :, t0:t0 + 128])
                        p_ = project(S, hT_, Wd, 512, 512)
                        plain_store(S, p_.t[:, 0:512], [p_], 512, VD[t0:t0 + 128, :])
                        p_ = project(S, hT_, Wd, 1024, 160)
                        cp("act", mkt.t[:, :, 64:96], p_.t[:, 128:160].unsqueeze(1).to_broadcast([128, 8, 32]), [p_], [mkt])
                        o_ = hnr(S, [(p_.t[:, 0:128], 0, 128)], [p_], 1, 128, gv(V_MKVAN, 128), None)
                        t_ = transp(S, o_, 1, 128)
                        o2 = S.ob[S.cnt["o"] % 2]
                        S.cnt["o"] += 1
                        for hh in range(2):
                            p2 = nextp(S)
                            mm(p2.t[:, 0:512], t_.t[:, 0:128], wkvb.t[:, hh * 512:(hh + 1) * 512], True, True, [t_, wkvb], [p2])
                            kv3 = p2.t[:, 0:512].rearrange("p (h d) -> p h d", h=4)
                            cp("dve", mkt.t[:, hh * 4:hh * 4 + 4, 0:64], kv3[:, :, 0:64], [p2], [mkt])
                            cp("dve", o2.t[:, hh * 256:hh * 256 + 256].rearrange("p (h d) -> p h d", h=4), kv3[:, :, 64:128], [p2], [o2])
                        dma("sp", VM[t0:t0 + 128, :], o2.t[:, 0:512], [o2], [])
                        o_ = hnr(S, [(mkt.t[:].rearrange("p h d -> p (h d)"), 0, 768)], [mkt], 8, 96, gv(V_MKN, 96), (64, 32, cosm, sinm, r_))
                        transp_store(S, o_, 8, 96, KM.rearrange("h d t -> d h t")[:, :, t0:t0 + 128])
                    run_interleaved(a1_tile, list(range(CFG['nA1'])))

                with fw.scope():
                    Wl = fw.sb("Wl", [128, 8, 3072], BF16)
                    for (dst0, c0, ncol) in ((0, 0, 1280), (1280, C_NQ, 1792)):
                        for s0 in range(0, ncol, 640):
                            sn = min(640, ncol - s0)
                            dma("pool", Wl.t[:, :, dst0 + s0:dst0 + s0 + sn],
                                w_in[:, c0 + s0:c0 + s0 + sn].rearrange("(j p) c -> p j c", p=128), [], [Wl])
                    L_WQ, L_WKV, L_DQ, L_NQ, L_NK, L_NV, L_MQA = 0, 512, 768, 1280, 1792, 2304, 2816

                    def a2_tile(i, S):
                        is_ctx = i >= 22
                        is_own = 3 <= i < 19
                        wantq = is_own or (is_ctx and want_ctx)
                        qs = (i - 3) if is_own else (16 + i - 22)
                        hT_, r_ = make_hT(S, local_src(i), rope_loc, i * 128, 1 if is_ctx else 0)
                        t0 = i * 128
                        q0 = qs * 128
                        cosh, sinh, cosm, sinm = r_.t[:, 0:32], r_.t[:, 32:64], r_.t[:, 64:80], r_.t[:, 80:96]
                        p_ = project(S, hT_, Wl, L_WKV, 256)
                        o_ = hnr(S, [(p_.t[:, 0:128], 0, 128)], [p_], 2, 64, gv(V_WKN, 64), (0, 64, cosh, sinh, r_))
                        transp_store(S, o_, 1, 128, KW[:, t0:t0 + 128].unsqueeze(1))
                        plain_store(S, p_.t[:, 128:256], [p_], 128, VW[t0:t0 + 128, :])
                        p_ = project(S, hT_, Wl, L_NK, 512)
                        o_ = hnr(S, [(p_.t[:, 0:512], 0, 512)], [p_], 8, 64, gv(V_NKN, 64), None)
                        transp_store(S, o_, 4, 128, KN.rearrange("(i q) t -> q i t", q=128)[:, :, t0:t0 + 128])
                        p_ = project(S, hT_, Wl, L_NV, 512)
                        plain_store(S, p_.t[:, 0:512], [p_], 512, VN[t0:t0 + 128, :])
                        if not wantq:
                            return
                        dma("sp", HTS[qs], hT_.t[:], [hT_], [])
                        for (lc, gain, dst, rope) in ((L_WQ, V_WQN, QW, True), (L_DQ, V_DQN, QD, True), (L_NQ, V_NQN, QN, False)):
                            p_ = project(S, hT_, Wl, lc, 512)
                            o_ = hnr(S, [(p_.t[:, 0:512], 0, 512)], [p_], 8, 64, gv(gain, 64), (0, 64, cosh, sinh, r_) if rope else None)
                            transp_store(S, o_, 4, 128, dst.rearrange("(i q) t -> q i t", q=128)[:, :, q0:q0 + 128])
                        p_ = project(S, hT_, Wl, L_MQA, 256)
                        o_ = hnr(S, [(p_.t[:, 0:256], 0, 256)], [p_], 1, 256, gv(V_MQAN, 256), None)
                        t_ = transp(S, o_, 2, 128)
                        pa_, pb_ = nextp(S), nextp(S)
                        for (pq, n0, nn) in ((pa_, 0, 512), (pb_, 512, 256)):
                            for j in range(2):
                                mm(pq.t[:, 0:nn], t_.t[:, j * 128:(j + 1) * 128], wqb.t[:, j, n0:n0 + nn], j == 0, j == 1, [t_, wqb], [pq])
                        o_ = hnr(S, [(pa_.t[:, 0:512], 0, 512), (pb_.t[:, 0:256], 512, 256)], [pa_, pb_], 8, 96, gv(V_MQN, 96), (64, 32, cosm, sinm, r_))
                        transp_store(S, o_, 8, 96, QM.rearrange("h d t -> d h t")[:, :, q0:q0 + 128])
                    run_interleaved(a2_tile, list(range(NT_LOC)) if CFG['nA2'] == NT_LOC else [0, 3, 22][:CFG['nA2']])

                with fw.scope():
                    Wg = [fw.sb("Wg%d" % i, [128, 8, 1024], BF16) for i in range(2)]
                    gt = [fw.sb("gt%d" % i, [128, 1024], BF16) for i in range(2)]
                    gp = [sets[0].pp[0], sets[0].pp[1], sets[1].pp[0], sets[1].pp[1]]
                    nq_tiles = NQT if want_ctx else 16
                    if CFG['nA2'] != NT_LOC:
                        nq_tiles = 0
                    HT = fw.sb("HT", [128, NQT, D], BF16)
                    for qs in range(nq_tiles):
                        dma("sp", HT.t[:, qs, :], HTS[qs], [], [HT])
                    n = 0
                    for gq in range(4):
                        W = Wg[gq % 2]
                        for s0 in (0, 512):
                            dma("pool", W.t[:, :, s0:s0 + 512],
                                w_in[:, C_G + gq * 1024 + s0:C_G + gq * 1024 + s0 + 512].rearrange("(j p) c -> p j c", p=128), [], [W])
                        for qs in range(nq_tiles):
                            g_ = gt[n % 2]
                            for hf in range(2):
                                p_ = gp[(2 * n + hf) % 4]
                                for j in range(8):
                                    mm(p_.t[:], HT.t[:, qs, j * 128:(j + 1) * 128], W.t[:, j, hf * 512:(hf + 1) * 512], j == 0, j == 7, [HT, W], [p_])
                                cp("act" if hf else "dve", g_.t[:, hf * 512:(hf + 1) * 512], p_.t[:], [p_], [g_])
                            n += 1
                            dma("sp", GATES[qs * 128:(qs + 1) * 128, gq * 1024:(gq + 1) * 1024], g_.t[:], [g_], [])

            if CFG['phases'] == 'all' or 'B' in CFG['phases']:
              with fw.scope():
                ES = fw.sb("ES", [128, 8], F32)
                act(ES.t[:], gv(V_SINK, 8), AF.Exp, [VEC], [ES])
                LM = fw.sb("LM", [128, 8], F32)
                lt = fw.sb("lt", [128, 128], F32)
                tt("dve", lt.t[:, 0:64], gv(V_LAM, 64), gv(V_LAM + 64, 64), ALU.mult, [VEC], [lt])
                tt("dve", lt.t[:, 64:128], gv(V_LAM + 128, 64), gv(V_LAM + 192, 64), ALU.mult, [VEC, lt], [lt])
                op("dve", lambda e: e.tensor_reduce(LM.t[:, 0:2], lt.t[:].rearrange("p (a d) -> p a d", a=2), AX.X, ALU.add), [lt], [LM])
                act(LM.t[:, 2:4], LM.t[:, 0:2], AF.Exp, [LM], [LM])
                tt("dve", LM.t[:, 4:5], LM.t[:, 2:3], LM.t[:, 3:4], ALU.subtract, [LM], [LM])
                ts("dve", LM.t[:, 5:6], LM.t[:, 4:5], -1.0, -lam_init, ALU.mult, ALU.add, [LM], [LM])
                NEGLAM = LM.t[:, 5:6]
                SG = fw.sb("SG", [128, 128], F32)
                ts("dve", SG.t[:], gv(V_SUBLN, 128), 1.0 - lam_init, None, ALU.mult, None, [VEC], [SG])

                acc = [fw.ps("acc%d" % i, [128, 512], F32) for i in range(4)]
                sps = [fw.ps("sps%d" % i, [128, 512], F32) for i in range(4)]
                pt = [fw.sb("pt%d" % i, [128, 512], BF16) for i in range(6)]
                fz = [fw.sb("fz%d" % i, [128, 8], F32) for i in range(4)]
                cs_ = [0]
                nqt = NQT if want_ctx else 16

                def attn(chunks, nacc, dv, scale, qn=512):
                    n = len(chunks)
                    GRP, LOOKG = 2, CFG.get('lookg', 2)
                    groups = [list(range(a, min(n, a + GRP))) for a in range(0, n, GRP)]
                    live = {}
                    for gi in range(len(groups) + LOOKG):
                        if gi < len(groups):
                            for it in groups[gi]:
                                ch = chunks[it]
                                k = cs_[0]
                                cs_[0] += 1
                                s_, p_ = sps[k % 4], pt[k % 6]
                                for (l_ap, r_ap, c0, ncl) in ch['mms']:
                                    mm(s_.t[:, c0:c0 + ncl], l_ap, r_ap, True, True, ch['rb'], [s_])
                                live[it] = (s_, p_)
                            for it in groups[gi]:
                                ch = chunks[it]
                                s_, p_ = live[it]
                                act(p_.t[:, :qn], s_.t[:, :qn], AF.Exp, [s_], [p_], scale=scale)
                                for mi, (m_ap, m_buf) in enumerate(ch.get('masks', ())):
                                    p3 = p_.t[:, :].rearrange("p (h q) -> p h q", h=4)
                                    tt("pool" if mi % 2 == 0 else "dve", p3, p3, m_ap, ALU.mult, [p_, m_buf], [p_])
                        if gi >= LOOKG:
                            for ci in groups[gi - LOOKG]:
                                ch = chunks[ci]
                                s_, p_ = live.pop(ci)
                                for j in range(nacc):
                                    mm(acc[j].t[:, 0:dv + 1], p_.t[:, j * 128:(j + 1) * 128], ch['v'][j], ci == 0, ci == n - 1,
                                       [p_] + ch['vb'], [acc[j]])

                def fin_simple(j, dst_ap, dst_buf, dv, sink_ap=None):
                    z_ = fz[j]
                    if sink_ap is not None:
                        tt("dve", z_.t[:, 0:1], acc[j].t[:, dv:dv + 1], sink_ap, ALU.add, [acc[j], ES], [z_])
                        op("dve", lambda e: e.reciprocal(z_.t[:, 1:2], z_.t[:, 0:1]), [z_], [z_])
                    else:
                        op("dve", lambda e: e.reciprocal(z_.t[:, 1:2], acc[j].t[:, dv:dv + 1]), [acc[j]], [z_])
                    ts("dve", dst_ap, acc[j].t[:, 0:dv], z_.t[:, 1:2], None, ALU.mult, None, [acc[j], z_], [dst_buf])

                Ysb = [fw.sb("Ysb%d" % i, [128, NQT, 512], BF16) for i in range(2)]

                def store_branch(n, ybuf):
                    dma("sp", Y[0:nqt * 128, n * 512:(n + 1) * 512].rearrange("(t p) c -> p t c", p=128), ybuf.t[:, 0:nqt, :], [ybuf], [])

                if 'nodense' not in CFG['phases']:
                  with fw.scope():
                    kb = [fw.sb("kb%d" % i, [96, NT_ALL * 128], BF16) for i in range(2)]
                    vb = [fw.sb("vb%d" % i, [128, NT_ALL, 129], BF16) for i in range(2)]
                    qb_ = [fw.sb("qb%d" % i, [96, NQT * 128], BF16) for i in range(2)]
                    D1 = fw.sb("D1", [128, NQT, 128], F32)
                    dtm = [fw.sb("dtm%d" % i, [128, 128], F32) for i in range(2)]
                    dsq = fw.sb("dsq", [128, 128], F32)
                    hcount = [0]

                    def dense_head(KTsrc, d, QTsrc, V1, dv, scale, fin):
                        hi = hcount[0]
                        hcount[0] += 1
                        KT, QT = kb[hi % 2], qb_[hi % 2]
                        dma("sp", KT.t[0:d, :], KTsrc, [], [KT])
                        dma("sp", QT.t[0:d, :], QTsrc, [], [QT])
                        for qblk in range(4):
                            chunks = [dict(mms=[(KT.t[0:d, c * 128:(c + 1) * 128], QT.t[0:d, qblk * 512:(qblk + 1) * 512], 0, 512)],
                                           rb=[KT, QT], v=[V1.t[:, c, 0:dv + 1]] * 4, vb=[V1]) for c in range(CFG.get('nkc', NT_ALL))]
                            attn(chunks, 4, dv, scale)
                            for j in range(4):
                                fin(j, qblk * 4 + j)
                        if want_ctx:
                            chunks = [dict(mms=[(KT.t[0:d, c * 128:(c + 1) * 128], QT.t[0:d, 2048:2304], 0, 256)],
                                           rb=[KT, QT], v=[V1.t[:, c, 0:dv + 1]] * 2, vb=[V1]) for c in range(2)]
                            attn(chunks, 2, dv, scale, qn=256)
                            for j in range(2):
                                fin(j, 16 + j)

                    Yd = Ysb[0]
                    for hd in range(4):
                        V1 = vb[hd % 2]
                        dma("sp", V1.t[:, :, 0:128], VD[:, hd * 128:(hd + 1) * 128].rearrange("(c p) d -> p c d", p=128), [], [V1])
                        op("pool", lambda e: e.memset(V1.t[:, :, 128:129], 1.0), [], [V1])
                        for i2 in range(2):
                            hs = 2 * hd + i2

                            def fin(j, qt, i2=i2, hd=hd):
                                if i2 == 0:
                                    fin_simple(j, D1.t[:, qt, :], D1, 128)
                                    return
                                t_ = dtm[j % 2]
                                z_ = fz[j]
                                fin_simple(j, t_.t[:], t_, 128)
                                stt("dve", t_.t[:], t_.t[:], NEGLAM, D1.t[:, qt, :], ALU.mult, ALU.add, [t_, LM, D1], [t_])
                                tt("pool", dsq.t[:], t_.t[:], t_.t[:], ALU.mult, [t_], [dsq])
                                op("dve", lambda e: e.tensor_reduce(z_.t[:, 2:3], dsq.t[:], AX.X, ALU.add), [dsq], [z_])
                                act(z_.t[:, 3:4], z_.t[:, 2:3], AF.Sqrt, [z_], [z_], bias=EPS, scale=1.0 / 128)
                                op("dve", lambda e: e.reciprocal(z_.t[:, 4:5], z_.t[:, 3:4]), [z_], [z_])
                                stt("dve", Yd.t[:, qt, hd * 128:(hd + 1) * 128], t_.t[:], z_.t[:, 4:5], SG.t[:], ALU.mult, ALU.mult,
                                    [t_, z_, SG], [Yd])
                            dense_head(KD[hs * 64:(hs + 1) * 64, :], 64, QD[hs * 64:(hs + 1) * 64, :], V1, 128, 0.125, fin)
                    store_branch(1, Yd)

                    Ym = Ysb[1]
                    for h in range(8):
                        V1 = vb[h % 2]
                        dma("sp", V1.t[:, :, 0:64], VM[:, h * 64:(h + 1) * 64].rearrange("(c p) d -> p c d", p=128), [], [V1])
                        op("pool", lambda e: e.memset(V1.t[:, :, 64:65], 1.0), [], [V1])

                        def fin(j, qt, h=h):
                            fin_simple(j, Ym.t[:, qt, h * 64:(h + 1) * 64], Ym, 64)
                        dense_head(KM[h], 96, QM[h], V1, 64, 96.0 ** -0.5, fin)
                    store_branch(3, Ym)

                with fw.scope():
                    Yw = Ysb[0]
                    WM = fw.sb("WM", [128, 4, 128], BF16)
                    dma("pool", WM.t[:], wmask.rearrange("m k q -> k m q"), [], [WM])
                    kw_ = [fw.sb("kw%d" % i, [64, NT_LOC * 128], BF16) for i in range(2)]
                    vw_ = [fw.sb("vw%d" % i, [128, NT_LOC, 65], BF16) for i in range(2)]
                    qw_ = [fw.sb("qw%d" % i, [64, NQT, 4, 128], BF16) for i in range(2)]
                    for g in range(2):
                        KT, V1, QT = kw_[g], vw_[g], qw_[g]
                        dma("sp", KT.t[:], KW[g * 64:(g + 1) * 64, :], [], [KT])
                        dma("sp", V1.t[:, :, 0:64], VW[:, g * 64:(g + 1) * 64].rearrange("(c p) d -> p c d", p=128), [], [V1])
                        op("pool", lambda e: e.memset(V1.t[:, :, 64:65], 1.0), [], [V1])
                        for hq in range(4):
                            dma("sp", QT.t[:, :, hq, :], QW[(g * 4 + hq) * 64:(g * 4 + hq + 1) * 64, :].rearrange("d (t q) -> d t q", q=128), [], [QT])
                        for qt in range(nqt):
                            rhs = QT.t[0:64, qt, :, :].rearrange("p h q -> p (h q)")
                            if qt < 16:
                                sl = [(qt + 2, WM.t[:, 0 if qt == 0 else 1, :]), (qt + 3, None), (qt + 4, WM.t[:, 3 if qt == 15 else 2, :]), (22, None), (23, None)]
                            else:
                                sl = [(22, None), (23, None)]
                            chunks = []
                            for (slot, m) in sl:
                                ch = dict(mms=[(KT.t[0:64, slot * 128:(slot + 1) * 128], rhs, 0, 512)], rb=[KT, QT],
                                          v=[V1.t[:, slot, 0:65]] * 4, vb=[V1])
                                if m is not None:
                                    ch['masks'] = [(m.unsqueeze(1).to_broadcast([128, 4, 128]), WM)]
                                chunks.append(ch)
                            attn(chunks, 4, 64, 0.125)
                            for j in range(4):
                                h = g * 4 + j
                                fin_simple(j, Yw.t[:, qt, h * 64:(h + 1) * 64], Yw, 64, sink_ap=ES.t[:, h:h + 1])
                    store_branch(0, Yw)

                with fw.scope():
                    Yn = Ysb[1]
                    NVm = fw.sb("NVm", [128, 5, 7, 128], BF16)
                    dma("pool", NVm.t[:], natv.rearrange("a c k q -> k a c q"), [], [NVm])
                    ebf = fw.sb("ebf", [128, 7, 4, 128], F32)
                    EB = [fw.sb("EB%d" % i, [128, 7, 4, 128], BF16) for i in range(2)]
                    kn_ = [fw.sb("kn%d" % i, [64, 4, NT_LOC * 128], BF16) for i in range(2)]
                    vn_ = [fw.sb("vn%d" % i, [128, NT_LOC, 4, 65], BF16) for i in range(2)]
                    qn_ = [fw.sb("qn%d" % i, [64, NQT, 4, 128], BF16) for i in range(2)]
                    for hh in range(2):
                        KT, V1, QT, EBh = kn_[hh], vn_[hh], qn_[hh], EB[hh]
                        for c in range(7):
                            dma("sp", ebf.t[:, c, :, :], natb[c, :, hh * 4:(hh + 1) * 4, :], [], [ebf])
                        act(EBh.t[:].rearrange("p c h q -> p (c h q)"), ebf.t[:].rearrange("p c h q -> p (c h q)"), AF.Exp, [ebf], [EBh])
                        for j in range(4):
                            h = hh * 4 + j
                            dma("sp", KT.t[:, j, :], KN[h * 64:(h + 1) * 64, :], [], [KT])
                            dma("sp", V1.t[:, :, j, 0:64], VN[:, h * 64:(h + 1) * 64].rearrange("(c p) d -> p c d", p=128), [], [V1])
                            dma("sp", QT.t[:, :, j, :], QN[h * 64:(h + 1) * 64, :].rearrange("d (t q) -> d t q", q=128), [], [QT])
                        op("pool", lambda e: e.memset(V1.t[:, :, :, 64:65], 1.0), [], [V1])
                        for qt in range(nqt):
                            chunks = []
                            if qt < 16:
                                cls = {0: 0, 1: 1, 14: 3, 15: 4}.get(qt, 2)
                                lst = [(qt + c + 3, c + 3) for c in range(-3, 4)] + [(22, None), (23, None)]
                            else:
                                lst = [(22, None), (23, None)]
                            for (slot, ci) in lst:
                                ch = dict(mms=[(KT.t[0:64, j, slot * 128:(slot + 1) * 128], QT.t[0:64, qt, j, :], j * 128, 128) for j in range(4)],
                                          rb=[KT, QT], v=[V1.t[:, slot, j, 0:65] for j in range(4)], vb=[V1])
                                if ci is not None:
                                    ch['masks'] = [(EBh.t[:, ci, :, :], EBh),
                                                   (NVm.t[:, cls, ci, :].unsqueeze(1).to_broadcast([128, 4, 128]), NVm)]
                                chunks.append(ch)
                            attn(chunks, 4, 64, 0.125)
                            for j in range(4):
                                h = hh * 4 + j
                                fin_simple(j, Yn.t[:, qt, h * 64:(h + 1) * 64], Yn, 64)
                    store_branch(2, Yn)
            if CFG['phases'] == 'all' or 'C' in CFG['phases']:
              with fw.scope():
                nqt = NQT if want_ctx else 16
                H2T = fw.sb("H2T", [128, 8, NQT * 128], BF16)
                WT = fw.sb("WT", [128, NQT, 16], F32)
                xslot = lambda qt: (3 + qt) if qt < 16 else (22 + qt - 16)
                with fw.scope():
                    G1 = [fw.sb("G1_%d" % w, [128, D], F32) for w in range(2)]
                    A2 = [fw.sb("A2_%d" % w, [128, D], F32) for w in range(2)]
                    SH2 = [fw.sb("SH2_%d" % w, [128, D], F32) for w in range(2)]
                    for w in range(2):
                        load_mod(G1[w], w, M_G1)
                        load_mod(A2[w], w, M_A2)
                        load_mod(SH2[w], w, M_SH2)
                    wbr = fw.sb("wbr", [128, 16, D], BF16)
                    wo = fw.sb("wo", [128, 8, D], BF16)
                    rw = fw.sb("rw", [128, 8, 16], F32)
                    for q4 in range(4):
                        dma("pool", wbr.t[:, q4 * 4:(q4 + 1) * 4, :], w_branch[q4 * 512:(q4 + 1) * 512, :].rearrange("(j p) c -> p j c", p=128), [], [wbr])
                    for q2 in range(2):
                        dma("pool", wo.t[:, q2 * 4:(q2 + 1) * 4, :], w_out[q2 * 512:(q2 + 1) * 512, :].rearrange("(j p) c -> p j c", p=128), [], [wo])
                    dma("sp", rw.t[:], router_w.rearrange("(j p) e -> p j e", p=128), [], [rw])
                    yt = [fw.sb("yt%d" % i, [128, 2048], BF16) for i in range(2)]
                    gt_ = [fw.sb("gtc%d" % i, [128, 4096], BF16) for i in range(2)]
                    sg = fw.sb("sg", [128, 4096], BF16)
                    YT = fw.sb("YT", [128, 16, 128], BF16)
                    macc = fw.sb("macc", [128, D], F32)
                    mtmp = fw.sb("mtmp", [128, 512], F32)
                    mbf = fw.sb("mbf", [128, D], BF16)
                    mT = fw.sb("mT", [128, 8, 128], BF16)
                    xin = [fw.sb("xin%d" % i, [128, D], F32) for i in range(2)]
                    xn = [fw.sb("xn%d" % i, [128, D], F32) for i in range(2)]
                    junk2 = fw.sb("junk2", [128, D], F32)
                    h2 = fw.sb("h2", [128, D], F32)
                    h2tf = fw.sb("h2tf", [128, 8, 128], F32)
                    s2 = [fw.sb("s2_%d" % i, [128, 8], F32) for i in range(2)]
                    rr = [fw.sb("rr%d" % i, [128, 160], F32) for i in range(2)]
                    ptc = [fw.ps("ptc%d" % i, [128, D], BF16) for i in range(2)]
                    pm = [fw.ps("pm%d" % i, [128, 512], F32) for i in range(3)]
                    pf = [fw.ps("pf%d" % i, [128, 512], F32) for i in range(2)]
                    pr = fw.ps("pr", [128, 16], F32)
                    pmc = [0]
                    for qt in range(nqt):
                        which = 1 if qt >= 16 else 0
                        y_, g_, x_, xn_, s_, r_ = yt[qt % 2], gt_[qt % 2], xin[qt % 2], xn[qt % 2], s2[qt % 2], rr[qt % 2]
                        dma("sp", y_.t[:], Y[qt * 128:(qt + 1) * 128, :], [], [y_])
                        dma("sp", g_.t[:], GATES[qt * 128:(qt + 1) * 128, :], [], [g_])
                        dma("sp", x_.t[:], local_src(xslot(qt)), [], [x_])
                        act(sg.t[:], g_.t[:], AF.Sigmoid, [g_], [sg])
                        for hf in range(2):
                            p_ = ptc[hf]
                            for i in range(8):
                                tr(p_.t[:, i * 128:(i + 1) * 128], y_.t[:, (hf * 8 + i) * 128:(hf * 8 + i + 1) * 128], identb.t[:], [y_, identb], [p_])
                            cp("dve" if hf else "act", YT.t[:, hf * 8:(hf + 1) * 8, :].rearrange("p a b -> p (a b)"), p_.t[:], [p_], [YT])
                        for n in range(4):
                            for half in range(2):
                                p_ = pm[pmc[0] % 3]
                                pmc[0] += 1
                                for k in range(4):
                                    mm(p_.t[:], YT.t[:, n * 4 + k, :], wbr.t[:, n * 4 + k, half * 512:(half + 1) * 512], k == 0, k == 3, [YT, wbr], [p_])
                                sgs = sg.t[:, n * 1024 + half * 512:n * 1024 + (half + 1) * 512]
                                if n == 0:
                                    tt("dve", macc.t[:, half * 512:(half + 1) * 512], p_.t[:], sgs, ALU.mult, [p_, sg], [macc])
                                else:
                                    tt("dve", mtmp.t[:], p_.t[:], sgs, ALU.mult, [p_, sg], [mtmp])
                                    tt("pool", macc.t[:, half * 512:(half + 1) * 512], macc.t[:, half * 512:(half + 1) * 512], mtmp.t[:], ALU.add, [macc, mtmp], [macc])
                        cp("act", mbf.t[:], macc.t[:], [macc], [mbf])
                        p_ = ptc[0]
                        for i in range(8):
                            tr(p_.t[:, i * 128:(i + 1) * 128], mbf.t[:, i * 128:(i + 1) * 128], identb.t[:], [mbf, identb], [p_])
                        cp("dve", mT.t[:].rearrange("p a b -> p (a b)"), p_.t[:], [p_], [mT])
                        for half in range(2):
                            p_ = pm[pmc[0] % 3]
                            pmc[0] += 1
                            for j in range(8):
                                mm(p_.t[:], mT.t[:, j, :], wo.t[:, j, half * 512:(half + 1) * 512], j == 0, j == 7, [mT, wo], [p_])
                            hs_ = slice(half * 512, (half + 1) * 512)
                            tt("dve", mtmp.t[:], p_.t[:], G1[which].t[:, hs_], ALU.mult, [p_, G1[which]], [mtmp])
                            tt("pool", xn_.t[:, hs_], mtmp.t[:], x_.t[:, hs_], ALU.add, [mtmp, x_], [xn_])
                        dma("sp", XN[qt * 128:(qt + 1) * 128, :], xn_.t[:], [xn_], [])
                        act(junk2.t[:], xn_.t[:], AF.Square, [xn_], [junk2, s_], accum_out=s_.t[:, 0:1])
                        act(s_.t[:, 1:2], s_.t[:, 0:1], AF.Sqrt, [s_], [s_], bias=EPS, scale=1.0 / D)
                        op("dve", lambda e: e.reciprocal(s_.t[:, 2:3], s_.t[:, 1:2]), [s_], [s_])
                        stt("dve", h2.t[:], xn_.t[:], s_.t[:, 2:3], A2[which].t[:], ALU.mult, ALU.mult, [xn_, s_, A2[which]], [h2])
                        tt("pool", h2.t[:], h2.t[:], SH2[which].t[:], ALU.add, [h2, SH2[which]], [h2])
                        for hf in range(2):
                            p_ = pf[hf]
                            for i in range(4):
                                j = hf * 4 + i
                                tr(p_.t[:, i * 128:(i + 1) * 128], h2.t[:, j * 128:(j + 1) * 128], identf.t[:], [h2, identf], [p_])
                            cp("dve" if hf else "act", h2tf.t[:, hf * 4:(hf + 1) * 4, :].rearrange("p a b -> p (a b)"), p_.t[:], [p_], [h2tf])
                        cp("pool", H2T.t[:, :, qt * 128:(qt + 1) * 128], h2tf.t[:], [h2tf], [H2T])
                        for j in range(8):
                            mm(pr.t[:], h2tf.t[:, j, :], rw.t[:, j, :], j == 0, j == 7, [h2tf, rw], [pr])
                        R = lambda a, b: r_.t[:, a:b]
                        v4 = lambda ap: ap.rearrange("p (g e) -> p g e", g=4)
                        act(R(0, 16), pr.t[:], AF.Sigmoid, [pr], [r_])
                        tt("dve", R(16, 32), R(0, 16), gv(V_RB, 16), ALU.add, [r_, VEC], [r_])
                        op("dve", lambda e: e.tensor_reduce(R(32, 36), v4(R(16, 32)), AX.X, ALU.max), [r_], [r_])
                        tt("dve", v4(R(48, 64)), v4(R(16, 32)), R(32, 36).unsqueeze(2).to_broadcast([128, 4, 4]), ALU.is_equal, [r_], [r_])
                        stt("dve", R(64, 80), R(48, 64), -1e9, R(16, 32), ALU.mult, ALU.add, [r_], [r_])
                        op("dve", lambda e: e.tensor_reduce(R(36, 40), v4(R(64, 80)), AX.X, ALU.max), [r_], [r_])
                        tt("dve", R(40, 44), R(32, 36), R(36, 40), ALU.add, [r_], [r_])
                        op("dve", lambda e: e.tensor_reduce(R(44, 45), R(40, 44), AX.X, ALU.max), [r_], [r_])
                        ts("dve", R(80, 84), R(40, 44), R(44, 45), None, ALU.is_equal, None, [r_], [r_])
                        tt("dve", v4(R(96, 112)), v4(R(16, 32)), R(36, 40).unsqueeze(2).to_broadcast([128, 4, 4]), ALU.is_ge, [r_], [r_])
                        tt("dve", v4(R(96, 112)), v4(R(96, 112)), R(80, 84).unsqueeze(2).to_broadcast([128, 4, 4]), ALU.mult, [r_], [r_])
                        tt("dve", R(112, 128), R(96, 112), R(0, 16), ALU.mult, [r_], [r_])
                        op("dve", lambda e: e.tensor_reduce(R(128, 129), R(112, 128), AX.X, ALU.add), [r_], [r_])
                        op("dve", lambda e: e.reciprocal(R(129, 130), R(128, 129)), [r_], [r_])
                        ts("dve", WT.t[:, qt, :], R(112, 128), R(129, 130), None, ALU.mult, None, [r_], [WT])
                    if False:
                        dbg_wt = dscr("dbg_wt", [128, NQT * 16], F32)
                        dma("sp", dbg_wt, WT.t[:].rearrange("p a b -> p (a b)"), [WT], [])
                with fw.scope():
                    Ft = [fw.sb("F%d" % i, [128, D], F32) for i in range(nqt)]
                    for f_ in Ft:
                        op("pool", lambda e: e.memset(f_.t[:], 0.0), [], [f_])
                    w1 = [fw.sb("w1_%d" % i, [128, 8, 512], BF16) for i in range(2)]
                    w3 = [fw.sb("w3_%d" % i, [128, 8, 512], BF16) for i in range(2)]
                    w2 = [fw.sb("w2_%d" % i, [128, 4, D], BF16) for i in range(2)]
                    GT = [fw.sb("GT%d" % i, [128, 4, 512], BF16) for i in range(2)]
                    sl = [fw.sb("sl%d" % i, [128, 512], F32) for i in range(2)]
                    pa = [fw.ps("pa%d" % i, [128, 512], F32) for i in range(2)]
                    pb = [fw.ps("pb%d" % i, [128, 512], F32) for i in range(2)]
                    py = [fw.ps("py%d" % i, [128, 512], F32) for i in range(3)]
                    blocks = [(i * 512, 512) for i in range(4)] + ([(2048, 256)] if want_ctx else [])
                    c1, c2 = [0], [0]
                    for e_ in range(CFG.get('nexp', 16)):
                        a1, a3, a2 = w1[e_ % 2], w3[e_ % 2], w2[e_ % 2]
                        dma("pool", a1.t[:], moe_w1[e_].rearrange("(j p) c -> p j c", p=128), [], [a1])
                        dma("pool", a3.t[:], moe_w3[e_].rearrange("(j p) c -> p j c", p=128), [], [a3])
                        dma("pool", a2.t[:], moe_w2[e_].rearrange("(j p) c -> p j c", p=128), [], [a2])
                        for bi_, (t0, nb) in enumerate(blocks):
                            G_ = GT[(e_ * 5 + bi_) % 2]
                            for m in range(4):
                                pa_, pb_, sl_ = pa[c1[0] % 2], pb[c1[0] % 2], sl[c1[0] % 2]
                                c1[0] += 1
                                for k in range(8):
                                    mm(pa_.t[:, :nb], a1.t[:, k, m * 128:(m + 1) * 128], H2T.t[:, k, t0:t0 + nb], k == 0, k == 7, [a1, H2T], [pa_])
                                for k in range(8):
                                    mm(pb_.t[:, :nb], a3.t[:, k, m * 128:(m + 1) * 128], H2T.t[:, k, t0:t0 + nb], k == 0, k == 7, [a3, H2T], [pb_])
                                act(sl_.t[:, :nb], pa_.t[:, :nb], AF.Silu, [pa_], [sl_])
                                tt("dve", G_.t[:, m, :nb], sl_.t[:, :nb], pb_.t[:, :nb], ALU.mult, [sl_, pb_], [G_])
                            for j in range(nb // 128):
                                qt = t0 // 128 + j
                                for half in range(2):
                                    py_ = py[c2[0] % 3]
                                    c2[0] += 1
                                    for m in range(4):
                                        mm(py_.t[:], G_.t[:, m, j * 128:(j + 1) * 128], a2.t[:, m, half * 512:(half + 1) * 512], m == 0, m == 3, [G_, a2], [py_])
                                    fs = Ft[qt].t[:, half * 512:(half + 1) * 512]
                                    stt("dve", fs, py_.t[:], WT.t[:, qt, e_:e_ + 1], fs, ALU.mult, ALU.add, [py_, WT, Ft[qt]], [Ft[qt]])
                    G2 = [fw.sb("G2_%d" % w, [128, D], F32) for w in range(2)]
                    for w in range(2):
                        load_mod(G2[w], w, M_G2)
                    xo = [fw.sb("xo%d" % i, [128, D], F32) for i in range(2)]
                    for qt in range(nqt):
                        which = 1 if qt >= 16 else 0
                        x_ = xo[qt % 2]
                        dma("sp", x_.t[:], XN[qt * 128:(qt + 1) * 128, :], [], [x_])
                        tt("dve", Ft[qt].t[:], Ft[qt].t[:], G2[which].t[:], ALU.mult, [Ft[qt], G2[which]], [Ft[qt]])
                        tt("pool", x_.t[:], x_.t[:], Ft[qt].t[:], ALU.add, [x_, Ft[qt]], [x_])
                        if lidx == 1:
                            dma("sp", x_out[qt * 128:(qt + 1) * 128, :], x_.t[:], [x_], [], out=True)
                        elif qt < 16:
                            dma("sp", X1[(seg * 16 + qt) * 128:(seg * 16 + qt + 1) * 128, :], x_.t[:], [x_], [])
                        else:
                            dma("sp", XC1[(qt - 16) * 128:(qt - 15) * 128, :], x_.t[:], [x_], [])

        for lidx in layers:
            for k_, seg in enumerate(segs0 if lidx == 0 else (0,)):
                layer_pass(lidx, seg, lidx == 0 and seg == 0, k_ == 0)
        fw.finish()
    return nc


def _rope_tables():
    t = np.arange(SEQ)
    rows = (t // 64).astype(np.float32)
    cols = (t % 64).astype(np.float32)
    out = []
    for dim in (64, 32):
        quarter = dim // 4
        inv = np.exp(-np.log(10000.0) * np.arange(quarter, dtype=np.float32) / quarter).astype(np.float32)
        ang = np.concatenate([rows[:, None] * inv, cols[:, None] * inv], axis=-1).astype(np.float32)
        out += [np.cos(ang).astype(np.float32), np.sin(ang).astype(np.float32)]
    tab = np.concatenate(out, axis=1)
    ident = np.zeros((1, 96), np.float32)
    ident[0, 0:32] = 1.0
    ident[0, 64:80] = 1.0
    return tab, ident


def _nat_consts(s):
    col = np.arange(64)
    cs = np.clip(col - 8, 0, 48)
    col_ok = (col[None, :] >= cs[:, None]) & (col[None, :] < cs[:, None] + 16)
    reps = (0, 1, 7, 14, 15)
    V = np.zeros((5, 7, 128, 128), np.float32)
    for ci, tl in enumerate(reps):
        t = 16 * s + tl
        for c in range(-3, 4):
            u = t + c
            if not (0 <= u < 64):
                continue
            for kr in range(2):
                for qr in range(2):
                    krow, qrow = 2 * u + kr, 2 * t + qr
                    st = min(max(qrow - 4, 0), 120)
                    if st <= krow < st + 8:
                        V[ci, c + 3, kr * 64:(kr + 1) * 64, qr * 64:(qr + 1) * 64] = col_ok.T
    return V


def _nat_bias_index():
    k = np.arange(128)
    q = np.arange(128)
    kr, kc = k // 64, k % 64
    qr, qc = q // 64, q % 64
    dcol = np.clip(kc[:, None] - qc[None, :] + 15, 0, 30)
    idx_r = np.zeros((7, 128, 128), np.int64)
    for c in range(-3, 4):
        idx_r[c + 3] = np.clip(2 * c + kr[:, None] - qr[None, :] + 7, 0, 14)
    return idx_r, np.broadcast_to(dcol, (7, 128, 128))


def prep_fused(inp):
    f32 = lambda a: np.ascontiguousarray(a, dtype=np.float32)
    tab, rid = _rope_tables()
    x = np.asarray(inp['x'], dtype=np.float32)
    xc = np.asarray(inp['ctx'], dtype=np.float32)
    idx_r, idx_c = _nat_bias_index()
    tri_prev = (np.arange(128)[:, None] >= np.arange(128)[None, :]).astype(np.float32)
    tri_next = (np.arange(128)[:, None] <= np.arange(128)[None, :]).astype(np.float32)
    common = dict(ident=np.eye(128, dtype=np.float32), router_w=f32(inp['router_w']))
    for l in range(2):
        vecs = [inp['win_q_norm'][l], inp['win_k_norm'][l], inp['dif_q_norm'][l], inp['dif_k_norm'][l],
                inp['nat_q_norm'][l], inp['nat_k_norm'][l], inp['mla_q_norm'][l], inp['mla_k_norm'][l],
                inp['mla_q_a_norm'][l], inp['mla_kv_a_norm'][l], inp['dif_subln'][l], inp['win_sink'][l],
                inp['dif_lambda'][l].reshape(-1), inp['router_b'], inp['ada_b'][l], inp['norm1_g'][l], inp['norm2_g'][l]]
        vec = f32(np.concatenate([np.asarray(v).reshape(-1) for v in vecs])[None, :])
        assert vec.shape[1] == NVEC
        rpb = np.asarray(inp['nat_rpb'][l])
        common.update({
            'vec_%d' % l: vec, 'ada_w_%d' % l: f32(inp['ada_w'][l]), 'w_in_%d' % l: f32(inp['w_in'][l]),
            'wq_b_%d' % l: f32(inp['mla_wq_b'][l]), 'wkv_b_%d' % l: f32(inp['mla_wkv_b'][l]),
            'w_branch_%d' % l: f32(np.asarray(inp['w_branch'][l]).reshape(2048, D)), 'w_out_%d' % l: f32(inp['w_out'][l]),
            'moe_w1_%d' % l: f32(inp['moe_w1'][l]), 'moe_w3_%d' % l: f32(inp['moe_w3'][l]), 'moe_w2_%d' % l: f32(inp['moe_w2'][l]),
            'natb_%d' % l: f32(np.transpose(rpb[:, idx_r, idx_c], (1, 2, 0, 3)))})
    maps = []
    zeros_h = np.zeros((384, D), np.float32)
    rid384 = np.repeat(rid, 384, 0)
    rc = np.repeat(rid, NCTX, 0)
    for core in range(8):
        b, s = core // 4, core % 4
        sigma = [s] + [g for g in range(4) if g != s]
        m = dict(common)
        m['xall'] = f32(np.concatenate([xc[b]] + [x[b, 2048 * g:2048 * (g + 1)] for g in sigma], 0))
        m['rope_all'] = f32(np.concatenate([rc] + [tab[2048 * g:2048 * (g + 1)] for g in sigma], 0))
        for j, g in enumerate(sigma):
            o0 = 2048 * g
            hb = x[b, o0 - 384:o0] if g > 0 else zeros_h
            ha = x[b, o0 + 2048:o0 + 2048 + 384] if g < 3 else zeros_h
            rb = tab[o0 - 384:o0] if g > 0 else rid384
            ra = tab[o0 + 2048:o0 + 2048 + 384] if g < 3 else rid384
            m['xloc%d' % j] = f32(np.concatenate([hb, x[b, o0:o0 + 2048], ha, xc[b]], 0))
            m['rope_loc%d' % j] = f32(np.concatenate([rb, tab[o0:o0 + 2048], ra, rc], 0))
            m['wmask%d' % j] = f32(np.stack([tri_prev if g > 0 else np.zeros_like(tri_prev), tri_prev, tri_next,
                                             tri_next if g < 3 else np.zeros_like(tri_next)], 0))
            m['natv%d' % j] = f32(_nat_consts(g))
        hs = np.zeros((128, 8), np.float32)
        for r in (1, 2, 3):
            if sigma[r] == s - 1:
                hs[:, r] = 1.0
            if sigma[r] == s + 1:
                hs[:, 4 + r] = 1.0
        m['hsel'] = hs
        m['cvec'] = f32(np.stack([inp['c'][b], inp['c_ctx']], 0).reshape(2, 8, 128).transpose(2, 0, 1).reshape(128, 16))
        maps.append(m)
    return maps


def kernel(**inputs):
    inp = {k: np.asarray(v) for k, v in inputs.items()}
    nc = build_fused()
    maps = prep_fused(inp)
    res = run_bass_kernel_spmd(nc, maps, core_ids=list(range(8)))
    out = np.empty((2, SEQ, D), np.float32)
    for core in range(8):
        b, s = core // 4, core % 4
        out[b, 2048 * s:2048 * (s + 1)] = np.asarray(res.results[core]['x_out'])
    return out
```
